# Optimizing a Trainium2 kernel written in Bass

```python
import math
import jax, jax.numpy as jnp
from jax import lax
import numpy as np

D_MODEL = 2048
BATCH = 4
SEQ = 4096
DEPTH = 4

N_HEADS = 16
HEAD_DIM = D_MODEL // N_HEADS
D_ATTN = N_HEADS * HEAD_DIM
ROPE_THETA = 10000.0
Q_BLOCK = 128
MASK_VALUE = -1e30
LN_EPS = 1e-5
N_MIXERS = 4
DILATED_GROUPS = ((128, 1), (512, 4), (2048, 16))
B_KV_HEADS = 4
GQA_GROUP = N_HEADS // B_KV_HEADS
KV_WIDTH = B_KV_HEADS * HEAD_DIM
IDX_HEADS = 16
IDX_DIM = 64
IDX_TOPK_MAX = 256
DSA_SIZES = (D_ATTN, KV_WIDTH, KV_WIDTH, IDX_HEADS * IDX_DIM, IDX_DIM, IDX_HEADS)
FORGET_BIAS_MEAN = 3.0
MOBA_BLOCK = 256
MOBA_TOPK = 3
MOBA_Q_CHUNK = 16
N_EXPERTS = 32
TOP_K = 4
D_EXPERT = 1024
SWIGLU_ALPHA = 1.702
SWIGLU_LIMIT = 7.0
MOE_ROW_BLOCK = 128
DEEPNORM_ALPHA = (2 * DEPTH) ** 0.25
DEEPNORM_BETA = (8 * DEPTH) ** -0.25
MIXER_IN_WIDTHS = (3 * D_ATTN, sum(DSA_SIZES), 3 * D_ATTN + N_HEADS, 3 * D_ATTN)

kernel_name = 'hybrid_dilated_dsa_fox_moba_moe_deepnorm'


def layer_norm(x, g, b):
    xf = x.astype(jnp.float32)
    mu = xf.mean(-1, keepdims=True)
    var = jnp.mean(jnp.square(xf - mu), -1, keepdims=True)
    y = (xf - mu) * lax.rsqrt(var + LN_EPS) * g.astype(jnp.float32) + b.astype(jnp.float32)
    return y.astype(x.dtype)


def rope(t, pos):
    half = t.shape[-1] // 2
    inv_freq = ROPE_THETA ** (-jnp.arange(half, dtype=jnp.float32) / half)
    ang = pos.astype(jnp.float32)[:, None] * inv_freq[None, :]
    cos = jnp.cos(ang)[:, None, :]
    sin = jnp.sin(ang)[:, None, :]
    t1 = t[..., :half].astype(jnp.float32)
    t2 = t[..., half:].astype(jnp.float32)
    return jnp.concatenate([t1 * cos - t2 * sin, t2 * cos + t1 * sin], -1).astype(t.dtype)


def _split_heads(t, n):
    return t.reshape(*t.shape[:-1], n, -1)


def banded_window_attention(q, k, v, w):
    L = q.shape[-2]
    nb = -(-L // w)
    padcfg = [(0, 0)] * (q.ndim - 2) + [(0, nb * w - L), (0, 0)]
    q, k, v = (jnp.pad(t, padcfg) for t in (q, k, v))
    lead = q.shape[:-2]
    dh = q.shape[-1]
    qb = q.reshape(*lead, nb, w, dh)
    kb = k.reshape(*lead, nb, w, dh)
    vb = v.reshape(*lead, nb, w, dh)
    prevcfg = [(0, 0)] * len(lead) + [(1, 0), (0, 0), (0, 0)]
    kcat = jnp.concatenate([jnp.pad(kb, prevcfg)[..., :-1, :, :], kb], axis=-2)
    vcat = jnp.concatenate([jnp.pad(vb, prevcfg)[..., :-1, :, :], vb], axis=-2)
    s = jnp.einsum('...nqd,...nkd->...nqk', qb, kcat).astype(jnp.float32) * (dh ** -0.5)
    qi = jnp.arange(w)[:, None] + w
    ki = jnp.arange(2 * w)[None, :]
    dist = qi - ki
    key_pos = jnp.arange(nb)[:, None, None] * w + ki[None] - w
    mask = (dist >= 0) & (dist <= w) & (key_pos >= 0)
    s = jnp.where(mask, s, MASK_VALUE)
    m = s.max(-1, keepdims=True)
    p = jnp.exp(s - m)
    l = p.sum(-1, keepdims=True)
    o = jnp.einsum('...nqk,...nkd->...nqd', (p / l).astype(v.dtype), vcat)
    lse = (m + jnp.log(l))[..., 0]
    o = o.reshape(*lead, nb * w, dh)[..., :L, :]
    lse = lse.reshape(*lead, nb * w)[..., :L]
    return o, lse


def dilated_branch(q, k, v, window, dil):
    bsz, nh, seq, dh = q.shape
    sp = -(-seq // dil) * dil

    def to_sub(t):
        t = jnp.pad(t, ((0, 0), (0, 0), (0, sp - seq), (0, 0)))
        return t.reshape(bsz, nh, sp // dil, dil, dh).swapaxes(2, 3)

    o, lse = banded_window_attention(to_sub(q), to_sub(k), to_sub(v), window // dil)
    o = o.swapaxes(2, 3).reshape(bsz, nh, sp, dh)[:, :, :seq]
    lse = lse.swapaxes(2, 3).reshape(bsz, nh, sp)[:, :, :seq]
    return o, lse


def mixer_dilated(x, pos, w_in, w_out):
    bsz, seq, _ = x.shape
    q, k, v = jnp.split(x @ w_in, 3, axis=-1)
    q = rope(_split_heads(q, N_HEADS), pos).transpose(0, 2, 1, 3)
    k = rope(_split_heads(k, N_HEADS), pos).transpose(0, 2, 1, 3)
    v = _split_heads(v, N_HEADS).transpose(0, 2, 1, 3)
    outs, lses = [], []
    for window, dil in DILATED_GROUPS:
        o, lse = dilated_branch(q, k, v, window, dil)
        outs.append(o)
        lses.append(lse)
    wts = jax.nn.softmax(jnp.stack(lses), axis=0)
    o = jnp.einsum('gbhs,gbhsd->bshd', wts, jnp.stack(outs).astype(jnp.float32))
    return o.reshape(bsz, seq, D_ATTN).astype(x.dtype) @ w_out


def mixer_dsa(x, pos, w_in, idx_norm_g, idx_norm_b, w_out):
    bsz, seq, _ = x.shape
    q, k, v, qi, ki, wi = jnp.split(x @ w_in, np.cumsum(DSA_SIZES)[:-1].tolist(), axis=-1)
    q = rope(_split_heads(q, N_HEADS), pos).reshape(bsz, seq, B_KV_HEADS, GQA_GROUP, HEAD_DIM)
    k = rope(_split_heads(k, B_KV_HEADS), pos)
    v = _split_heads(v, B_KV_HEADS)
    qi = rope(_split_heads(qi, IDX_HEADS), pos)
    ki = rope(layer_norm(ki, idx_norm_g, idx_norm_b)[:, :, None, :], pos)[:, :, 0]
    wi = wi.astype(jnp.float32) * (IDX_HEADS ** -0.5 * IDX_DIM ** -0.5)
    topk = min(IDX_TOPK_MAX, seq // 4)
    nqb = seq // Q_BLOCK
    scale = HEAD_DIM ** -0.5
    key_pos = jnp.arange(seq)

    def blk(t):
        return t.reshape(bsz, nqb, Q_BLOCK, *t.shape[2:]).swapaxes(0, 1)

    def block(args):
        qb, qib, wib, j = args
        tq = j * Q_BLOCK + jnp.arange(Q_BLOCK)
        logits = jnp.einsum('bqhd,bsd->bqhs', qib, ki).astype(jnp.float32)
        score = jnp.einsum('bqh,bqhs->bqs', wib, jax.nn.relu(logits))
        score = jnp.where(key_pos[None, None, :] <= tq[None, :, None], score, -jnp.inf)
        _, sel = lax.top_k(score, topk)
        kg = jax.vmap(lambda kk, ii: kk[ii])(k, sel)
        vg = jax.vmap(lambda vv, ii: vv[ii])(v, sel)
        s = jnp.einsum('bqgrd,bqkgd->bqgrk', qb, kg).astype(jnp.float32) * scale
        ok = (sel <= tq[None, :, None])[:, :, None, None, :]
        p = jax.nn.softmax(jnp.where(ok, s, MASK_VALUE), axis=-1).astype(v.dtype)
        return jnp.einsum('bqgrk,bqkgd->bqgrd', p, vg)

    o = lax.map(block, (blk(q), blk(qi), blk(wi), jnp.arange(nqb)))
    return o.swapaxes(0, 1).reshape(bsz, seq, D_ATTN) @ w_out


def mixer_fox(x, w_in, b_forget, w_out):
    bsz, seq, _ = x.shape
    q, k, v, f_pre = jnp.split(x @ w_in, [D_ATTN, 2 * D_ATTN, 3 * D_ATTN], axis=-1)
    q, k, v = (_split_heads(t, N_HEADS).transpose(0, 2, 1, 3) for t in (q, k, v))
    log_f = jax.nn.log_sigmoid((f_pre + b_forget).astype(jnp.float32))
    cum = jnp.cumsum(log_f, axis=1).transpose(0, 2, 1)
    nqb = seq // Q_BLOCK
    q_blocks = q.reshape(bsz, N_HEADS, nqb, Q_BLOCK, HEAD_DIM).transpose(2, 0, 1, 3, 4)
    c_blocks = cum.reshape(bsz, N_HEADS, nqb, Q_BLOCK).transpose(2, 0, 1, 3)
    key_pos = jnp.arange(seq)
    scale = HEAD_DIM ** -0.5

    def block(args):
        qb, cq, j = args
        tq = j * Q_BLOCK + jnp.arange(Q_BLOCK)
        s = (jnp.einsum('bhqd,bhkd->bhqk', qb, k).astype(jnp.float32) * scale
             + cq[..., None] - cum[:, :, None, :])
        s = jnp.where(key_pos[None, :] <= tq[:, None], s, MASK_VALUE)
        p = jax.nn.softmax(s, axis=-1).astype(v.dtype)
        return jnp.einsum('bhqk,bhkd->bhqd', p, v)

    o = lax.map(block, (q_blocks, c_blocks, jnp.arange(nqb)))
    return o.transpose(1, 0, 3, 2, 4).reshape(bsz, seq, D_ATTN) @ w_out


def mixer_moba(x, pos, w_in, w_out):
    bsz, seq, _ = x.shape
    q, k, v = jnp.split(x @ w_in, 3, axis=-1)
    q = rope(_split_heads(q, N_HEADS), pos).transpose(0, 2, 1, 3)
    k = rope(_split_heads(k, N_HEADS), pos).transpose(0, 2, 1, 3)
    v = _split_heads(v, N_HEADS).transpose(0, 2, 1, 3)
    nblk = -(-seq // MOBA_BLOCK)
    padk = ((0, 0), (0, 0), (0, nblk * MOBA_BLOCK - seq), (0, 0))
    kb = jnp.pad(k, padk).reshape(bsz, N_HEADS, nblk, MOBA_BLOCK, HEAD_DIM)
    vb = jnp.pad(v, padk).reshape(bsz, N_HEADS, nblk, MOBA_BLOCK, HEAD_DIM)
    kmean = kb.mean(axis=3)
    n_sel = min(MOBA_TOPK, nblk)
    n_chunks = seq // MOBA_Q_CHUNK
    q_chunks = q.reshape(bsz, N_HEADS, n_chunks, MOBA_Q_CHUNK, HEAD_DIM).transpose(2, 0, 1, 3, 4)
    b_idx = jnp.arange(bsz)[:, None, None, None]
    h_idx = jnp.arange(N_HEADS)[None, :, None, None]
    scale = HEAD_DIM ** -0.5

    def chunk(args):
        qc, c = args
        tq = c * MOBA_Q_CHUNK + jnp.arange(MOBA_Q_CHUNK)
        own = (c * MOBA_Q_CHUNK) // MOBA_BLOCK
        gate = jnp.einsum('bhqd,bhnd->bhqn', qc, kmean).astype(jnp.float32)
        gate = jnp.where(jnp.arange(nblk) < own, gate, -jnp.inf)
        _, sel = lax.top_k(gate, n_sel)
        sel_ok = sel < own
        kg = kb[b_idx, h_idx, sel]
        vg = vb[b_idx, h_idx, sel]
        s_sel = jnp.einsum('bhqd,bhqnkd->bhqnk', qc, kg).astype(jnp.float32) * scale
        s_sel = jnp.where(sel_ok[..., None], s_sel, MASK_VALUE)
        k_own = lax.dynamic_index_in_dim(kb, own, axis=2, keepdims=False)
        v_own = lax.dynamic_index_in_dim(vb, own, axis=2, keepdims=False)
        s_own = jnp.einsum('bhqd,bhkd->bhqk', qc, k_own).astype(jnp.float32) * scale
        own_pos = own * MOBA_BLOCK + jnp.arange(MOBA_BLOCK)
        s_own = jnp.where(own_pos[None, :] <= tq[:, None], s_own, MASK_VALUE)
        n_sk = n_sel * MOBA_BLOCK
        s = jnp.concatenate([s_sel.reshape(bsz, N_HEADS, MOBA_Q_CHUNK, n_sk), s_own], axis=-1)
        p = jax.nn.softmax(s, axis=-1).astype(v.dtype)
        p_sel = p[..., :n_sk].reshape(bsz, N_HEADS, MOBA_Q_CHUNK, n_sel, MOBA_BLOCK)
        return (jnp.einsum('bhqnk,bhqnkd->bhqd', p_sel, vg)
                + jnp.einsum('bhqk,bhkd->bhqd', p[..., n_sk:], v_own))

    o = lax.map(chunk, (q_chunks, jnp.arange(n_chunks)))
    return o.transpose(1, 0, 3, 2, 4).reshape(bsz, seq, D_ATTN) @ w_out


def moe_ffn(x, router_w, router_b, w_gu, b_gu, w_dn, b_dn):
    bsz, seq, dm = x.shape
    xt = x.reshape(-1, dm)
    n_tok = xt.shape[0]
    n_assign = n_tok * TOP_K
    logits = (xt @ router_w + router_b).astype(jnp.float32)
    top_logit, top_exp = lax.top_k(logits, TOP_K)
    gates = jax.nn.softmax(top_logit, axis=-1)
    flat_exp = top_exp.reshape(-1)
    order = jnp.argsort(flat_exp)
    sorted_exp = flat_exp[order]
    sorted_tok = (order // TOP_K).astype(jnp.int32)
    sorted_gate = gates.reshape(-1)[order]
    counts = jnp.bincount(flat_exp, length=N_EXPERTS)
    padded = (counts + MOE_ROW_BLOCK - 1) // MOE_ROW_BLOCK * MOE_ROW_BLOCK
    start = jnp.cumsum(counts) - counts
    pad_end = jnp.cumsum(padded)
    pad_start = pad_end - padded
    dest = pad_start[sorted_exp] + jnp.arange(n_assign) - start[sorted_exp]
    n_blocks = -(-n_assign // MOE_ROW_BLOCK) + N_EXPERTS
    n_rows = n_blocks * MOE_ROW_BLOCK
    row_tok = jnp.full((n_rows,), n_tok, jnp.int32).at[dest].set(sorted_tok)
    row_gate = jnp.zeros((n_rows,), jnp.float32).at[dest].set(sorted_gate)
    block_exp = jnp.minimum(
        jnp.searchsorted(pad_end, jnp.arange(n_blocks) * MOE_ROW_BLOCK, side='right'), N_EXPERTS - 1)
    x_rows = jnp.concatenate([xt, jnp.zeros((1, dm), xt.dtype)])[row_tok]
    x_rows = x_rows.reshape(n_blocks, MOE_ROW_BLOCK, dm)

    def expert_block(args):
        xb, e = args
        h = xb @ w_gu[e] + b_gu[e]
        glu, lin = jnp.split(h, 2, axis=-1)
        glu = jnp.minimum(glu, SWIGLU_LIMIT)
        lin = jnp.clip(lin, -SWIGLU_LIMIT, SWIGLU_LIMIT)
        act = glu * jax.nn.sigmoid(SWIGLU_ALPHA * glu) * (lin + 1.0)
        return act @ w_dn[e] + b_dn[e]

    y_rows = lax.map(expert_block, (x_rows, block_exp)).reshape(n_rows, dm)
    y = jax.ops.segment_sum(y_rows * row_gate[:, None], row_tok, num_segments=n_tok + 1)[:n_tok]
    return y.reshape(bsz, seq, dm).astype(x.dtype)


def setup_inputs(seed: int = 0) -> dict:
    keys = iter(jax.random.split(jax.random.key(seed), 16 * DEPTH + 1))

    def rnd(shape, scale, offset=0.0):
        return offset + scale * jax.random.normal(next(keys), shape, jnp.float32)

    inputs = {'x': rnd((BATCH, SEQ, D_MODEL), 1.0)}
    for i in range(DEPTH):
        kind = i % N_MIXERS
        p = 'l%d_' % i
        inputs[p + 'w_in'] = rnd((D_MODEL, MIXER_IN_WIDTHS[kind]), D_MODEL ** -0.5)
        if kind == 1:
            inputs[p + 'idx_norm_g'] = rnd((IDX_DIM,), 0.02, 1.0)
            inputs[p + 'idx_norm_b'] = rnd((IDX_DIM,), 0.02)
        if kind == 2:
            inputs[p + 'b_forget'] = rnd((N_HEADS,), 0.5, FORGET_BIAS_MEAN)
        inputs[p + 'w_out'] = rnd((D_ATTN, D_MODEL), D_ATTN ** -0.5 * DEEPNORM_BETA)
        inputs[p + 'ln1_g'] = rnd((D_MODEL,), 0.02, 1.0)
        inputs[p + 'ln1_b'] = rnd((D_MODEL,), 0.02)
        inputs[p + 'router_w'] = rnd((D_MODEL, N_EXPERTS), D_MODEL ** -0.5)
        inputs[p + 'router_b'] = rnd((N_EXPERTS,), 0.01)
        inputs[p + 'w_gu'] = rnd((N_EXPERTS, D_MODEL, 2 * D_EXPERT), D_MODEL ** -0.5)
        inputs[p + 'b_gu'] = rnd((N_EXPERTS, 2 * D_EXPERT), 0.02)
        inputs[p + 'w_dn'] = rnd((N_EXPERTS, D_EXPERT, D_MODEL), D_EXPERT ** -0.5 * DEEPNORM_BETA)
        inputs[p + 'b_dn'] = rnd((N_EXPERTS, D_MODEL), 0.02)
        inputs[p + 'ln2_g'] = rnd((D_MODEL,), 0.02, 1.0)
        inputs[p + 'ln2_b'] = rnd((D_MODEL,), 0.02)
    return inputs


def reference(x,
              l0_w_in, l0_w_out, l0_ln1_g, l0_ln1_b, l0_router_w, l0_router_b,
              l0_w_gu, l0_b_gu, l0_w_dn, l0_b_dn, l0_ln2_g, l0_ln2_b,
              l1_w_in, l1_idx_norm_g, l1_idx_norm_b, l1_w_out, l1_ln1_g, l1_ln1_b,
              l1_router_w, l1_router_b, l1_w_gu, l1_b_gu, l1_w_dn, l1_b_dn, l1_ln2_g, l1_ln2_b,
              l2_w_in, l2_b_forget, l2_w_out, l2_ln1_g, l2_ln1_b, l2_router_w, l2_router_b,
              l2_w_gu, l2_b_gu, l2_w_dn, l2_b_dn, l2_ln2_g, l2_ln2_b,
              l3_w_in, l3_w_out, l3_ln1_g, l3_ln1_b, l3_router_w, l3_router_b,
              l3_w_gu, l3_b_gu, l3_w_dn, l3_b_dn, l3_ln2_g, l3_ln2_b):
    pos = jnp.arange(x.shape[1], dtype=jnp.int32)
    mixer_params = ((l0_w_in, l0_w_out),
                    (l1_w_in, l1_idx_norm_g, l1_idx_norm_b, l1_w_out),
                    (l2_w_in, l2_b_forget, l2_w_out),
                    (l3_w_in, l3_w_out))
    ln1 = ((l0_ln1_g, l0_ln1_b), (l1_ln1_g, l1_ln1_b), (l2_ln1_g, l2_ln1_b), (l3_ln1_g, l3_ln1_b))
    ffn = ((l0_router_w, l0_router_b, l0_w_gu, l0_b_gu, l0_w_dn, l0_b_dn),
           (l1_router_w, l1_router_b, l1_w_gu, l1_b_gu, l1_w_dn, l1_b_dn),
           (l2_router_w, l2_router_b, l2_w_gu, l2_b_gu, l2_w_dn, l2_b_dn),
           (l3_router_w, l3_router_b, l3_w_gu, l3_b_gu, l3_w_dn, l3_b_dn))
    ln2 = ((l0_ln2_g, l0_ln2_b), (l1_ln2_g, l1_ln2_b), (l2_ln2_g, l2_ln2_b), (l3_ln2_g, l3_ln2_b))
    for i in range(DEPTH):
        kind = i % N_MIXERS
        if kind == 0:
            h = mixer_dilated(x, pos, *mixer_params[i])
        elif kind == 1:
            h = mixer_dsa(x, pos, *mixer_params[i])
        elif kind == 2:
            h = mixer_fox(x, *mixer_params[i])
        else:
            h = mixer_moba(x, pos, *mixer_params[i])
        x = layer_norm(DEEPNORM_ALPHA * x + h, *ln1[i])
        x = layer_norm(DEEPNORM_ALPHA * x + moe_ffn(x, *ffn[i]), *ln2[i])
    return x
```

```python
import contextlib
import numpy as np
import ml_dtypes
import concourse.bass as bass
import concourse.mybir as mybir
from concourse.bass_utils import run_bass_kernel_spmd

F32 = mybir.dt.float32
BF16 = mybir.dt.bfloat16
ALU = mybir.AluOpType
AF = mybir.ActivationFunctionType
AX = mybir.AxisListType

NCORES = 8
D = 2048
KC = 16
NT = 16
TOK = 2048
NEG = -1.0e30
ALPHA = 8 ** 0.25
LN_EPS = 1e-5
SCALE = 128 ** -0.5
W_IN = (6144, 4176, 6160, 6144)
PAIRS = [[0, 1], [2, 3], [4, 5], [6, 7]]
WORLD = [list(range(8))]


class Prog:
    CENG = ("pe", "act", "dve", "pool")
    ND = 6

    def __init__(self, nc, stack):
        self.nc = nc
        self.csem = {e: stack.enter_context(nc.semaphore("c_" + e)) for e in self.CENG}
        self.dsem = {q: [stack.enter_context(nc.semaphore("d_%s%d" % (q, i))) for i in range(self.ND)]
                     for q in ("sp", "pool", "act")}
        self.ccsem = [stack.enter_context(nc.semaphore("cc%d" % i)) for i in range(30)]
        self.reset()

    def all_sems(self):
        s = list(self.csem.values()) + list(self.ccsem)
        for q in self.dsem:
            s += self.dsem[q]
        return s

    def reset(self):
        self.ops = {e: [] for e in ("pe", "act", "dve", "pool", "sp")}
        self.cnt = {e: 0 for e in self.CENG}
        self.dcnt = {q: [0] * self.ND for q in self.dsem}
        self.drr = {q: 0 for q in self.dsem}
        self.ccn = 0
        self.last_w = {}
        self.readers = {}
        self.waited = {e: {} for e in self.ops}

    def _deps(self, eng, reads, writes):
        deps = []
        for r in reads:
            if r in self.last_w:
                deps.append(self.last_w[r])
        for w in writes:
            if w in self.last_w:
                deps.append(self.last_w[w])
            deps.extend(self.readers.get(w, ()))
        return deps

    def _finish(self, eng, deps, fn, tok, amt, reads, writes):
        best = {}
        for (sem, v) in deps:
            if eng == "pe" and sem is self.csem["pe"]:
                continue
            k = id(sem)
            if k not in best or best[k][1] < v:
                best[k] = (sem, v)
        waits = []
        wd = self.waited[eng]
        for k, (sem, v) in best.items():
            if wd.get(k, 0) >= v:
                continue
            wd[k] = v
            waits.append((sem, v))
        self.ops[eng].append((waits, fn, tok[0], amt))
        for w in writes:
            self.last_w[w] = tok
            self.readers[w] = []
        for r in reads:
            self.readers.setdefault(r, []).append(tok)

    def op(self, eng, fn, reads=(), writes=()):
        deps = self._deps(eng, reads, writes)
        self.cnt[eng] += 1
        tok = (self.csem[eng], self.cnt[eng])
        self._finish(eng, deps, fn, tok, 1, reads, writes)

    def i(self, eng, name, reads, writes, *args, **kw):
        self.op(eng, lambda e: getattr(e, name)(*args, **kw), reads=reads, writes=writes)

    def dma(self, out, in_, reads=(), writes=(), q="sp"):
        eng = q
        deps = self._deps(eng, reads, writes)
        i = self.drr[q] % self.ND
        self.drr[q] += 1
        sem = self.dsem[q][i]
        if self.dcnt[q][i] > 0:
            deps.append((sem, 16 * self.dcnt[q][i]))
        self.dcnt[q][i] += 1
        tok = (sem, 16 * self.dcnt[q][i])
        self._finish(eng, deps, lambda e: e.dma_start(out=out, in_=in_), tok, 16, reads, writes)

    def collective(self, kind, op, groups, in_ap, out_ap, reads=(), writes=()):
        deps = self._deps("pool", reads, writes)
        sem = self.ccsem[self.ccn]
        self.ccn += 1
        tok = (sem, 1)

        def fn(e):
            return e.collective_compute(kind, op, replica_groups=groups, ins=[in_ap], outs=[out_ap])
        self._finish("pool", deps, fn, tok, None, reads, writes)

    def emit(self):
        nc = self.nc
        ops = self.ops
        for q in self.dsem:
            eng = q
            waits = [(self.dsem[q][i], 16 * self.dcnt[q][i]) for i in range(self.ND) if self.dcnt[q][i] > 0]
            if q == "pool":
                waits += [(self.ccsem[i], 1) for i in range(self.ccn)]
            if waits:
                ops[eng].append((waits, None, None, None))

        def replay(e, lst):
            for waits, fn, sem, amt in lst:
                for (s, v) in waits:
                    e.wait_ge(s, v)
                if fn is None:
                    continue
                ins = fn(e)
                if amt is None:
                    ins.then_inc(sem)
                else:
                    ins.then_inc(sem, amt)

        with nc.Block() as block:
            @block.sync
            def _(e):
                replay(e, ops["sp"])

            @block.scalar
            def _(e):
                replay(e, ops["act"])

            @block.vector
            def _(e):
                replay(e, ops["dve"])

            @block.gpsimd
            def _(e):
                replay(e, ops["pool"])

            @block.tensor
            def _(e):
                replay(e, ops["pe"])
        sems = self.all_sems()
        with nc.Block() as block:
            @block.gpsimd
            def _(e):
                for s in sems:
                    e.sem_clear(s)
        self.reset()


class Ctx:
    pass


_UNIQ = [0]


def sb(st, nc, name, shape, dt):
    _UNIQ[0] += 1
    return st.enter_context(nc.sbuf_tensor("s%d_%s" % (_UNIQ[0], name), shape, dt))


def ps(st, nc, name, shape, dt):
    _UNIQ[0] += 1
    return st.enter_context(nc.psum_tensor("p%d_%s" % (_UNIQ[0], name), shape, dt))


def emit_xT(P, C, src_ap, src_key, j, dstT, tag):
    i = C.xt_i
    C.xt_i += 1
    xb = C.xb[i % 2]
    xtt = C.xtt[i % 2]
    pt = C.ptx[i % 2]
    ptk = ("ptx", i % 2 if C.ptx[0] is not C.ptx[1] else 0)
    P.i("act", "activation", [src_key], [("xb", i % 2)], out=xb[:], in_=src_ap, func=AF.Copy)
    for k in range(KC):
        P.i("pe", "transpose", [("xb", i % 2), "ident"], [ptk], out=pt[:, k * 128:(k + 1) * 128], in_=xb[:, k * 128:(k + 1) * 128],
                                              identity=C.ident[:])
    P.i("dve", "tensor_copy", [ptk], [("xtt", i % 2)], out=xtt[:], in_=pt[:])
    P.dma(dstT.rearrange("k p t -> p k t")[:, :, j * 128:(j + 1) * 128],
          xtt[:].rearrange("p (k t) -> p k t", k=KC),
          reads=[("xtt", i % 2)], writes=[(tag, j)])


def alloc_xT(st, nc, C, npt=2):
    C.xt_i = 0
    C.xb = [sb(st, nc, "xb%d" % i, [128, D], BF16) for i in range(2)]
    C.xtt = [sb(st, nc, "xtt%d" % i, [128, D], BF16) for i in range(2)]
    C.ptx = [ps(st, nc, "ptx%d" % i, [128, D], BF16) for i in range(npt)]
    if npt == 1:
        C.ptx = C.ptx * 2


def load_const(P, st, nc, name, dram_ap, shape, dt, key):
    t = sb(st, nc, name, shape, dt)
    P.dma(t[:], dram_ap, reads=[], writes=[key])
    return t


def emit_ln(P, C, z, zkey, gi, out_ap, out_key, i):
    st6 = C.ln_st[i % 2]
    mv = C.ln_mv[i % 2]
    for c in range(4):
        P.i("dve", "bn_stats", [zkey], [("lnst", i % 2)], out=st6[:, c * 6:(c + 1) * 6], in_=z[:, c * 512:(c + 1) * 512])
    P.i("dve", "bn_aggr", [("lnst", i % 2)], [("lnmv", i % 2)], out=mv[:, 0:2], in_=st6[:])
    P.i("dve", "tensor_scalar", [("lnmv", i % 2)], [("lnmv", i % 2)], out=mv[:, 2:3], in0=mv[:, 1:2], scalar1=LN_EPS, scalar2=None,
        op0=ALU.add)
    P.i("act", "activation", [("lnmv", i % 2)], [("lnmv", i % 2)], out=mv[:, 2:3], in_=mv[:, 2:3], func=AF.Sqrt)
    P.i("dve", "reciprocal", [("lnmv", i % 2)], [("lnmv", i % 2)], out=mv[:, 2:3], in_=mv[:, 2:3])
    P.i("dve", "tensor_scalar", [zkey, ("lnmv", i % 2)], [zkey], out=z, in0=z, scalar1=mv[:, 0:1], scalar2=mv[:, 2:3],
                                          op0=ALU.subtract, op1=ALU.mult)
    P.i("pool", "tensor_tensor", [zkey, "lnp"], [zkey], out=z, in0=z, in1=C.lnp[:, gi, :], op=ALU.mult)
    P.i("pool", "tensor_tensor", [zkey, "lnp"], [out_key], out=out_ap, in0=z, in1=C.lnp[:, gi + 1, :], op=ALU.add)


def phase_prep(nc, P, G):
    with contextlib.ExitStack() as st:
        C = Ctx()
        alloc_xT(st, nc, C)
        C.ident = load_const(P, st, nc, "ident", G.ident, [128, 128], BF16, "ident")
        xs = [sb(st, nc, "xs%d" % i, [128, D], F32) for i in range(2)]
        for j in range(NT):
            P.dma(xs[j % 2][:], G.x[j * 128:(j + 1) * 128, :], reads=[], writes=[("xs", j % 2)])
            emit_xT(P, C, xs[j % 2][:], ("xs", j % 2), j, G.xT, "xT")
        P.emit()


def proj_blocks(kind):
    blocks = []
    if kind in (0, 3):
        for hb in range(8):
            blocks.append(("fm_rope", hb * 256, 256, ("qT", hb * 2)))
        for hb in range(8):
            blocks.append(("fm_rope", 2048 + hb * 256, 256, ("kT", hb * 2)))
        for vb in range(8):
            blocks.append(("tm", 4096 + vb * 256, 256, ("v", vb * 256)))
    elif kind == 2:
        for hb in range(8):
            blocks.append(("fm_plain", hb * 256, 256, ("qT", hb * 2)))
        for hb in range(8):
            blocks.append(("fm_plain", 2048 + hb * 256, 256, ("kT", hb * 2)))
        for vb in range(8):
            blocks.append(("tm", 4096 + vb * 256, 256, ("v", vb * 256)))
        blocks.append(("fox", 6144, 16, None))
    else:
        for hb in range(8):
            blocks.append(("fm_rope", hb * 256, 256, ("qT", hb * 2)))
        for hb in range(2):
            blocks.append(("fm_rope", 2048 + hb * 256, 256, ("kT", hb * 2)))
        for vb in range(2):
            blocks.append(("tm", 2560 + vb * 256, 256, ("v", vb * 256)))
        for hb in range(4):
            blocks.append(("fm_rope64", 3072 + hb * 256, 256, ("qiT", hb * 2)))
        blocks.append(("kiwi", 4096, 80, None))
    return blocks


def phase_proj(nc, P, G, l):
    kind = l % 4
    w_full = G.w_in_full[l]
    W = W_IN[kind]
    with contextlib.ExitStack() as st:
        C = Ctx()
        xT_sb = sb(st, nc, "xT_sb", [128, KC, TOK], BF16)
        for k in range(KC):
            P.dma(xT_sb[:, k, :], G.xT[k], reads=[("xT", j) for j in range(NT)] if k == 0 else [],
                  writes=[("xT_sb", k)])
        xT_keys = [("xT_sb", k) for k in range(KC)]
        cosT = load_const(P, st, nc, "cosT", G.cosT, [128, TOK], F32, "cosT")
        sinT = load_const(P, st, nc, "sinT", G.sinT, [128, TOK], F32, "sinT")
        if kind == 1:
            cos64 = load_const(P, st, nc, "cos64", G.cos64T, [128, TOK], F32, "cos64")
            sin64 = load_const(P, st, nc, "sin64", G.sin64T, [128, TOK], F32, "sin64")
            kcos = load_const(P, st, nc, "kcos", G.kcos, [128, NT, 32], F32, "kcos")
            ksin = load_const(P, st, nc, "ksin", G.ksin, [128, NT, 32], F32, "ksin")
            idxg = load_const(P, st, nc, "idxg", G.idx_g[l], [128, 64], F32, "idxg")
            idxb = load_const(P, st, nc, "idxb", G.idx_b[l], [128, 64], F32, "idxb")
            identf = load_const(P, st, nc, "identf", G.identf, [128, 128], F32, "identf")
        if kind == 2:
            negbf = load_const(P, st, nc, "negbf", G.negbf[l], [16, 1], F32, "negbf")
        wst = [sb(st, nc, "wst%d" % i, [128, KC, 256], F32) for i in range(2)]
        wbf = [sb(st, nc, "wbf%d" % i, [128, KC, 256], BF16) for i in range(2)]
        wsw = [sb(st, nc, "wsw%d" % i, [128, KC, 256], BF16) for i in range(2)]
        pa = [ps(st, nc, "pa%d" % i, [128, 512], F32) for i in range(2)]
        pb = [ps(st, nc, "pb%d" % i, [128, 512], F32) for i in range(2)]
        pv = [ps(st, nc, "pv%d" % i, [128, 512], F32) for i in range(2)]
        t1 = [sb(st, nc, "t1_%d" % i, [128, 512], F32) for i in range(2)]
        t2 = [sb(st, nc, "t2_%d" % i, [128, 512], F32) for i in range(2)]
        ob = [sb(st, nc, "ob%d" % i, [128, 512], BF16) for i in range(2)]
        vb_ = [sb(st, nc, "vbo%d" % i, [128, 256], BF16) for i in range(2)]
        if kind == 1:
            kw = [sb(st, nc, "kw%d" % i, [128, 80], F32) for i in range(2)]
            kst = [sb(st, nc, "kst%d" % i, [128, 8], F32) for i in range(2)]
            kr = [sb(st, nc, "kr%d" % i, [128, 64], F32) for i in range(2)]
            kt_ = [sb(st, nc, "ktmp%d" % i, [128, 64], F32) for i in range(2)]
            kiTs = [sb(st, nc, "kiTs%d" % i, [128, 128], BF16) for i in range(2)]
            wis = [sb(st, nc, "wis%d" % i, [128, 16], F32) for i in range(2)]
            pk = [ps(st, nc, "pk%d" % i, [128, 128], F32) for i in range(2)]
        if kind == 2:
            fx = sb(st, nc, "fx", [16, TOK], F32)
        blocks = proj_blocks(kind)
        cnt = {"a": 0, "v": 0, "o": 0, "k": 0}

        def load_block(bi):
            typ, c0, ncol, _ = blocks[bi]
            s = bi % 2
            P.dma(wst[s][:, :, 0:ncol], w_full[:, c0:c0 + ncol].rearrange("(k p) c -> p k c", p=128),
                  reads=["w_in_full"], writes=[("wst", s)])
            P.i("pool", "tensor_copy", [("wst", s)], [("wbf", s)], out=wbf[s][:, :, 0:ncol], in_=wst[s][:, :, 0:ncol])
            if typ in ("fm_rope", "fm_rope64"):
                hw = 64 if typ == "fm_rope" else 32
                v4 = wbf[s][:].rearrange("p k (g two h) -> p k g two h", two=2, h=hw)
                o4 = wsw[s][:].rearrange("p k (g two h) -> p k g two h", two=2, h=hw)
                for k in range(KC):
                    P.i("pool", "tensor_copy", [("wbf", s)], [("wsw", s)], out=o4[:, k, :, 0, :], in_=v4[:, k, :, 1, :])
                    P.i("pool", "tensor_copy", [("wbf", s)], [("wsw", s)], out=o4[:, k, :, 1, :], in_=v4[:, k, :, 0, :])

        load_block(0)
        for bi, (typ, c0, ncol, dst) in enumerate(blocks):
            if bi + 1 < len(blocks):
                load_block(bi + 1)
            s = bi % 2
            if typ in ("fm_rope", "fm_plain", "fm_rope64"):
                for hh in range(2):
                    if dst[0] == "qT":
                        dram = G.qT[dst[1] + hh]
                    elif dst[0] == "kT":
                        dram = G.kT_own[dst[1] + hh]
                    else:
                        dram = G.qiT[dst[1] + hh]
                    for c4 in range(4):
                        a = cnt["a"] % 2
                        cnt["a"] += 1
                        tsl = slice(c4 * 512, (c4 + 1) * 512)
                        for k in range(KC):
                            P.i("pe", "matmul", [("wbf", s), ("xT_sb", k)], [("pa", a)], pa[a][:], lhsT=wbf[s][:, k, hh * 128:(hh + 1) * 128], rhs=xT_sb[:, k, tsl],
                                start=(k == 0), stop=(k == KC - 1))
                        o = cnt["o"] % 2
                        cnt["o"] += 1
                        if typ == "fm_plain":
                            P.i("act", "activation", [("pa", a)], [("ob", o)], out=ob[o][:], in_=pa[a][:], func=AF.Copy)
                        else:
                            for k in range(KC):
                                P.i("pe", "matmul", [("wsw", s), ("xT_sb", k)], [("pb", a)], pb[a][:], lhsT=wsw[s][:, k, hh * 128:(hh + 1) * 128], rhs=xT_sb[:, k, tsl],
                                    start=(k == 0), stop=(k == KC - 1))
                            ct, sn = (cosT, sinT) if typ == "fm_rope" else (cos64, sin64)
                            ck, sk = ("cosT", "sinT") if typ == "fm_rope" else ("cos64", "sin64")
                            P.i("dve", "tensor_tensor", [("pa", a), ck], [("t1", o)], out=t1[o][:], in0=pa[a][:], in1=ct[:, tsl], op=ALU.mult)
                            P.i("dve", "tensor_tensor", [("pb", a), sk], [("t2", o)], out=t2[o][:], in0=pb[a][:], in1=sn[:, tsl], op=ALU.mult)
                            P.i("pool", "tensor_tensor", [("t1", o), ("t2", o)], [("ob", o)], out=ob[o][:], in0=t1[o][:], in1=t2[o][:],
                                                                        op=ALU.add)
                        P.dma(dram[:, tsl], ob[o][:], reads=[("ob", o)], writes=[dst[0] + "_own"])
            elif typ == "tm":
                for j in range(NT):
                    a = cnt["v"] % 2
                    cnt["v"] += 1
                    for k in range(KC):
                        P.i("pe", "matmul", [("wbf", s), ("xT_sb", k)], [("pv", a)], pv[a][:, 0:ncol], lhsT=xT_sb[:, k, j * 128:(j + 1) * 128], rhs=wbf[s][:, k, 0:ncol],
                            start=(k == 0), stop=(k == KC - 1))
                    P.i("act", "activation", [("pv", a)], [("vbo", a)], out=vb_[a][:, 0:ncol], in_=pv[a][:, 0:ncol], func=AF.Copy)
                    P.dma((G.v_own4 if kind == 1 else G.v_own)[j * 128:(j + 1) * 128, dst[1]:dst[1] + ncol], vb_[a][:, 0:ncol],
                          reads=[("vbo", a)], writes=["v_own"])
            elif typ == "fox":
                for c4 in range(4):
                    a = cnt["a"] % 2
                    cnt["a"] += 1
                    tsl = slice(c4 * 512, (c4 + 1) * 512)
                    for k in range(KC):
                        P.i("pe", "matmul", [("wbf", s), ("xT_sb", k)], [("pa", a)], pa[a][0:16, :], lhsT=wbf[s][:, k, 0:16], rhs=xT_sb[:, k, tsl],
                            start=(k == 0), stop=(k == KC - 1))
                    P.i("dve", "tensor_scalar", [("pa", a), "negbf"], ["fx"], out=fx[:, tsl], in0=pa[a][0:16, :],
                                                                         scalar1=negbf[:, 0:1], scalar2=None, op0=ALU.add)
                P.i("act", "activation", ["fx"], ["fx"], out=fx[:], in_=fx[:], func=AF.Exp, scale=-1.0)
                P.i("act", "activation", ["fx"], ["fx"], out=fx[:], in_=fx[:], func=AF.Ln, bias=1.0, scale=1.0)
                P.dma(G.sp_own[:, :], fx[:], reads=["fx"], writes=["sp_own"])
            elif typ == "kiwi":
                for j in range(NT):
                    a = cnt["k"] % 2
                    cnt["k"] += 1
                    for k in range(KC):
                        P.i("pe", "matmul", [("wbf", s), ("xT_sb", k)], [("pv", a)], pv[a][:, 0:80], lhsT=xT_sb[:, k, j * 128:(j + 1) * 128], rhs=wbf[s][:, k, 0:80],
                            start=(k == 0), stop=(k == KC - 1))
                    P.i("act", "activation", [("pv", a)], [("kw", a)], out=kw[a][:], in_=pv[a][:, 0:80], func=AF.Copy)
                    P.i("dve", "tensor_scalar", [("kw", a)], [("wis", a)], out=wis[a][:], in0=kw[a][:, 64:80],
                                                               scalar1=1.0 / 32.0, scalar2=None, op0=ALU.mult)
                    P.dma(G.wi[j * 128:(j + 1) * 128, :], wis[a][:], reads=[("wis", a)], writes=["wi"])
                    P.i("dve", "bn_stats", [("kw", a)], [("kst", a)], out=kst[a][:, 0:6], in_=kw[a][:, 0:64])
                    P.i("dve", "bn_aggr", [("kst", a)], [("kst", a)], out=kst[a][:, 6:8], in_=kst[a][:, 0:6])
                    P.i("dve", "tensor_scalar", [("kst", a)], [("kst", a)], out=kst[a][:, 0:1], in0=kst[a][:, 7:8], scalar1=LN_EPS,
                        scalar2=None, op0=ALU.add)
                    P.i("act", "activation", [("kst", a)], [("kst", a)], out=kst[a][:, 0:1], in_=kst[a][:, 0:1], func=AF.Sqrt)
                    P.i("dve", "reciprocal", [("kst", a)], [("kst", a)], out=kst[a][:, 0:1], in_=kst[a][:, 0:1])
                    P.i("dve", "tensor_scalar", [("kw", a), ("kst", a)], [("kr", a)], out=kr[a][:], in0=kw[a][:, 0:64], scalar1=kst[a][:, 6:7],
                                                               scalar2=kst[a][:, 0:1], op0=ALU.subtract, op1=ALU.mult)
                    P.i("dve", "tensor_tensor", [("kr", a), "idxg"], [("kr", a)], out=kr[a][:], in0=kr[a][:], in1=idxg[:], op=ALU.mult)
                    P.i("dve", "tensor_tensor", [("kr", a), "idxb"], [("kr", a)], out=kr[a][:], in0=kr[a][:], in1=idxb[:], op=ALU.add)
                    x1 = kr[a][:, 0:32]
                    x2 = kr[a][:, 32:64]
                    P.i("dve", "tensor_tensor", [("kr", a), "kcos"], [("ktmp", a)], out=kt_[a][:, 0:32], in0=x1, in1=kcos[:, j, :],
                                                                           op=ALU.mult)
                    P.i("dve", "tensor_tensor", [("kr", a), "ksin"], [("ktmp", a)], out=kt_[a][:, 32:64], in0=x2, in1=ksin[:, j, :],
                                                                           op=ALU.mult)
                    P.i("dve", "tensor_tensor", [("ktmp", a)], [("kw", a)], out=kw[a][:, 0:32], in0=kt_[a][:, 0:32], in1=kt_[a][:, 32:64],
                                                               op=ALU.subtract)
                    P.i("dve", "tensor_tensor", [("kr", a), "kcos", ("kw", a)], [("ktmp", a)], out=kt_[a][:, 0:32], in0=x2, in1=kcos[:, j, :],
                                                                           op=ALU.mult)
                    P.i("dve", "tensor_tensor", [("kr", a), "ksin"], [("ktmp", a)], out=kt_[a][:, 32:64], in0=x1, in1=ksin[:, j, :],
                                                                           op=ALU.mult)
                    P.i("dve", "tensor_tensor", [("ktmp", a)], [("kw", a)], out=kw[a][:, 32:64], in0=kt_[a][:, 0:32], in1=kt_[a][:, 32:64],
                                                               op=ALU.add)
                    P.i("pe", "transpose", [("kw", a), "identf"], [("pk", a)], out=pk[a][0:64, :], in_=kw[a][:, 0:64], identity=identf[:])
                    P.i("act", "activation", [("pk", a)], [("kiTs", a)], out=kiTs[a][0:64, :], in_=pk[a][0:64, :], func=AF.Copy)
                    P.dma(G.kiT_own[0:64, j * 128:(j + 1) * 128], kiTs[a][0:64, :], reads=[("kiTs", a)],
                          writes=["kiT_own"])
                    P.dma(G.kiT_own[64:128, j * 128:(j + 1) * 128], kiTs[a][0:64, :], reads=[("kiTs", a)],
                          writes=["kiT_own"])
        nkc = 1 if kind == 1 else 4
        for k in range(nkc):
            P.collective("AllGather", ALU.bypass, PAIRS,
                         G.kT_own[4 * k:4 * k + 4].rearrange("h d t -> (h d) t"),
                         G.kT_all[k].rearrange("r h d t -> (r h d) t"),
                         reads=["kT_own"], writes=["kT_all"])
        if kind == 1:
            P.collective("AllGather", ALU.bypass, PAIRS, G.v_own4[:, :], G.v_all4.rearrange("r t c -> (r t) c"),
                         reads=["v_own"], writes=["v_all"])
            P.collective("AllGather", ALU.bypass, PAIRS, G.kiT_own[:, :], G.kiT_all.rearrange("r d t -> (r d) t"),
                         reads=["kiT_own"], writes=["kiT_all"])
        else:
            for k in range(4):
                P.collective("AllGather", ALU.bypass, PAIRS, G.v_own[k * 512:(k + 1) * 512, :],
                             G.v_all[k].rearrange("r t c -> (r t) c"),
                             reads=["v_own"], writes=["v_all"])
        if kind == 2:
            P.collective("AllGather", ALU.bypass, PAIRS, G.sp_own[:, :], G.sp_all.rearrange("r h t -> (r h) t"),
                         reads=["sp_own"], writes=["sp_all"])
        P.emit()


def phase_attn(nc, P, G, l):
    kind = l % 4
    with contextlib.ExitStack() as st:
        ident = load_const(P, st, nc, "ident", G.ident, [128, 128], BF16, "ident")
        maskA = load_const(P, st, nc, "maskA", G.maskA, [128, 128], F32, "maskA")
        maskB = load_const(P, st, nc, "maskB", G.maskB, [128, 128], F32, "maskB")
        masks = (maskA, maskB)
        if kind == 0:
            dilb = load_const(P, st, nc, "dilb", G.dilb, [128, 2, 9 * 128], F32, "dilb")
        nkv = 4 if kind == 1 else 2
        if kind == 1:
            KT = [sb(st, nc, "KT4", [128, 4, 2, TOK], BF16)]
            V = [sb(st, nc, "V4", [128, 2, NT, 512], BF16)]
            QT = [sb(st, nc, "QTj%d" % i, [128, 16, 128], BF16) for i in range(2)]
            Bias = [sb(st, nc, "Bias%d" % i, [128, 2, TOK], BF16) for i in range(2)]
        else:
            KT = [sb(st, nc, "KT%d" % i, [128, 2, TOK], BF16) for i in range(2)]
            V = [sb(st, nc, "V%d" % i, [128, 2, NT, 128], BF16) for i in range(2)]
            QT = [sb(st, nc, "QT%d" % i, [128, TOK], BF16) for i in range(2)]
        if kind == 2:
            Bias = [sb(st, nc, "Bias%d" % i, [128, 2, TOK], F32) for i in range(2)]
        S = [sb(st, nc, "S%d" % i, [128, 2, TOK], F32) for i in range(2)]
        Pb = [sb(st, nc, "Pb%d" % i, [128, 2, TOK], BF16) for i in range(2)]
        PT = [sb(st, nc, "PT%d" % i, [128, 32 * 128], BF16) for i in range(2)]
        sm = [sb(st, nc, "sm%d" % i, [128, 8], F32) for i in range(2)]
        Osb = [sb(st, nc, "Osb%d" % i, [128, 128], BF16) for i in range(2)]
        OT = [sb(st, nc, "OT%d" % i, [128, 128], BF16) for i in range(2)]
        sps = [ps(st, nc, "sps%d" % i, [128, 512], F32) for i in range(3)]
        ptp = [ps(st, nc, "ptp%d" % i, [128, 1024], BF16) for i in range(2)]
        pmisc = ps(st, nc, "pmisc", [128, 512], F32)
        otp = ps(st, nc, "otp", [128, 128], BF16)
        cn = {"sps": 0, "ptp": 0, "it": 0}

        if kind == 2:
            spg = sb(st, nc, "spg", [16, 2 * TOK], F32)
            ones = sb(st, nc, "ones", [16, 2 * TOK], F32)
            cum = sb(st, nc, "cum", [16, 2 * TOK], F32)
            for r in range(2):
                P.dma(spg[:].rearrange("h (j r i) -> h j r i", r=2, i=128)[:, :, r, :],
                      G.sp_all[r].rearrange("h (j i) -> h j i", i=128), reads=[], writes=["spg"])
            P.i("pool", "memset", [], ["ones"], ones[:], 1.0)
            P.i("dve", "tensor_tensor_scan", ["spg", "ones"], ["cum"], out=cum[:], data0=ones[:], data1=spg[:],
                initial=0.0, op0=ALU.mult, op1=ALU.add)
            for r in range(2):
                P.dma(G.negck.rearrange("h (r j i) -> h r j i", r=2, i=128)[:, r],
                      cum[:].rearrange("h (j r i) -> h j r i", r=2, i=128)[:, :, r, :], reads=["cum"], writes=["negck"])
        if kind == 3:
            km2 = [sb(st, nc, "km2_%d" % i, [128, 2, NT], F32) for i in range(2)]
            kmT = [sb(st, nc, "kmT%d" % i, [128, NT], BF16) for i in range(2)]
            gm = [sb(st, nc, "gm%d" % i, [128, 16], F32) for i in range(2)]
            m8 = [sb(st, nc, "m8_%d" % i, [128, 8], F32) for i in range(2)]
            bb = [sb(st, nc, "bb%d" % i, [128, 16], F32) for i in range(2)]

        def load_head(h):
            hb = h % 2
            for r in range(2):
                P.dma(KT[hb][:, r, :], G.kT_all[h // 4, r, h % 4], reads=[], writes=[("KT", hb)])
                for ck in range(4):
                    P.dma(V[hb][:, r, ck * 4:(ck + 1) * 4, :],
                          G.v_all[ck, r, :, h * 128:(h + 1) * 128].rearrange("(jj p) d -> p jj d", p=128),
                          reads=[], writes=[("V", hb)])
            P.dma(QT[hb][:], G.qT[h], reads=[], writes=[("QT", hb)])
            if kind == 2:
                P.dma(Bias[hb][:].rearrange("p r t -> p (r t)"), G.negck[h:h + 1, :].broadcast_to([128, 2 * TOK]),
                      reads=["negck"], writes=[("Bias", hb)])
            if kind == 3:
                P.i("dve", "tensor_reduce", [("KT", hb)], [("km2", hb)], out=km2[hb][:],
                    in_=KT[hb][:].rearrange("p r (j i) -> p r j i", i=128), axis=AX.X, op=ALU.add)
                P.i("dve", "tensor_tensor", [("km2", hb)], [("km2", hb)], out=km2[hb][:, 0, :], in0=km2[hb][:, 0, :],
                    in1=km2[hb][:, 1, :], op=ALU.add)
                P.i("dve", "tensor_scalar", [("km2", hb)], [("kmT", hb)], out=kmT[hb][:], in0=km2[hb][:, 0, :],
                    scalar1=1.0 / 256.0, scalar2=None, op0=ALU.mult)

        def core(h, j, qT_ap, qkeys, kt_fn, kkeys, v_fn, vkeys, bias_fn, bkeys):
            it = cn["it"]
            cn["it"] += 1
            p = it % 2
            jlo = max(0, j - 8) if kind == 0 else 0
            lo, hi = jlo * 128, (j + 1) * 128
            skeys = []
            if kind == 3:
                hb = h % 2
                P.i("pe", "matmul", qkeys + [("kmT", hb)], [("pgate", 0)], pmisc[:, 256:272], lhsT=qT_ap, rhs=kmT[hb][:],
                    start=True, stop=True)
                P.i("pool", "memset", [], [("gm", p)], gm[p][:], NEG)
                if j > 0:
                    P.i("dve", "tensor_copy", [("pgate", 0), ("gm", p)], [("gm", p)], out=gm[p][:, 0:j], in_=pmisc[:, 256:256 + j])
                P.i("dve", "max", [("gm", p)], [("m8", p)], out=m8[p][:], in_=gm[p][:])
                P.i("dve", "tensor_scalar", [("gm", p), ("m8", p)], [("bb", p)], out=bb[p][:], in0=gm[p][:],
                    scalar1=m8[p][:, 2:3], scalar2=None, op0=ALU.is_ge)
                P.i("dve", "tensor_scalar", [("bb", p)], [("bb", p)], out=bb[p][:], in0=bb[p][:], scalar1=-1.0,
                    scalar2=1.0e30, op0=ALU.add, op1=ALU.mult)
            for r in range(2):
                if kind == 3:
                    chunks = [(c0, min(512, j * 128 - c0)) for c0 in range(0, j * 128, 512)] + [(j * 128, 128)]
                else:
                    chunks = [(c0, min(512, hi - c0)) for c0 in range(lo, hi, 512)]
                for (c0, w) in chunks:
                    b = cn["sps"] % 3
                    cn["sps"] += 1
                    P.i("pe", "matmul", qkeys + kkeys, [("sps", b)], sps[b][:, 0:w], lhsT=qT_ap, rhs=kt_fn(r, c0, w),
                        start=True, stop=True)
                    key = ("S", p, r, c0)
                    skeys.append(key)
                    if kind == 0:
                        off = c0 - (j - 8) * 128
                        P.i("dve", "scalar_tensor_tensor", [("sps", b), "dilb"], [key], out=S[p][:, r, c0:c0 + w],
                            in0=sps[b][:, 0:w], scalar=SCALE, in1=dilb[:, r, off:off + w], op0=ALU.mult, op1=ALU.add)
                    elif kind == 3:
                        if c0 == j * 128:
                            P.i("dve", "scalar_tensor_tensor", [("sps", b), "maskA", "maskB"], [key],
                                out=S[p][:, r, c0:c0 + w], in0=sps[b][:, 0:w], scalar=SCALE, in1=masks[r][:],
                                op0=ALU.mult, op1=ALU.add)
                        else:
                            nt = w // 128
                            t0 = c0 // 128
                            P.i("dve", "scalar_tensor_tensor", [("sps", b), ("bb", p)], [key],
                                out=S[p][:, r, c0:c0 + w].rearrange("p (t i) -> p t i", i=128),
                                in0=sps[b][:, 0:w].rearrange("p (t i) -> p t i", i=128), scalar=SCALE,
                                in1=bb[p][:, t0:t0 + nt].unsqueeze(2).broadcast_to([128, nt, 128]),
                                op0=ALU.mult, op1=ALU.add)
                    else:
                        P.i("dve", "scalar_tensor_tensor", [("sps", b)] + bkeys, [key], out=S[p][:, r, c0:c0 + w],
                            in0=sps[b][:, 0:w], scalar=SCALE, in1=bias_fn(r, c0, w), op0=ALU.mult, op1=ALU.add)
                if kind in (1, 2):
                    key = [k for k in skeys if k[2] == r][-1]
                    P.i("dve", "tensor_tensor", [key, "maskA", "maskB"], [key], out=S[p][:, r, j * 128:(j + 1) * 128],
                        in0=S[p][:, r, j * 128:(j + 1) * 128], in1=masks[r][:], op=ALU.add)
            P.i("dve", "tensor_reduce", skeys, [("mx", p)], out=sm[p][:, 0:1], in_=S[p][:, :, lo:hi], axis=AX.XY, op=ALU.max)
            P.i("dve", "tensor_scalar", [("mx", p)], [("nmx", p)], out=sm[p][:, 1:2], in0=sm[p][:, 0:1], scalar1=-1.0,
                scalar2=None, op0=ALU.mult)
            P.i("pool", "memset", [], [("rs", p)], sm[p][:, 2:4], 0.0)
            for r in range(2):
                P.i("act", "activation", [k for k in skeys if k[2] == r] + [("nmx", p), ("rs", p)], [("Pb", p, r), ("rs", p)],
                    out=Pb[p][:, r, lo:hi], in_=S[p][:, r, lo:hi], func=AF.Exp, bias=sm[p][:, 1:2], scale=1.0,
                    accum_out=sm[p][:, 2 + r:3 + r])
            P.i("dve", "tensor_tensor", [("rs", p)], [("rinv", p)], out=sm[p][:, 4:5], in0=sm[p][:, 2:3], in1=sm[p][:, 3:4],
                op=ALU.add)
            P.i("dve", "reciprocal", [("rinv", p)], [("rinv", p)], out=sm[p][:, 4:5], in_=sm[p][:, 4:5])
            tiles = [(r, jj) for r in range(2) for jj in range(jlo, j + 1)]
            ngr = (len(tiles) + 7) // 8
            for g in range(ngr):
                grp = tiles[g * 8:(g + 1) * 8]
                pb_ = cn["ptp"] % 2
                cn["ptp"] += 1
                for i_, (r, jj) in enumerate(grp):
                    P.i("pe", "transpose", [("Pb", p, r), "ident"], [("ptp", pb_)], out=ptp[pb_][:, i_ * 128:(i_ + 1) * 128],
                        in_=Pb[p][:, r, jj * 128:(jj + 1) * 128], identity=ident[:])
                n = len(grp)
                eng = "act" if g % 2 == 0 else "dve"
                if eng == "act":
                    P.i("act", "activation", [("ptp", pb_)], [("PT", p, g)], out=PT[p][:, g * 1024:g * 1024 + n * 128],
                        in_=ptp[pb_][:, 0:n * 128], func=AF.Copy)
                else:
                    P.i("dve", "tensor_copy", [("ptp", pb_)], [("PT", p, g)], out=PT[p][:, g * 1024:g * 1024 + n * 128],
                        in_=ptp[pb_][:, 0:n * 128])
            po = pmisc[:, p * 128:(p + 1) * 128]
            for ti, (r, jj) in enumerate(tiles):
                P.i("pe", "matmul", [("PT", p, ti // 8)] + vkeys, [("po", p)], po, lhsT=PT[p][:, ti * 128:(ti + 1) * 128],
                    rhs=v_fn(r, jj), start=(ti == 0), stop=(ti == len(tiles) - 1))
            P.i("dve", "tensor_scalar", [("po", p), ("rinv", p)], [("Osb", p)], out=Osb[p][:], in0=po, scalar1=sm[p][:, 4:5],
                scalar2=None, op0=ALU.mult)
            P.i("pe", "transpose", [("Osb", p), "ident"], [("otp", 0)], out=otp[:], in_=Osb[p][:], identity=ident[:])
            P.i("act", "activation", [("otp", 0)], [("OT", p)], out=OT[p][:], in_=otp[:], func=AF.Copy)
            P.dma(G.oT[h][:, j * 128:(j + 1) * 128], OT[p][:], reads=[("OT", p)], writes=["oT"])

        if kind == 1:
            for r in range(2):
                for kv in range(4):
                    P.dma(KT[0][:, kv, r, :], G.kT_all[0, r, kv], reads=[], writes=["KT4"])
                P.dma(V[0][:, r, :, :], G.v_all4[r].rearrange("(j p) c -> p j c", p=128), reads=[], writes=["V4"])
            for j in range(NT):
                jb = j % 2
                P.dma(QT[jb][:], G.qT.rearrange("h d t -> d h t")[:, :, j * 128:(j + 1) * 128], reads=[], writes=[("QT", jb)])
                P.dma(Bias[jb][:].rearrange("p r t -> p (r t)"), G.dsab[j], reads=["dsab"], writes=[("Bias", jb)])
                for h in range(16):
                    kv = h // 4
                    core(h, j, QT[jb][:, h, :], [("QT", jb)],
                         lambda r, c0, w, kv=kv: KT[0][:, kv, r, c0:c0 + w], ["KT4"],
                         lambda r, jj, kv=kv: V[0][:, r, jj, kv * 128:(kv + 1) * 128], ["V4"],
                         lambda r, c0, w, jb=jb: Bias[jb][:, r, c0:c0 + w], [("Bias", jb)])
        else:
            load_head(0)
            for h in range(16):
                if h + 1 < 16:
                    load_head(h + 1)
                hb = h % 2
                for j in range(NT):
                    core(h, j, QT[hb][:, j * 128:(j + 1) * 128], [("QT", hb)],
                         lambda r, c0, w, hb=hb: KT[hb][:, r, c0:c0 + w], [("KT", hb)],
                         lambda r, jj, hb=hb: V[hb][:, r, jj, :], [("V", hb)],
                         (lambda r, c0, w, hb=hb: Bias[hb][:, r, c0:c0 + w]) if kind == 2 else None,
                         [("Bias", hb)] if kind == 2 else [])
        P.emit()


def phase_dsa_index(nc, P, G, l):
    with contextlib.ExitStack() as st:
        maskA = load_const(P, st, nc, "maskA", G.maskA, [128, 128], F32, "maskA")
        maskB = load_const(P, st, nc, "maskB", G.maskB, [128, 128], F32, "maskB")
        masks = (maskA, maskB)
        kiT = sb(st, nc, "kiT", [128, 2, TOK], BF16)
        for r in range(2):
            P.dma(kiT[:, r, :], G.kiT_all[r], reads=[], writes=["kiT"])
        qiT = [sb(st, nc, "qiT%d" % i, [128, 8, 128], BF16) for i in range(2)]
        wi = [sb(st, nc, "wi%d" % i, [128, 16], F32) for i in range(2)]
        sc = [sb(st, nc, "sc%d" % i, [128, 2 * TOK], F32) for i in range(2)]
        junk = sb(st, nc, "junk", [128, 2 * TOK], F32)
        mbf = [sb(st, nc, "mbf%d" % i, [128, 2 * TOK], BF16) for i in range(2)]
        rl = [sb(st, nc, "rl%d" % i, [128, 512], F32) for i in range(3)]
        bs = [sb(st, nc, "bs%d" % i, [128, 8], F32) for i in range(2)]
        lps = [ps(st, nc, "lps%d" % i, [128, 512], F32) for i in range(4)]
        cn = {"l": 0, "r": 0}
        for j in range(NT):
            p = j % 2
            hi = (j + 1) * 128
            P.dma(qiT[p][:], G.qiT.rearrange("g d t -> d g t")[:, :, j * 128:(j + 1) * 128], reads=[], writes=[("qiT", p)])
            P.dma(wi[p][:], G.wi[j * 128:(j + 1) * 128, :], reads=[], writes=[("wi", p)])
            for r in range(2):
                for c0 in range(0, hi, 512):
                    w = min(512, hi - c0)
                    key = ("sc", p, r, c0)
                    dst = sc[p][:, r * hi + c0:r * hi + c0 + w]
                    for ih in range(16):
                        g, half = ih // 2, ih % 2
                        b = cn["l"] % 4
                        cn["l"] += 1
                        P.i("pe", "matmul", [("qiT", p), "kiT"], [("lps", b)], lps[b][:, 0:w],
                            lhsT=qiT[p][half * 64:(half + 1) * 64, g, :], rhs=kiT[half * 64:(half + 1) * 64, r, c0:c0 + w],
                            start=True, stop=True)
                        q_ = cn["r"] % 3
                        cn["r"] += 1
                        P.i("act", "activation", [("lps", b)], [("rl", q_)], out=rl[q_][:, 0:w], in_=lps[b][:, 0:w], func=AF.Relu)
                        if ih == 0:
                            P.i("dve", "tensor_scalar", [("rl", q_), ("wi", p)], [key], out=dst, in0=rl[q_][:, 0:w],
                                scalar1=wi[p][:, 0:1], scalar2=None, op0=ALU.mult)
                        else:
                            P.i("dve", "scalar_tensor_tensor", [("rl", q_), ("wi", p), key], [key], out=dst, in0=rl[q_][:, 0:w],
                                scalar=wi[p][:, ih:ih + 1], in1=dst, op0=ALU.mult, op1=ALU.add)
            allk = [("sc", p, r, c0) for r in range(2) for c0 in range(0, hi, 512)]
            sk = ("scall", p)
            B = bs[p]
            bk = ("bs", p)
            P.i("dve", "tensor_reduce", allk, [bk], out=B[:, 6:7], in_=sc[p][:, 0:2 * hi], axis=AX.X, op=ALU.max,
                apply_absolute_value=True)
            for r in range(2):
                sl = sc[p][:, r * hi + j * 128:r * hi + (j + 1) * 128]
                P.i("dve", "tensor_tensor", allk + [bk, "maskA", "maskB"], [sk], out=sl, in0=sl, in1=masks[r][:], op=ALU.add)
            P.i("dve", "tensor_scalar", [bk], [bk], out=B[:, 1:2], in0=B[:, 6:7], scalar1=1.0, scalar2=None, op0=ALU.add)
            P.i("dve", "tensor_scalar", [bk], [bk], out=B[:, 0:1], in0=B[:, 1:2], scalar1=-1.0, scalar2=None, op0=ALU.mult)
            for it in range(26):
                P.i("dve", "tensor_scalar", [bk], [bk], out=B[:, 2:3], in0=B[:, 0:1], scalar1=B[:, 1:2], scalar2=0.5,
                    op0=ALU.add, op1=ALU.mult)
                P.i("dve", "tensor_scalar", [sk, bk], ["junk", bk], out=junk[:, 0:2 * hi], in0=sc[p][:, 0:2 * hi],
                    scalar1=B[:, 2:3], scalar2=0.0, op0=ALU.is_ge, op1=ALU.add, accum_out=B[:, 3:4])
                P.i("dve", "tensor_scalar", [bk], [bk], out=B[:, 4:5], in0=B[:, 3:4], scalar1=255.5, scalar2=None, op0=ALU.is_ge)
                P.i("dve", "tensor_tensor", [bk], [bk], out=B[:, 5:6], in0=B[:, 2:3], in1=B[:, 0:1], op=ALU.subtract)
                P.i("dve", "tensor_tensor", [bk], [bk], out=B[:, 7:8], in0=B[:, 1:2], in1=B[:, 2:3], op=ALU.subtract)
                P.i("dve", "scalar_tensor_tensor", [bk], [bk], out=B[:, 0:1], in0=B[:, 5:6], scalar=B[:, 4:5], in1=B[:, 0:1],
                    op0=ALU.mult, op1=ALU.add)
                P.i("dve", "scalar_tensor_tensor", [bk], [bk], out=B[:, 1:2], in0=B[:, 7:8], scalar=B[:, 4:5], in1=B[:, 2:3],
                    op0=ALU.mult, op1=ALU.add)
            P.i("dve", "tensor_scalar", [sk, bk], ["junk"], out=junk[:, 0:2 * hi], in0=sc[p][:, 0:2 * hi], scalar1=B[:, 0:1],
                scalar2=None, op0=ALU.is_ge)
            P.i("dve", "tensor_scalar", ["junk"], [("mbf", p)], out=mbf[p][:, 0:2 * hi], in0=junk[:, 0:2 * hi], scalar1=-1.0,
                scalar2=1.0e30, op0=ALU.add, op1=ALU.mult)
            for r in range(2):
                P.dma(G.dsab[j][:, r * TOK:r * TOK + hi], mbf[p][:, r * hi:(r + 1) * hi], reads=[("mbf", p)], writes=["dsab"])
        P.emit()


PAIRS_A = [[0, 1], [2, 3], [4, 5], [6, 7]]
PAIRS_B = [[0, 2], [1, 3], [4, 6], [5, 7]]
PAIRS_C = [[0, 4], [1, 5], [2, 6], [3, 7]]
QUADS = [[0, 1, 2, 3], [4, 5, 6, 7]]


def phase_out_ln1(nc, P, G, l):
    xin = G.x if l == 0 else G.xres
    with contextlib.ExitStack() as st:
        C = Ctx()
        alloc_xT(st, nc, C, npt=1)
        C.ident = load_const(P, st, nc, "ident", G.ident, [128, 128], BF16, "ident")
        C.lnp = sb(st, nc, "lnp", [128, 2, D], F32)
        P.dma(C.lnp[:], G.lnp[l][0:2, :].partition_broadcast(128), reads=[], writes=["lnp"])
        C.ln_st = [sb(st, nc, "lnst%d" % i, [128, 24], F32) for i in range(2)]
        C.ln_mv = [sb(st, nc, "lnmv%d" % i, [128, 4], F32) for i in range(2)]
        wo = sb(st, nc, "wo", [128, KC, D], BF16)
        wst = [sb(st, nc, "wost%d" % i, [128, D], F32) for i in range(2)]
        for k in range(KC):
            P.dma(wst[k % 2][:], G.w_out_full[l][k * 128:(k + 1) * 128, :], reads=[], writes=[("wost", k % 2)])
            P.i("pool", "tensor_copy", [("wost", k % 2)], [("wo", k)], out=wo[:, k, :], in_=wst[k % 2][:])
        oTt = [sb(st, nc, "oTt%d" % i, [128, 16, 128], BF16) for i in range(2)]
        xs = [sb(st, nc, "xs%d" % i, [128, D], F32) for i in range(2)]
        z = [sb(st, nc, "z%d" % i, [128, D], F32) for i in range(2)]
        x1 = [sb(st, nc, "x1_%d" % i, [128, D], F32) for i in range(2)]
        hps = [ps(st, nc, "hps%d" % i, [128, 512], F32) for i in range(4)]
        for j in range(NT):
            p = j % 2
            P.dma(oTt[p][:], G.oT.rearrange("h d t -> d h t")[:, :, j * 128:(j + 1) * 128], reads=["oT"], writes=[("oTt", p)])
            P.dma(xs[p][:], xin[j * 128:(j + 1) * 128, :], reads=[], writes=[("xs", p)])
            for n in range(4):
                for h in range(16):
                    P.i("pe", "matmul", [("oTt", p), ("wo", h)], [("hps", n)], hps[n][:], lhsT=oTt[p][:, h, :],
                        rhs=wo[:, h, n * 512:(n + 1) * 512], start=(h == 0), stop=(h == 15))
                P.i("dve", "scalar_tensor_tensor", [("hps", n), ("xs", p)], [("z", p)], out=z[p][:, n * 512:(n + 1) * 512],
                    in0=xs[p][:, n * 512:(n + 1) * 512], scalar=ALPHA, in1=hps[n][:], op0=ALU.mult, op1=ALU.add)
            emit_ln(P, C, z[p][:], ("z", p), 0, x1[p][:], ("x1", p), j)
            P.dma(G.xres[j * 128:(j + 1) * 128, :], x1[p][:], reads=[("x1", p)], writes=["xres"])
            emit_xT(P, C, x1[p][:], ("x1", p), j, G.x1T_own, "x1T")
        P.emit()
    src = G.x1T_own.rearrange("k p t -> (k p) t")
    for k in range(8):
        P.collective("AllGather", ALU.bypass, QUADS, src[k * 256:(k + 1) * 256, :],
                     G.ag1[k].rearrange("r t c -> (r t) c"), reads=[], writes=[("ag1", k)])
    P.emit()
    for k in range(8):
        for hh in range(2):
            P.collective("AllGather", ALU.bypass, PAIRS_C, G.ag1[k, 2 * hh:2 * hh + 2].rearrange("r t c -> (r t) c"),
                         G.x1T_all[k, hh].rearrange("c r t d -> (c r t) d"), reads=[], writes=[("agC", k, hh)])
    P.emit()


def phase_moe(nc, P, G, l):
    with contextlib.ExitStack() as st:
        wgu = sb(st, nc, "wgu", [128, KC, D], BF16)
        wdn = sb(st, nc, "wdn", [128, 8, D], BF16)
        wst = [sb(st, nc, "mwst%d" % i, [128, D], F32) for i in range(2)]
        xc = [sb(st, nc, "xc%d" % i, [128, KC, 512], BF16) for i in range(2)]
        actT = sb(st, nc, "actT", [128, 8, 512], BF16)
        tg = [sb(st, nc, "tg%d" % i, [128, 512], F32) for i in range(2)]
        tsg = [sb(st, nc, "tsg%d" % i, [128, 512], F32) for i in range(2)]
        tl = [sb(st, nc, "tl%d" % i, [128, 512], F32) for i in range(2)]
        ysb = [sb(st, nc, "ysb%d" % i, [128, D], F32) for i in range(2)]
        ybf = [sb(st, nc, "ybf%d" % i, [128, D], BF16) for i in range(2)]
        tmp = [sb(st, nc, "ytmp%d" % i, [128, 512], F32) for i in range(2)]
        bdn = sb(st, nc, "bdn", [128, D], F32)
        bgu = sb(st, nc, "bgu", [128, 16], F32)
        rwst = sb(st, nc, "rwst", [128, KC, 32], F32)
        rw = sb(st, nc, "rw", [128, KC, 32], BF16)
        rb = sb(st, nc, "rb", [128, 4, 32], F32)
        Gsb = sb(st, nc, "Gsb", [128, 128, 4], F32)
        lg = [sb(st, nc, "lg%d" % i, [128, 4, 32], F32) for i in range(2)]
        ex = [sb(st, nc, "ex%d" % i, [128, 32], F32) for i in range(2)]
        sel = [sb(st, nc, "sel%d" % i, [128, 32], F32) for i in range(2)]
        rsm = [sb(st, nc, "rsm%d" % i, [128, 16], F32) for i in range(2)]
        hg = [ps(st, nc, "hg%d" % i, [128, 512], F32) for i in range(2)]
        hl = [ps(st, nc, "hl%d" % i, [128, 512], F32) for i in range(2)]
        yp = [ps(st, nc, "yp%d" % i, [128, 512], F32) for i in range(3)]
        prt = ps(st, nc, "prt", [128, 512], F32)
        cn = {"w": 0, "h": 0, "y": 0, "t": 0, "r": 0}
        P.dma(rwst[:], G.rw[l].rearrange("(k p) e -> p k e", p=128), reads=[], writes=["rwst"])
        P.i("pool", "tensor_copy", ["rwst"], ["rw"], out=rw[:], in_=rwst[:])
        for t in range(4):
            P.dma(rb[:, t, :], G.rb[l][0:1, :].broadcast_to([128, 32]), reads=[], writes=["rb"])

        def load_expert(e):
            for k in range(KC):
                w_ = cn["w"] % 2
                cn["w"] += 1
                P.dma(wst[w_][:], G.wgu[l][e, k * 128:(k + 1) * 128, :], reads=[], writes=[("mwst", w_)])
                P.i("pool", "tensor_copy", [("mwst", w_)], [("wgu", k)], out=wgu[:, k, :], in_=wst[w_][:])
            for k in range(8):
                w_ = cn["w"] % 2
                cn["w"] += 1
                P.dma(wst[w_][:], G.wdn[l][e, k * 128:(k + 1) * 128, :], reads=[], writes=[("mwst", w_)])
                P.i("pool", "tensor_copy", [("mwst", w_)], [("wdn", k)], out=wdn[:, k, :], in_=wst[w_][:])
            P.dma(bdn[:], G.bdn[l][e:e + 1, :].broadcast_to([128, D]), reads=[], writes=["bdn"])
            P.dma(bgu[:], G.bgu[l][e], reads=[], writes=["bgu"])

        def x_src(tc):
            s_ = tc // 4
            lc = tc % 4
            rl, hh, rc = s_ & 1, (s_ >> 1) & 1, (s_ >> 2) & 1
            return [(G.x1T_all[k, hh, rc, rl].rearrange("(kk p) t -> p kk t", p=128)[:, :, lc * 512:(lc + 1) * 512], k)
                    for k in range(8)]

        def load_x(tc):
            b = tc % 2
            for (ap, k) in x_src(tc):
                P.dma(xc[b][:, 2 * k:2 * k + 2, :], ap, reads=[], writes=[("xc", b)])

        for e in range(4):
            load_expert(e)
            load_x(0)
            for tc in range(32):
                if tc + 1 < 32:
                    load_x(tc + 1)
                b = tc % 2
                s_ = tc // 4
                lc = tc % 4
                if e == 0:
                    r_ = cn["r"] % 2
                    cn["r"] += 1
                    for t in range(4):
                        for k in range(KC):
                            P.i("pe", "matmul", [("xc", b), "rw"], ["prt"], prt[:, t * 32:(t + 1) * 32],
                                lhsT=xc[b][:, k, t * 128:(t + 1) * 128], rhs=rw[:, k, :], start=(k == 0), stop=(k == KC - 1))
                    P.i("dve", "tensor_tensor", ["prt", "rb"], [("lg", r_)], out=lg[r_][:].rearrange("p t e -> p (t e)"),
                        in0=prt[:, 0:128], in1=rb[:].rearrange("p t e -> p (t e)"), op=ALU.add)
                    for t in range(4):
                        ti = tc * 4 + t
                        q_ = ti % 2
                        P.i("dve", "max", [("lg", r_)], [("rsm", q_)], out=rsm[q_][:, 0:8], in_=lg[r_][:, t, :])
                        P.i("dve", "tensor_scalar", [("rsm", q_)], [("rsm", q_)], out=rsm[q_][:, 8:9], in0=rsm[q_][:, 0:1],
                            scalar1=-1.0, scalar2=None, op0=ALU.mult)
                        P.i("act", "activation", [("lg", r_), ("rsm", q_)], [("ex", q_)], out=ex[q_][:], in_=lg[r_][:, t, :],
                            func=AF.Exp, bias=rsm[q_][:, 8:9], scale=1.0)
                        P.i("dve", "tensor_scalar", [("lg", r_), ("rsm", q_)], [("sel", q_)], out=sel[q_][:], in0=lg[r_][:, t, :],
                            scalar1=rsm[q_][:, 3:4], scalar2=None, op0=ALU.is_ge)
                        P.i("dve", "tensor_tensor", [("sel", q_), ("ex", q_)], [("sel", q_)], out=sel[q_][:], in0=sel[q_][:],
                            in1=ex[q_][:], op=ALU.mult)
                        P.i("dve", "tensor_reduce", [("sel", q_)], [("rsm", q_)], out=rsm[q_][:, 9:10], in_=sel[q_][:],
                            axis=AX.X, op=ALU.add)
                        P.i("dve", "reciprocal", [("rsm", q_)], [("rsm", q_)], out=rsm[q_][:, 9:10], in_=rsm[q_][:, 9:10])
                        P.i("dve", "tensor_scalar", [("sel", q_), ("rsm", q_)], [("Gsb", ti)], out=Gsb[:, ti, :],
                            in0=sel[q_][:, 0:4], scalar1=rsm[q_][:, 9:10], scalar2=None, op0=ALU.mult)
                for fo in range(8):
                    hb_ = cn["h"] % 2
                    cn["h"] += 1
                    for k in range(KC):
                        P.i("pe", "matmul", [("xc", b), ("wgu", k)], [("hg", hb_)], hg[hb_][:],
                            lhsT=wgu[:, k, fo * 128:(fo + 1) * 128], rhs=xc[b][:, k, :], start=(k == 0), stop=(k == KC - 1))
                    for k in range(KC):
                        P.i("pe", "matmul", [("xc", b), ("wgu", k)], [("hl", hb_)], hl[hb_][:],
                            lhsT=wgu[:, k, 1024 + fo * 128:1024 + (fo + 1) * 128], rhs=xc[b][:, k, :], start=(k == 0),
                            stop=(k == KC - 1))
                    P.i("dve", "tensor_scalar", [("hg", hb_), "bgu"], [("tg", hb_)], out=tg[hb_][:], in0=hg[hb_][:],
                        scalar1=bgu[:, fo:fo + 1], scalar2=7.0, op0=ALU.add, op1=ALU.min)
                    P.i("act", "activation", [("tg", hb_)], [("tsg", hb_)], out=tsg[hb_][:], in_=tg[hb_][:], func=AF.Sigmoid,
                        scale=1.702)
                    P.i("dve", "tensor_scalar", [("hl", hb_), "bgu"], [("tl", hb_)], out=tl[hb_][:], in0=hl[hb_][:],
                        scalar1=bgu[:, 8 + fo:9 + fo], scalar2=7.0, op0=ALU.add, op1=ALU.min)
                    P.i("pool", "tensor_scalar", [("tl", hb_)], [("tl", hb_)], out=tl[hb_][:], in0=tl[hb_][:], scalar1=-7.0,
                        scalar2=1.0, op0=ALU.max, op1=ALU.add)
                    P.i("pool", "tensor_tensor", [("tg", hb_), ("tsg", hb_)], [("tg", hb_)], out=tg[hb_][:], in0=tg[hb_][:],
                        in1=tsg[hb_][:], op=ALU.mult)
                    P.i("pool", "tensor_tensor", [("tg", hb_), ("tl", hb_)], [("act", fo)], out=actT[:, fo, :], in0=tg[hb_][:],
                        in1=tl[hb_][:], op=ALU.mult)
                for t in range(4):
                    ti = tc * 4 + t
                    yb_ = cn["t"] % 2
                    cn["t"] += 1
                    row = slice(s_ * TOK + lc * 512 + t * 128, s_ * TOK + lc * 512 + (t + 1) * 128)
                    if e > 0:
                        P.dma(ysb[yb_][:], G.yacc[row, :], reads=[("yacc", ti)], writes=[("ysb", yb_)])
                    for n in range(4):
                        y_ = cn["y"] % 3
                        cn["y"] += 1
                        for fc in range(8):
                            P.i("pe", "matmul", [("act", fc), ("wdn", fc)], [("yp", y_)], yp[y_][:],
                                lhsT=actT[:, fc, t * 128:(t + 1) * 128], rhs=wdn[:, fc, n * 512:(n + 1) * 512],
                                start=(fc == 0), stop=(fc == 7))
                        tm = tmp[cn["y"] % 2]
                        tk = ("ytmp", cn["y"] % 2)
                        P.i("dve", "tensor_tensor", [("yp", y_), "bdn"], [tk], out=tm[:], in0=yp[y_][:],
                            in1=bdn[:, n * 512:(n + 1) * 512], op=ALU.add)
                        if e == 0:
                            P.i("dve", "tensor_scalar", [tk, ("Gsb", ti)], [("ysb", yb_)], out=ysb[yb_][:, n * 512:(n + 1) * 512],
                                in0=tm[:], scalar1=Gsb[:, ti, e:e + 1], scalar2=None, op0=ALU.mult)
                        else:
                            P.i("dve", "scalar_tensor_tensor", [tk, ("Gsb", ti), ("ysb", yb_)], [("ysb", yb_)],
                                out=ysb[yb_][:, n * 512:(n + 1) * 512], in0=tm[:], scalar=Gsb[:, ti, e:e + 1],
                                in1=ysb[yb_][:, n * 512:(n + 1) * 512], op0=ALU.mult, op1=ALU.add)
                    if e < 3:
                        P.dma(G.yacc[row, :], ysb[yb_][:], reads=[("ysb", yb_)], writes=[("yacc", ti)])
                    else:
                        P.i("act", "activation", [("ysb", yb_)], [("ybf", yb_)], out=ybf[yb_][:], in_=ysb[yb_][:], func=AF.Copy)
                        o4, oc = s_ % 4, s_ >> 2
                        tt = lc * 4 + t
                        P.dma(G.ypart[o4, tt // 4, oc, (tt % 4) * 128:(tt % 4 + 1) * 128, :], ybf[yb_][:],
                              reads=[("ybf", yb_)], writes=["ypart"])
        P.emit()
    for o4 in range(4):
        for q4 in range(4):
            P.collective("ReduceScatter", ALU.add, PAIRS_C, G.ypart[o4, q4].rearrange("c t d -> (c t) d"),
                         G.y2[q4, o4], reads=[], writes=[("y2", q4, o4)])
    P.emit()
    for q4 in range(4):
        P.collective("ReduceScatter", ALU.add, QUADS, G.y2[q4].rearrange("c t d -> (c t) d"),
                     G.yown[q4 * 512:(q4 + 1) * 512, :], reads=[], writes=[("yown", q4)])
    P.emit()


def phase_ln2(nc, P, G, l, last):
    with contextlib.ExitStack() as st:
        C = Ctx()
        alloc_xT(st, nc, C, npt=1)
        C.ident = load_const(P, st, nc, "ident", G.ident, [128, 128], BF16, "ident")
        C.lnp = sb(st, nc, "lnp", [128, 2, D], F32)
        P.dma(C.lnp[:], G.lnp[l][2:4, :].partition_broadcast(128), reads=[], writes=["lnp"])
        C.ln_st = [sb(st, nc, "lnst%d" % i, [128, 24], F32) for i in range(2)]
        C.ln_mv = [sb(st, nc, "lnmv%d" % i, [128, 4], F32) for i in range(2)]
        xs = [sb(st, nc, "xs%d" % i, [128, D], F32) for i in range(2)]
        yb = [sb(st, nc, "yb%d" % i, [128, D], BF16) for i in range(2)]
        z = [sb(st, nc, "z%d" % i, [128, D], F32) for i in range(2)]
        x2 = [sb(st, nc, "x2_%d" % i, [128, D], F32) for i in range(2)]
        for j in range(NT):
            p = j % 2
            P.dma(xs[p][:], G.xres[j * 128:(j + 1) * 128, :], reads=[("xres", j)], writes=[("xs", p)])
            P.dma(yb[p][:], G.yown[j * 128:(j + 1) * 128, :], reads=[], writes=[("yb", p)])
            P.i("dve", "scalar_tensor_tensor", [("xs", p), ("yb", p)], [("z", p)], out=z[p][:], in0=xs[p][:], scalar=ALPHA,
                in1=yb[p][:], op0=ALU.mult, op1=ALU.add)
            emit_ln(P, C, z[p][:], ("z", p), 0, x2[p][:], ("x2", p), j)
            if last:
                P.dma(G.out[j * 128:(j + 1) * 128, :], x2[p][:], reads=[("x2", p)], writes=["out"])
            else:
                P.dma(G.xres[j * 128:(j + 1) * 128, :], x2[p][:], reads=[("x2", p), ("xs", p)], writes=[("xres", j)])
                emit_xT(P, C, x2[p][:], ("x2", p), j, G.xT, "xT")
        P.emit()


LAST_INPUT_NAMES = []


def build(nlayers=4, debug=None, stop_after=None, moe=True, only_layer=None):
    nc = bass.Bass("TRN2", target_bir_lowering=False)
    G = Ctx()
    G.stop_after = stop_after

    del LAST_INPUT_NAMES[:]

    def inp(name, shape, dt=F32):
        LAST_INPUT_NAMES.append(name)
        return nc.dram_tensor(name, list(shape), dt, kind="ExternalInput").ap()

    def scr(name, shape, dt=F32):
        return nc.dram_tensor(name, list(shape), dt).ap()

    G.x = inp("x", [TOK, D])
    G.ident = inp("ident", [128, 128], BF16)
    G.identf = inp("identf", [128, 128])
    G.cosT = inp("cosT", [128, TOK])
    G.sinT = inp("sinT", [128, TOK])
    G.cos64T = inp("cos64T", [128, TOK])
    G.sin64T = inp("sin64T", [128, TOK])
    G.kcos = inp("kcos", [128, NT, 32])
    G.ksin = inp("ksin", [128, NT, 32])
    G.maskA = inp("maskA", [128, 128])
    G.maskB = inp("maskB", [128, 128])
    G.dilb = inp("dilb", [128, 2, 9 * 128])
    G.w_in_sh, G.w_out_sh, G.w_in_b, G.w_out_b, G.w_in_full, G.w_out_full = {}, {}, {}, {}, {}, {}
    G.lnp, G.rw, G.rb, G.wgu, G.bgu, G.wdn, G.bdn = {}, {}, {}, {}, {}, {}, {}
    G.idx_g, G.idx_b, G.negbf = {}, {}, {}
    for l in range(nlayers):
        if only_layer is not None and l != only_layer:
            continue
        W = W_IN[l % 4]
        G.w_in_full[l] = inp("l%d_w_in" % l, [D, W])
        G.w_out_full[l] = inp("l%d_w_out" % l, [D, D])
        G.lnp[l] = inp("l%d_lnp" % l, [4, D])
        G.rw[l] = inp("l%d_rw" % l, [D, 32])
        G.rb[l] = inp("l%d_rb" % l, [1, 32])
        if moe:
            G.wgu[l] = inp("l%d_wgu" % l, [4, D, D])
            G.bgu[l] = inp("l%d_bgu" % l, [4, 128, 16])
            G.wdn[l] = inp("l%d_wdn" % l, [4, 1024, D])
            G.bdn[l] = inp("l%d_bdn" % l, [4, D])
        if l % 4 == 1:
            G.idx_g[l] = inp("l%d_idxg" % l, [128, 64])
            G.idx_b[l] = inp("l%d_idxb" % l, [128, 64])
        if l % 4 == 2:
            G.negbf[l] = inp("l%d_bf" % l, [16, 1])
    G.out = nc.dram_tensor("out", [TOK, D], F32, kind="ExternalOutput").ap()
    G.xres = scr("xres", [TOK, D])
    G.xT = scr("xT", [KC, 128, TOK], BF16)
    G.qT = scr("qT", [16, 128, TOK], BF16)
    G.kT_own = scr("kT_own", [16, 128, TOK], BF16)
    G.kT_all = scr("kT_all", [4, 2, 4, 128, TOK], BF16)
    G.v_own = scr("v_own", [TOK, D], BF16)
    G.v_all = scr("v_all", [4, 2, 512, D], BF16)
    G.v_own4 = scr("v_own4", [TOK, 512], BF16)
    G.v_all4 = scr("v_all4", [2, TOK, 512], BF16)
    G.qiT = scr("qiT", [8, 128, TOK], BF16)
    G.kiT_own = scr("kiT_own", [128, TOK], BF16)
    G.kiT_all = scr("kiT_all", [2, 128, TOK], BF16)
    G.wi = scr("wi", [TOK, 16])
    G.sp_own = scr("sp_own", [16, TOK])
    G.sp_all = scr("sp_all", [2, 16, TOK])
    G.negck = scr("negck", [16, 2 * TOK])
    G.dsab = scr("dsab", [NT, 128, 2 * TOK], BF16)
    G.oT = scr("oT", [16, 128, TOK], BF16)
    G.x1T_own = scr("x1T_own", [KC, 128, TOK], BF16)
    G.x1T_all = scr("x1T_allc", [8, 2, 2, 2, 256, TOK], BF16)
    G.ag1 = scr("ag1", [8, 4, 256, TOK], BF16)
    G.yacc = scr("yacc", [8 * TOK, D])
    G.ypart = scr("ypart", [4, 4, 2, 512, D], BF16)
    G.y2 = scr("y2", [4, 4, 512, D], BF16)
    G.yown = scr("yown", [TOK, D], BF16)
    dbg = {}
    if debug:
        for name, shape, dt in debug:
            dbg[name] = nc.dram_tensor("dbg_" + name, list(shape), dt, kind="ExternalOutput").ap()
    G.dbg = dbg

    with contextlib.ExitStack() as st:
        P = Prog(nc, st)
        sems = P.all_sems()
        with nc.Block() as block:
            @block.gpsimd
            def _(e):
                for s in sems:
                    e.sem_clear(s)
        phase_prep(nc, P, G)
        for l in range(nlayers):
            if stop_after in (("weights",), ("prep",)):
                break
            if only_layer is not None and l != only_layer:
                continue
            phase_proj(nc, P, G, l)
            if G.stop_after == ("proj", l):
                break
            if l % 4 == 1:
                phase_dsa_index(nc, P, G, l)
            phase_attn(nc, P, G, l)
            if G.stop_after == ("attn", l):
                break
            phase_out_ln1(nc, P, G, l)
            if G.stop_after == ("ln1", l):
                break
            phase_moe(nc, P, G, l)
            if G.stop_after == ("moe", l):
                break
            phase_ln2(nc, P, G, l, l == nlayers - 1)
        for name, ap in dbg.items():
            src = G.w_out_full[0] if name == "w_out_full0" else getattr(G, name)
            n0 = src.shape[0]
            flat_s = src if len(src.shape) == 2 else src.rearrange(
                {3: "a b c -> (a b) c", 4: "a b c d -> (a b c) d", 5: "a b c d e -> (a b c d) e"}[len(src.shape)])
            flat_d = ap if len(ap.shape) == 2 else ap.rearrange(
                {3: "a b c -> (a b) c", 4: "a b c d -> (a b c) d", 5: "a b c d e -> (a b c d) e"}[len(ap.shape)])
            P.dma(flat_d, flat_s, reads=[], writes=[("dbg", name)])
        fin = sb(st, nc, "fin", [128, 8], F32)
        P.i("dve", "memset", [("dbg", n) for n in dbg] + ["out"], ["fin"], fin[:], 0.0)
        P.emit()
    return nc


def _consts(p):
    t = np.arange(TOK)
    pos = ((2 * (t // 128) + p) * 128 + (t % 128)).astype(np.float32)
    c = {}
    c["ident"] = np.eye(128, dtype=np.float32).astype(ml_dtypes.bfloat16)
    c["identf"] = np.eye(128, dtype=np.float32)
    d = np.arange(128)
    inv = (10000.0 ** (-(np.arange(64, dtype=np.float32)) / 64)).astype(np.float32)
    ang = pos[None, :] * inv[d % 64][:, None]
    c["cosT"] = np.cos(ang).astype(np.float32)
    c["sinT"] = (np.sin(ang) * np.where(d < 64, -1.0, 1.0)[:, None]).astype(np.float32)
    inv32 = (10000.0 ** (-(np.arange(32, dtype=np.float32)) / 32)).astype(np.float32)
    ang = pos[None, :] * inv32[d % 32][:, None]
    c["cos64T"] = np.cos(ang).astype(np.float32)
    c["sin64T"] = (np.sin(ang) * np.where((d % 64) < 32, -1.0, 1.0)[:, None]).astype(np.float32)
    angk = pos.reshape(NT, 128).T[:, :, None] * inv32[None, None, :]
    c["kcos"] = np.cos(angk).astype(np.float32)
    c["ksin"] = np.sin(angk).astype(np.float32)
    qi = np.arange(128)[:, None]
    ki = np.arange(128)[None, :]
    tri = np.where(ki <= qi, 0.0, NEG).astype(np.float32)
    allneg = np.full((128, 128), NEG, np.float32)
    zer = np.zeros((128, 128), np.float32)
    c["maskA"] = tri if p == 0 else zer
    c["maskB"] = allneg if p == 0 else tri
    dil = np.zeros((128, 2, 9, 128), np.float32)
    for r in range(2):
        for kp in range(9):
            m = 2 * (8 - kp) + p - r
            dd = m * 128 + qi - ki
            cnt = ((dd >= 0) & (dd <= 128)).astype(np.int32) + ((dd >= 0) & (dd <= 512) & (dd % 4 == 0)) \
                + ((dd >= 0) & (dd <= 2048) & (dd % 16 == 0))
            dil[:, r, kp, :] = np.where(cnt > 0, np.log(np.maximum(cnt, 1)), NEG)
    c["dilb"] = dil.reshape(128, 2, 9 * 128)
    return c


def make_in_maps(inputs, nlayers=4):
    maps = []
    x = np.asarray(inputs["x"])
    for c in range(NCORES):
        b, p = c // 2, c % 2
        m = dict(_consts(p))
        m["x"] = np.ascontiguousarray(x[b].reshape(16, 2, 128, D)[:, p].reshape(TOK, D))
        for l in range(nlayers):
            pre = "l%d_" % l
            m[pre + "w_in"] = np.asarray(inputs[pre + "w_in"])
            m[pre + "w_out"] = np.asarray(inputs[pre + "w_out"])
            m[pre + "lnp"] = np.stack([inputs[pre + "ln1_g"], inputs[pre + "ln1_b"],
                                       inputs[pre + "ln2_g"], inputs[pre + "ln2_b"]]).astype(np.float32)
            perm = list(range(4 * c, 4 * c + 4)) + [e for e in range(32) if not (4 * c <= e < 4 * c + 4)]
            m[pre + "rw"] = np.ascontiguousarray(np.asarray(inputs[pre + "router_w"])[:, perm])
            m[pre + "rb"] = np.ascontiguousarray(np.asarray(inputs[pre + "router_b"])[perm][None, :])
            m[pre + "wgu"] = np.ascontiguousarray(inputs[pre + "w_gu"][4 * c:4 * c + 4])
            m[pre + "bgu"] = np.ascontiguousarray(np.asarray(inputs[pre + "b_gu"][4 * c:4 * c + 4]).reshape(4, 16, 128).transpose(0, 2, 1))
            m[pre + "wdn"] = np.ascontiguousarray(inputs[pre + "w_dn"][4 * c:4 * c + 4])
            m[pre + "bdn"] = np.ascontiguousarray(inputs[pre + "b_dn"][4 * c:4 * c + 4])
            if l % 4 == 1:
                m[pre + "idxg"] = np.ascontiguousarray(np.broadcast_to(np.asarray(inputs[pre + "idx_norm_g"])[None, :], (128, 64)))
                m[pre + "idxb"] = np.ascontiguousarray(np.broadcast_to(np.asarray(inputs[pre + "idx_norm_b"])[None, :], (128, 64)))
            if l % 4 == 2:
                m[pre + "bf"] = np.ascontiguousarray(np.asarray(inputs[pre + "b_forget"])[:, None])
        maps.append(m)
    return maps


def kernel(**inputs):
    nc = build(4)
    maps = make_in_maps(inputs, 4)
    res = run_bass_kernel_spmd(nc, maps, core_ids=list(range(NCORES)))
    out = np.zeros((4, 4096, D), np.float32)
    for c in range(NCORES):
        b, p = c // 2, c % 2
        out[b].reshape(16, 2, 128, D)[:, p] = res.results[c]["out"].reshape(16, 128, D)
    return out
```

```python
import contextlib
import numpy as np
import ml_dtypes
import concourse.bass as bass
import concourse.mybir as mybir
from concourse.bass_utils import run_bass_kernel_spmd

F32 = mybir.dt.float32
BF16 = mybir.dt.bfloat16
ALU = mybir.AluOpType
AF = mybir.ActivationFunctionType
AX = mybir.AxisListType

NCORES = 8
D = 2048
KC = 16
NT = 16
TOK = 2048
NEG = -1.0e30
ALPHA = 8 ** 0.25
LN_EPS = 1e-5
SCALE = 128 ** -0.5
W_IN = (6144, 4176, 6160, 6144)
PAIRS = [[0, 1], [2, 3], [4, 5], [6, 7]]
WORLD = [list(range(8))]


class Prog:
    CENG = ("pe", "act", "dve", "pool")
    ND = 6

    def __init__(self, nc, stack):
        self.nc = nc
        self.csem = {e: stack.enter_context(nc.semaphore("c_" + e)) for e in self.CENG}
        self.dsem = {q: [stack.enter_context(nc.semaphore("d_%s%d" % (q, i))) for i in range(self.ND)]
                     for q in ("sp", "pool", "act")}
        self.ccsem = [stack.enter_context(nc.semaphore("cc%d" % i)) for i in range(30)]
        self.reset()

    def all_sems(self):
        s = list(self.csem.values()) + list(self.ccsem)
        for q in self.dsem:
            s += self.dsem[q]
        return s

    def reset(self):
        self.ops = {e: [] for e in ("pe", "act", "dve", "pool", "sp")}
        self.cnt = {e: 0 for e in self.CENG}
        self.dcnt = {q: [0] * self.ND for q in self.dsem}
        self.drr = {q: 0 for q in self.dsem}
        self.ccn = 0
        self.last_w = {}
        self.readers = {}
        self.waited = {e: {} for e in self.ops}

    def _deps(self, eng, reads, writes):
        deps = []
        for r in reads:
            if r in self.last_w:
                deps.append(self.last_w[r])
        for w in writes:
            if w in self.last_w:
                deps.append(self.last_w[w])
            deps.extend(self.readers.get(w, ()))
        return deps

    def _finish(self, eng, deps, fn, tok, amt, reads, writes):
        best = {}
        for (sem, v) in deps:
            if eng == "pe" and sem is self.csem["pe"]:
                continue
            k = id(sem)
            if k not in best or best[k][1] < v:
                best[k] = (sem, v)
        waits = []
        wd = self.waited[eng]
        for k, (sem, v) in best.items():
            if wd.get(k, 0) >= v:
                continue
            wd[k] = v
            waits.append((sem, v))
        self.ops[eng].append((waits, fn, tok[0], amt))
        for w in writes:
            self.last_w[w] = tok
            self.readers[w] = []
        for r in reads:
            self.readers.setdefault(r, []).append(tok)

    def op(self, eng, fn, reads=(), writes=()):
        deps = self._deps(eng, reads, writes)
        self.cnt[eng] += 1
        tok = (self.csem[eng], self.cnt[eng])
        self._finish(eng, deps, fn, tok, 1, reads, writes)

    def i(self, eng, name, reads, writes, *args, **kw):
        self.op(eng, lambda e: getattr(e, name)(*args, **kw), reads=reads, writes=writes)

    def dma(self, out, in_, reads=(), writes=(), q="sp"):
        eng = q
        deps = self._deps(eng, reads, writes)
        i = self.drr[q] % self.ND
        self.drr[q] += 1
        sem = self.dsem[q][i]
        if self.dcnt[q][i] > 0:
            deps.append((sem, 16 * self.dcnt[q][i]))
        self.dcnt[q][i] += 1
        tok = (sem, 16 * self.dcnt[q][i])
        self._finish(eng, deps, lambda e: e.dma_start(out=out, in_=in_), tok, 16, reads, writes)

    def collective(self, kind, op, groups, in_ap, out_ap, reads=(), writes=()):
        deps = self._deps("pool", reads, writes)
        sem = self.ccsem[self.ccn]
        self.ccn += 1
        tok = (sem, 1)

        def fn(e):
            return e.collective_compute(kind, op, replica_groups=groups, ins=[in_ap], outs=[out_ap])
        self._finish("pool", deps, fn, tok, None, reads, writes)

    def emit(self):
        nc = self.nc
        ops = self.ops
        for q in self.dsem:
            eng = q
            waits = [(self.dsem[q][i], 16 * self.dcnt[q][i]) for i in range(self.ND) if self.dcnt[q][i] > 0]
            if q == "pool":
                waits += [(self.ccsem[i], 1) for i in range(self.ccn)]
            if waits:
                ops[eng].append((waits, None, None, None))

        def replay(e, lst):
            for waits, fn, sem, amt in lst:
                for (s, v) in waits:
                    e.wait_ge(s, v)
                if fn is None:
                    continue
                ins = fn(e)
                if amt is None:
                    ins.then_inc(sem)
                else:
                    ins.then_inc(sem, amt)

        with nc.Block() as block:
            @block.sync
            def _(e):
                replay(e, ops["sp"])

            @block.scalar
            def _(e):
                replay(e, ops["act"])

            @block.vector
            def _(e):
                replay(e, ops["dve"])

            @block.gpsimd
            def _(e):
                replay(e, ops["pool"])

            @block.tensor
            def _(e):
                replay(e, ops["pe"])
        sems = self.all_sems()
        with nc.Block() as block:
            @block.gpsimd
            def _(e):
                for s in sems:
                    e.sem_clear(s)
        self.reset()


class Ctx:
    pass


_UNIQ = [0]


def sb(st, nc, name, shape, dt):
    _UNIQ[0] += 1
    return st.enter_context(nc.sbuf_tensor("s%d_%s" % (_UNIQ[0], name), shape, dt))


def ps(st, nc, name, shape, dt):
    _UNIQ[0] += 1
    return st.enter_context(nc.psum_tensor("p%d_%s" % (_UNIQ[0], name), shape, dt))


def emit_xT(P, C, src_ap, src_key, j, dstT, tag):
    i = C.xt_i
    C.xt_i += 1
    xb = C.xb[i % 2]
    xtt = C.xtt[i % 2]
    pt = C.ptx[i % 2]
    ptk = ("ptx", i % 2 if C.ptx[0] is not C.ptx[1] else 0)
    P.i("act", "activation", [src_key], [("xb", i % 2)], out=xb[:], in_=src_ap, func=AF.Copy)
    for k in range(KC):
        P.i("pe", "transpose", [("xb", i % 2), "ident"], [ptk], out=pt[:, k * 128:(k + 1) * 128], in_=xb[:, k * 128:(k + 1) * 128],
                                              identity=C.ident[:])
    P.i("dve", "tensor_copy", [ptk], [("xtt", i % 2)], out=xtt[:], in_=pt[:])
    P.dma(dstT.rearrange("k p t -> p k t")[:, :, j * 128:(j + 1) * 128],
          xtt[:].rearrange("p (k t) -> p k t", k=KC),
          reads=[("xtt", i % 2)], writes=[(tag, j)])


def alloc_xT(st, nc, C, npt=2):
    C.xt_i = 0
    C.xb = [sb(st, nc, "xb%d" % i, [128, D], BF16) for i in range(2)]
    C.xtt = [sb(st, nc, "xtt%d" % i, [128, D], BF16) for i in range(2)]
    C.ptx = [ps(st, nc, "ptx%d" % i, [128, D], BF16) for i in range(npt)]
    if npt == 1:
        C.ptx = C.ptx * 2


def load_const(P, st, nc, name, dram_ap, shape, dt, key):
    t = sb(st, nc, name, shape, dt)
    P.dma(t[:], dram_ap, reads=[], writes=[key])
    return t


def emit_ln(P, C, z, zkey, gi, out_ap, out_key, i):
    st6 = C.ln_st[i % 2]
    mv = C.ln_mv[i % 2]
    for c in range(4):
        P.i("dve", "bn_stats", [zkey], [("lnst", i % 2)], out=st6[:, c * 6:(c + 1) * 6], in_=z[:, c * 512:(c + 1) * 512])
    P.i("dve", "bn_aggr", [("lnst", i % 2)], [("lnmv", i % 2)], out=mv[:, 0:2], in_=st6[:])
    P.i("dve", "tensor_scalar", [("lnmv", i % 2)], [("lnmv", i % 2)], out=mv[:, 2:3], in0=mv[:, 1:2], scalar1=LN_EPS, scalar2=None,
        op0=ALU.add)
    P.i("act", "activation", [("lnmv", i % 2)], [("lnmv", i % 2)], out=mv[:, 2:3], in_=mv[:, 2:3], func=AF.Sqrt)
    P.i("dve", "reciprocal", [("lnmv", i % 2)], [("lnmv", i % 2)], out=mv[:, 2:3], in_=mv[:, 2:3])
    P.i("dve", "tensor_scalar", [zkey, ("lnmv", i % 2)], [zkey], out=z, in0=z, scalar1=mv[:, 0:1], scalar2=mv[:, 2:3],
                                          op0=ALU.subtract, op1=ALU.mult)
    P.i("pool", "tensor_tensor", [zkey, "lnp"], [zkey], out=z, in0=z, in1=C.lnp[:, gi, :], op=ALU.mult)
    P.i("pool", "tensor_tensor", [zkey, "lnp"], [out_key], out=out_ap, in0=z, in1=C.lnp[:, gi + 1, :], op=ALU.add)


def phase_prep(nc, P, G):
    with contextlib.ExitStack() as st:
        C = Ctx()
        alloc_xT(st, nc, C)
        C.ident = load_const(P, st, nc, "ident", G.ident, [128, 128], BF16, "ident")
        xs = [sb(st, nc, "xs%d" % i, [128, D], F32) for i in range(2)]
        for j in range(NT):
            P.dma(xs[j % 2][:], G.x[j * 128:(j + 1) * 128, :], reads=[], writes=[("xs", j % 2)])
            emit_xT(P, C, xs[j % 2][:], ("xs", j % 2), j, G.xT, "xT")
        P.emit()


def proj_blocks(kind):
    blocks = []
    if kind in (0, 3):
        for hb in range(8):
            blocks.append(("fm_rope", hb * 256, 256, ("qT", hb * 2)))
        for hb in range(8):
            blocks.append(("fm_rope", 2048 + hb * 256, 256, ("kT", hb * 2)))
        for vb in range(8):
            blocks.append(("tm", 4096 + vb * 256, 256, ("v", vb * 256)))
    elif kind == 2:
        for hb in range(8):
            blocks.append(("fm_plain", hb * 256, 256, ("qT", hb * 2)))
        for hb in range(8):
            blocks.append(("fm_plain", 2048 + hb * 256, 256, ("kT", hb * 2)))
        for vb in range(8):
            blocks.append(("tm", 4096 + vb * 256, 256, ("v", vb * 256)))
        blocks.append(("fox", 6144, 16, None))
    else:
        for hb in range(8):
            blocks.append(("fm_rope", hb * 256, 256, ("qT", hb * 2)))
        for hb in range(2):
            blocks.append(("fm_rope", 2048 + hb * 256, 256, ("kT", hb * 2)))
        for vb in range(2):
            blocks.append(("tm", 2560 + vb * 256, 256, ("v", vb * 256)))
        for hb in range(4):
            blocks.append(("fm_rope64", 3072 + hb * 256, 256, ("qiT", hb * 2)))
        blocks.append(("kiwi", 4096, 80, None))
    return blocks


def phase_proj(nc, P, G, l):
    kind = l % 4
    w_full = G.w_in_full[l]
    W = W_IN[kind]
    with contextlib.ExitStack() as st:
        C = Ctx()
        xT_sb = sb(st, nc, "xT_sb", [128, KC, TOK], BF16)
        for k in range(KC):
            P.dma(xT_sb[:, k, :], G.xT[k], reads=[("xT", j) for j in range(NT)] if k == 0 else [],
                  writes=[("xT_sb", k)])
        xT_keys = [("xT_sb", k) for k in range(KC)]
        cosT = load_const(P, st, nc, "cosT", G.cosT, [128, TOK], F32, "cosT")
        sinT = load_const(P, st, nc, "sinT", G.sinT, [128, TOK], F32, "sinT")
        if kind == 1:
            cos64 = load_const(P, st, nc, "cos64", G.cos64T, [128, TOK], F32, "cos64")
            sin64 = load_const(P, st, nc, "sin64", G.sin64T, [128, TOK], F32, "sin64")
            kcos = load_const(P, st, nc, "kcos", G.kcos, [128, NT, 32], F32, "kcos")
            ksin = load_const(P, st, nc, "ksin", G.ksin, [128, NT, 32], F32, "ksin")
            idxg = load_const(P, st, nc, "idxg", G.idx_g[l], [128, 64], F32, "idxg")
            idxb = load_const(P, st, nc, "idxb", G.idx_b[l], [128, 64], F32, "idxb")
            identf = load_const(P, st, nc, "identf", G.identf, [128, 128], F32, "identf")
        if kind == 2:
            negbf = load_const(P, st, nc, "negbf", G.negbf[l], [16, 1], F32, "negbf")
        wst = [sb(st, nc, "wst%d" % i, [128, KC, 256], F32) for i in range(2)]
        wbf = [sb(st, nc, "wbf%d" % i, [128, KC, 256], BF16) for i in range(2)]
        wsw = [sb(st, nc, "wsw%d" % i, [128, KC, 256], BF16) for i in range(2)]
        pa = [ps(st, nc, "pa%d" % i, [128, 512], F32) for i in range(2)]
        pb = [ps(st, nc, "pb%d" % i, [128, 512], F32) for i in range(2)]
        pv = [ps(st, nc, "pv%d" % i, [128, 512], F32) for i in range(2)]
        t1 = [sb(st, nc, "t1_%d" % i, [128, 512], F32) for i in range(2)]
        t2 = [sb(st, nc, "t2_%d" % i, [128, 512], F32) for i in range(2)]
        ob = [sb(st, nc, "ob%d" % i, [128, 512], BF16) for i in range(2)]
        vb_ = [sb(st, nc, "vbo%d" % i, [128, 256], BF16) for i in range(2)]
        if kind == 1:
            kw = [sb(st, nc, "kw%d" % i, [128, 80], F32) for i in range(2)]
            kst = [sb(st, nc, "kst%d" % i, [128, 8], F32) for i in range(2)]
            kr = [sb(st, nc, "kr%d" % i, [128, 64], F32) for i in range(2)]
            kt_ = [sb(st, nc, "ktmp%d" % i, [128, 64], F32) for i in range(2)]
            kiTs = [sb(st, nc, "kiTs%d" % i, [128, 128], BF16) for i in range(2)]
            wis = [sb(st, nc, "wis%d" % i, [128, 16], F32) for i in range(2)]
            pk = [ps(st, nc, "pk%d" % i, [128, 128], F32) for i in range(2)]
        if kind == 2:
            fx = sb(st, nc, "fx", [16, TOK], F32)
        blocks = proj_blocks(kind)
        cnt = {"a": 0, "v": 0, "o": 0, "k": 0}

        def load_block(bi):
            typ, c0, ncol, _ = blocks[bi]
            s = bi % 2
            P.dma(wst[s][:, :, 0:ncol], w_full[:, c0:c0 + ncol].rearrange("(k p) c -> p k c", p=128),
                  reads=["w_in_full"], writes=[("wst", s)])
            P.i("pool", "tensor_copy", [("wst", s)], [("wbf", s)], out=wbf[s][:, :, 0:ncol], in_=wst[s][:, :, 0:ncol])
            if typ in ("fm_rope", "fm_rope64"):
                hw = 64 if typ == "fm_rope" else 32
                v4 = wbf[s][:].rearrange("p k (g two h) -> p k g two h", two=2, h=hw)
                o4 = wsw[s][:].rearrange("p k (g two h) -> p k g two h", two=2, h=hw)
                for k in range(KC):
                    P.i("pool", "tensor_copy", [("wbf", s)], [("wsw", s)], out=o4[:, k, :, 0, :], in_=v4[:, k, :, 1, :])
                    P.i("pool", "tensor_copy", [("wbf", s)], [("wsw", s)], out=o4[:, k, :, 1, :], in_=v4[:, k, :, 0, :])

        load_block(0)
        for bi, (typ, c0, ncol, dst) in enumerate(blocks):
            if bi + 1 < len(blocks):
                load_block(bi + 1)
            s = bi % 2
            if typ in ("fm_rope", "fm_plain", "fm_rope64"):
                for hh in range(2):
                    if dst[0] == "qT":
                        dram = G.qT[dst[1] + hh]
                    elif dst[0] == "kT":
                        dram = G.kT_own[dst[1] + hh]
                    else:
                        dram = G.qiT[dst[1] + hh]
                    for c4 in range(4):
                        a = cnt["a"] % 2
                        cnt["a"] += 1
                        tsl = slice(c4 * 512, (c4 + 1) * 512)
                        for k in range(KC):
                            P.i("pe", "matmul", [("wbf", s), ("xT_sb", k)], [("pa", a)], pa[a][:], lhsT=wbf[s][:, k, hh * 128:(hh + 1) * 128], rhs=xT_sb[:, k, tsl],
                                start=(k == 0), stop=(k == KC - 1))
                        o = cnt["o"] % 2
                        cnt["o"] += 1
                        if typ == "fm_plain":
                            P.i("act", "activation", [("pa", a)], [("ob", o)], out=ob[o][:], in_=pa[a][:], func=AF.Copy)
                        else:
                            for k in range(KC):
                                P.i("pe", "matmul", [("wsw", s), ("xT_sb", k)], [("pb", a)], pb[a][:], lhsT=wsw[s][:, k, hh * 128:(hh + 1) * 128], rhs=xT_sb[:, k, tsl],
                                    start=(k == 0), stop=(k == KC - 1))
                            ct, sn = (cosT, sinT) if typ == "fm_rope" else (cos64, sin64)
                            ck, sk = ("cosT", "sinT") if typ == "fm_rope" else ("cos64", "sin64")
                            P.i("dve", "tensor_tensor", [("pa", a), ck], [("t1", o)], out=t1[o][:], in0=pa[a][:], in1=ct[:, tsl], op=ALU.mult)
                            P.i("dve", "tensor_tensor", [("pb", a), sk], [("t2", o)], out=t2[o][:], in0=pb[a][:], in1=sn[:, tsl], op=ALU.mult)
                            P.i("pool", "tensor_tensor", [("t1", o), ("t2", o)], [("ob", o)], out=ob[o][:], in0=t1[o][:], in1=t2[o][:],
                                                                        op=ALU.add)
                        P.dma(dram[:, tsl], ob[o][:], reads=[("ob", o)], writes=[dst[0] + "_own"])
            elif typ == "tm":
                for j in range(NT):
                    a = cnt["v"] % 2
                    cnt["v"] += 1
                    for k in range(KC):
                        P.i("pe", "matmul", [("wbf", s), ("xT_sb", k)], [("pv", a)], pv[a][:, 0:ncol], lhsT=xT_sb[:, k, j * 128:(j + 1) * 128], rhs=wbf[s][:, k, 0:ncol],
                            start=(k == 0), stop=(k == KC - 1))
                    P.i("act", "activation", [("pv", a)], [("vbo", a)], out=vb_[a][:, 0:ncol], in_=pv[a][:, 0:ncol], func=AF.Copy)
                    P.dma((G.v_own4 if kind == 1 else G.v_own)[j * 128:(j + 1) * 128, dst[1]:dst[1] + ncol], vb_[a][:, 0:ncol],
                          reads=[("vbo", a)], writes=["v_own"])
            elif typ == "fox":
                for c4 in range(4):
                    a = cnt["a"] % 2
                    cnt["a"] += 1
                    tsl = slice(c4 * 512, (c4 + 1) * 512)
                    for k in range(KC):
                        P.i("pe", "matmul", [("wbf", s), ("xT_sb", k)], [("pa", a)], pa[a][0:16, :], lhsT=wbf[s][:, k, 0:16], rhs=xT_sb[:, k, tsl],
                            start=(k == 0), stop=(k == KC - 1))
                    P.i("dve", "tensor_scalar", [("pa", a), "negbf"], ["fx"], out=fx[:, tsl], in0=pa[a][0:16, :],
                                                                         scalar1=negbf[:, 0:1], scalar2=None, op0=ALU.add)
                P.i("act", "activation", ["fx"], ["fx"], out=fx[:], in_=fx[:], func=AF.Exp, scale=-1.0)
                P.i("act", "activation", ["fx"], ["fx"], out=fx[:], in_=fx[:], func=AF.Ln, bias=1.0, scale=1.0)
                P.dma(G.sp_own[:, :], fx[:], reads=["fx"], writes=["sp_own"])
            elif typ == "kiwi":
                for j in range(NT):
                    a = cnt["k"] % 2
                    cnt["k"] += 1
                    for k in range(KC):
                        P.i("pe", "matmul", [("wbf", s), ("xT_sb", k)], [("pv", a)], pv[a][:, 0:80], lhsT=xT_sb[:, k, j * 128:(j + 1) * 128], rhs=wbf[s][:, k, 0:80],
                            start=(k == 0), stop=(k == KC - 1))
                    P.i("act", "activation", [("pv", a)], [("kw", a)], out=kw[a][:], in_=pv[a][:, 0:80], func=AF.Copy)
                    P.i("dve", "tensor_scalar", [("kw", a)], [("wis", a)], out=wis[a][:], in0=kw[a][:, 64:80],
                                                               scalar1=1.0 / 32.0, scalar2=None, op0=ALU.mult)
                    P.dma(G.wi[j * 128:(j + 1) * 128, :], wis[a][:], reads=[("wis", a)], writes=["wi"])
                    P.i("dve", "bn_stats", [("kw", a)], [("kst", a)], out=kst[a][:, 0:6], in_=kw[a][:, 0:64])
                    P.i("dve", "bn_aggr", [("kst", a)], [("kst", a)], out=kst[a][:, 6:8], in_=kst[a][:, 0:6])
                    P.i("dve", "tensor_scalar", [("kst", a)], [("kst", a)], out=kst[a][:, 0:1], in0=kst[a][:, 7:8], scalar1=LN_EPS,
                        scalar2=None, op0=ALU.add)
                    P.i("act", "activation", [("kst", a)], [("kst", a)], out=kst[a][:, 0:1], in_=kst[a][:, 0:1], func=AF.Sqrt)
                    P.i("dve", "reciprocal", [("kst", a)], [("kst", a)], out=kst[a][:, 0:1], in_=kst[a][:, 0:1])
                    P.i("dve", "tensor_scalar", [("kw", a), ("kst", a)], [("kr", a)], out=kr[a][:], in0=kw[a][:, 0:64], scalar1=kst[a][:, 6:7],
                                                               scalar2=kst[a][:, 0:1], op0=ALU.subtract, op1=ALU.mult)
                    P.i("dve", "tensor_tensor", [("kr", a), "idxg"], [("kr", a)], out=kr[a][:], in0=kr[a][:], in1=idxg[:], op=ALU.mult)
                    P.i("dve", "tensor_tensor", [("kr", a), "idxb"], [("kr", a)], out=kr[a][:], in0=kr[a][:], in1=idxb[:], op=ALU.add)
                    x1 = kr[a][:, 0:32]
                    x2 = kr[a][:, 32:64]
                    P.i("dve", "tensor_tensor", [("kr", a), "kcos"], [("ktmp", a)], out=kt_[a][:, 0:32], in0=x1, in1=kcos[:, j, :],
                                                                           op=ALU.mult)
                    P.i("dve", "tensor_tensor", [("kr", a), "ksin"], [("ktmp", a)], out=kt_[a][:, 32:64], in0=x2, in1=ksin[:, j, :],
                                                                           op=ALU.mult)
                    P.i("dve", "tensor_tensor", [("ktmp", a)], [("kw", a)], out=kw[a][:, 0:32], in0=kt_[a][:, 0:32], in1=kt_[a][:, 32:64],
                                                               op=ALU.subtract)
                    P.i("dve", "tensor_tensor", [("kr", a), "kcos", ("kw", a)], [("ktmp", a)], out=kt_[a][:, 0:32], in0=x2, in1=kcos[:, j, :],
                                                                           op=ALU.mult)
                    P.i("dve", "tensor_tensor", [("kr", a), "ksin"], [("ktmp", a)], out=kt_[a][:, 32:64], in0=x1, in1=ksin[:, j, :],
                                                                           op=ALU.mult)
                    P.i("dve", "tensor_tensor", [("ktmp", a)], [("kw", a)], out=kw[a][:, 32:64], in0=kt_[a][:, 0:32], in1=kt_[a][:, 32:64],
                                                               op=ALU.add)
                    P.i("pe", "transpose", [("kw", a), "identf"], [("pk", a)], out=pk[a][0:64, :], in_=kw[a][:, 0:64], identity=identf[:])
                    P.i("act", "activation", [("pk", a)], [("kiTs", a)], out=kiTs[a][0:64, :], in_=pk[a][0:64, :], func=AF.Copy)
                    P.dma(G.kiT_own[0:64, j * 128:(j + 1) * 128], kiTs[a][0:64, :], reads=[("kiTs", a)],
                          writes=["kiT_own"])
                    P.dma(G.kiT_own[64:128, j * 128:(j + 1) * 128], kiTs[a][0:64, :], reads=[("kiTs", a)],
                          writes=["kiT_own"])
        nkc = 1 if kind == 1 else 4
        for k in range(nkc):
            P.collective("AllGather", ALU.bypass, PAIRS,
                         G.kT_own[4 * k:4 * k + 4].rearrange("h d t -> (h d) t"),
                         G.kT_all[k].rearrange("r h d t -> (r h d) t"),
                         reads=["kT_own"], writes=["kT_all"])
        if kind == 1:
            P.collective("AllGather", ALU.bypass, PAIRS, G.v_own4[:, :], G.v_all4.rearrange("r t c -> (r t) c"),
                         reads=["v_own"], writes=["v_all"])
            P.collective("AllGather", ALU.bypass, PAIRS, G.kiT_own[:, :], G.kiT_all.rearrange("r d t -> (r d) t"),
                         reads=["kiT_own"], writes=["kiT_all"])
        else:
            for k in range(4):
                P.collective("AllGather", ALU.bypass, PAIRS, G.v_own[k * 512:(k + 1) * 512, :],
                             G.v_all[k].rearrange("r t c -> (r t) c"),
                             reads=["v_own"], writes=["v_all"])
        if kind == 2:
            P.collective("AllGather", ALU.bypass, PAIRS, G.sp_own[:, :], G.sp_all.rearrange("r h t -> (r h) t"),
                         reads=["sp_own"], writes=["sp_all"])
        P.emit()


def phase_fox_cum(nc, P, G, l):
    with contextlib.ExitStack() as st:
        spg = sb(st, nc, "spg", [16, 2 * TOK], F32)
        ones = sb(st, nc, "ones", [16, 2 * TOK], F32)
        cum = sb(st, nc, "cum", [16, 2 * TOK], F32)
        for r in range(2):
            P.dma(spg[:].rearrange("h (j r i) -> h j r i", r=2, i=128)[:, :, r, :],
                  G.sp_all[r].rearrange("h (j i) -> h j i", i=128), reads=[], writes=["spg"])
        P.i("pool", "memset", [], ["ones"], ones[:], 1.0)
        P.i("dve", "tensor_tensor_scan", ["spg", "ones"], ["cum"], out=cum[:], data0=ones[:], data1=spg[:],
            initial=0.0, op0=ALU.mult, op1=ALU.add)
        for r in range(2):
            P.dma(G.negck.rearrange("h (r j i) -> h r j i", r=2, i=128)[:, r],
                  cum[:].rearrange("h (j r i) -> h j r i", r=2, i=128)[:, :, r, :], reads=["cum"], writes=["negck"])
        P.emit()


def phase_attn(nc, P, G, l):
    kind = l % 4
    with contextlib.ExitStack() as st:
        ident = load_const(P, st, nc, "ident", G.ident, [128, 128], BF16, "ident")
        maskA = load_const(P, st, nc, "maskA", G.maskA, [128, 128], F32, "maskA")
        maskB = load_const(P, st, nc, "maskB", G.maskB, [128, 128], F32, "maskB")
        masks = (maskA, maskB)
        if kind == 0:
            dilb = load_const(P, st, nc, "dilb", G.dilb, [128, 2, 9 * 128], F32, "dilb")
        nkv = 4 if kind == 1 else 2
        if kind == 1:
            KT = [sb(st, nc, "KT4", [128, 4, 2, TOK], BF16)]
            V = [sb(st, nc, "V4", [128, 2, NT, 512], BF16)]
            QT = [sb(st, nc, "QTj%d" % i, [128, 16, 128], BF16) for i in range(2)]
            Bias = [sb(st, nc, "Bias%d" % i, [128, 2, TOK], BF16) for i in range(2)]
        else:
            KT = [sb(st, nc, "KT%d" % i, [128, 2, TOK], BF16) for i in range(2)]
            V = [sb(st, nc, "V%d" % i, [128, 2, NT, 128], BF16) for i in range(2)]
            QT = [sb(st, nc, "QT%d" % i, [128, TOK], BF16) for i in range(2)]
        if kind == 2:
            Bias = [sb(st, nc, "Bias%d" % i, [128, 2, TOK], F32) for i in range(2)]
        S = [sb(st, nc, "S%d" % i, [128, 2, TOK], F32) for i in range(3)]
        Pb = [sb(st, nc, "Pb%d" % i, [128, 2, TOK], BF16) for i in range(3)]
        PT = [sb(st, nc, "PT%d" % i, [128, 32 * 128], BF16) for i in range(3)]
        sm = [sb(st, nc, "sm%d" % i, [128, 8], F32) for i in range(3)]
        Osb = [sb(st, nc, "Osb%d" % i, [128, 128], BF16) for i in range(3)]
        OT = [sb(st, nc, "OT%d" % i, [128, 128], BF16) for i in range(3)]
        sps = [ps(st, nc, "sps%d" % i, [128, 512], F32) for i in range(3)]
        ptp = [ps(st, nc, "ptp%d" % i, [128, 1024], BF16) for i in range(2)]
        pmisc = ps(st, nc, "pmisc", [128, 512], F32)
        otp = ps(st, nc, "otp", [128, 128], BF16)
        cn = {"sps": 0, "ptp": 0, "it": 0}

        if kind == 3:
            km2 = [sb(st, nc, "km2_%d" % i, [128, 2, NT], F32) for i in range(2)]
            kmT = [sb(st, nc, "kmT%d" % i, [128, NT], BF16) for i in range(2)]
            gm = [sb(st, nc, "gm%d" % i, [128, 16], F32) for i in range(3)]
            m8 = [sb(st, nc, "m8_%d" % i, [128, 8], F32) for i in range(3)]
            bb = [sb(st, nc, "bb%d" % i, [128, 16], F32) for i in range(3)]

        def load_head(h):
            hb = h % 2
            for r in range(2):
                P.dma(KT[hb][:, r, :], G.kT_all[h // 4, r, h % 4], reads=[], writes=[("KT", hb)])
                for ck in range(4):
                    P.dma(V[hb][:, r, ck * 4:(ck + 1) * 4, :],
                          G.v_all[ck, r, :, h * 128:(h + 1) * 128].rearrange("(jj p) d -> p jj d", p=128),
                          reads=[], writes=[("V", hb)])
            P.dma(QT[hb][:], G.qT[h], reads=[], writes=[("QT", hb)])
            if kind == 2:
                P.dma(Bias[hb][:].rearrange("p r t -> p (r t)"), G.negck[h:h + 1, :].broadcast_to([128, 2 * TOK]),
                      reads=["negck"], writes=[("Bias", hb)])
            if kind == 3:
                P.i("dve", "tensor_reduce", [("KT", hb)], [("km2", hb)], out=km2[hb][:],
                    in_=KT[hb][:].rearrange("p r (j i) -> p r j i", i=128), axis=AX.X, op=ALU.add)
                P.i("dve", "tensor_tensor", [("km2", hb)], [("km2", hb)], out=km2[hb][:, 0, :], in0=km2[hb][:, 0, :],
                    in1=km2[hb][:, 1, :], op=ALU.add)
                P.i("dve", "tensor_scalar", [("km2", hb)], [("kmT", hb)], out=kmT[hb][:], in0=km2[hb][:, 0, :],
                    scalar1=1.0 / 256.0, scalar2=None, op0=ALU.mult)

        def core(h, j, qT_ap, qkeys, kt_fn, kkeys, v_fn, vkeys, bias_fn, bkeys):
            it = cn["it"]
            cn["it"] += 1
            p = it % 3
            jlo = max(0, j - 8) if kind == 0 else 0
            lo, hi = jlo * 128, (j + 1) * 128
            skeys = []
            if kind == 3:
                hb = h % 2
                P.i("pe", "matmul", qkeys + [("kmT", hb)], [("pgate", 0)], pmisc[:, 384:400], lhsT=qT_ap, rhs=kmT[hb][:],
                    start=True, stop=True)
                P.i("pool", "memset", [], [("gm", p)], gm[p][:], NEG)
                if j > 0:
                    P.i("dve", "tensor_copy", [("pgate", 0), ("gm", p)], [("gm", p)], out=gm[p][:, 0:j], in_=pmisc[:, 384:384 + j])
                P.i("dve", "max", [("gm", p)], [("m8", p)], out=m8[p][:], in_=gm[p][:])
                P.i("dve", "tensor_scalar", [("gm", p), ("m8", p)], [("bb", p)], out=bb[p][:], in0=gm[p][:],
                    scalar1=m8[p][:, 2:3], scalar2=None, op0=ALU.is_ge)
                P.i("dve", "tensor_scalar", [("bb", p)], [("bb", p)], out=bb[p][:], in0=bb[p][:], scalar1=-1.0,
                    scalar2=1.0e30, op0=ALU.add, op1=ALU.mult)
            for r in range(2):
                if kind == 3:
                    chunks = [(c0, min(512, j * 128 - c0)) for c0 in range(0, j * 128, 512)] + [(j * 128, 128)]
                else:
                    chunks = [(c0, min(512, hi - c0)) for c0 in range(lo, hi, 512)]
                for (c0, w) in chunks:
                    b = cn["sps"] % 3
                    cn["sps"] += 1
                    P.i("pe", "matmul", qkeys + kkeys, [("sps", b)], sps[b][:, 0:w], lhsT=qT_ap, rhs=kt_fn(r, c0, w),
                        start=True, stop=True)
                    key = ("S", p, r, c0)
                    skeys.append(key)
                    if kind == 0:
                        off = c0 - (j - 8) * 128
                        P.i("dve", "scalar_tensor_tensor", [("sps", b), "dilb"], [key], out=S[p][:, r, c0:c0 + w],
                            in0=sps[b][:, 0:w], scalar=SCALE, in1=dilb[:, r, off:off + w], op0=ALU.mult, op1=ALU.add)
                    elif kind == 3:
                        if c0 == j * 128:
                            P.i("dve", "scalar_tensor_tensor", [("sps", b), "maskA", "maskB"], [key],
                                out=S[p][:, r, c0:c0 + w], in0=sps[b][:, 0:w], scalar=SCALE, in1=masks[r][:],
                                op0=ALU.mult, op1=ALU.add)
                        else:
                            nt = w // 128
                            t0 = c0 // 128
                            P.i("dve", "scalar_tensor_tensor", [("sps", b), ("bb", p)], [key],
                                out=S[p][:, r, c0:c0 + w].rearrange("p (t i) -> p t i", i=128),
                                in0=sps[b][:, 0:w].rearrange("p (t i) -> p t i", i=128), scalar=SCALE,
                                in1=bb[p][:, t0:t0 + nt].unsqueeze(2).broadcast_to([128, nt, 128]),
                                op0=ALU.mult, op1=ALU.add)
                    else:
                        P.i("dve", "scalar_tensor_tensor", [("sps", b)] + bkeys, [key], out=S[p][:, r, c0:c0 + w],
                            in0=sps[b][:, 0:w], scalar=SCALE, in1=bias_fn(r, c0, w), op0=ALU.mult, op1=ALU.add)
                if kind in (1, 2):
                    key = [k for k in skeys if k[2] == r][-1]
                    P.i("dve", "tensor_tensor", [key, "maskA", "maskB"], [key], out=S[p][:, r, j * 128:(j + 1) * 128],
                        in0=S[p][:, r, j * 128:(j + 1) * 128], in1=masks[r][:], op=ALU.add)
            P.i("dve", "tensor_reduce", skeys, [("mx", p)], out=sm[p][:, 0:1], in_=S[p][:, :, lo:hi], axis=AX.XY, op=ALU.max)
            P.i("dve", "tensor_scalar", [("mx", p)], [("nmx", p)], out=sm[p][:, 1:2], in0=sm[p][:, 0:1], scalar1=-1.0,
                scalar2=None, op0=ALU.mult)
            P.i("pool", "memset", [], [("rs", p)], sm[p][:, 2:4], 0.0)
            for r in range(2):
                P.i("act", "activation", [k for k in skeys if k[2] == r] + [("nmx", p), ("rs", p)], [("Pb", p, r), ("rs", p)],
                    out=Pb[p][:, r, lo:hi], in_=S[p][:, r, lo:hi], func=AF.Exp, bias=sm[p][:, 1:2], scale=1.0,
                    accum_out=sm[p][:, 2 + r:3 + r])
            P.i("dve", "tensor_tensor", [("rs", p)], [("rinv", p)], out=sm[p][:, 4:5], in0=sm[p][:, 2:3], in1=sm[p][:, 3:4],
                op=ALU.add)
            P.i("dve", "reciprocal", [("rinv", p)], [("rinv", p)], out=sm[p][:, 4:5], in_=sm[p][:, 4:5])
            tiles = [(r, jj) for r in range(2) for jj in range(jlo, j + 1)]
            ngr = (len(tiles) + 7) // 8
            for g in range(ngr):
                grp = tiles[g * 8:(g + 1) * 8]
                pb_ = cn["ptp"] % 2
                cn["ptp"] += 1
                for i_, (r, jj) in enumerate(grp):
                    P.i("pe", "transpose", [("Pb", p, r), "ident"], [("ptp", pb_)], out=ptp[pb_][:, i_ * 128:(i_ + 1) * 128],
                        in_=Pb[p][:, r, jj * 128:(jj + 1) * 128], identity=ident[:])
                n = len(grp)
                eng = "act" if g % 2 == 0 else "dve"
                if eng == "act":
                    P.i("act", "activation", [("ptp", pb_)], [("PT", p, g)], out=PT[p][:, g * 1024:g * 1024 + n * 128],
                        in_=ptp[pb_][:, 0:n * 128], func=AF.Copy)
                else:
                    P.i("dve", "tensor_copy", [("ptp", pb_)], [("PT", p, g)], out=PT[p][:, g * 1024:g * 1024 + n * 128],
                        in_=ptp[pb_][:, 0:n * 128])
            po = pmisc[:, p * 128:(p + 1) * 128]
            for ti, (r, jj) in enumerate(tiles):
                P.i("pe", "matmul", [("PT", p, ti // 8)] + vkeys, [("po", p)], po, lhsT=PT[p][:, ti * 128:(ti + 1) * 128],
                    rhs=v_fn(r, jj), start=(ti == 0), stop=(ti == len(tiles) - 1))
            P.i("dve", "tensor_scalar", [("po", p), ("rinv", p)], [("Osb", p)], out=Osb[p][:], in0=po, scalar1=sm[p][:, 4:5],
                scalar2=None, op0=ALU.mult)
            P.i("pe", "transpose", [("Osb", p), "ident"], [("otp", 0)], out=otp[:], in_=Osb[p][:], identity=ident[:])
            P.i("act", "activation", [("otp", 0)], [("OT", p)], out=OT[p][:], in_=otp[:], func=AF.Copy)
            P.dma(G.oT[h][:, j * 128:(j + 1) * 128], OT[p][:], reads=[("OT", p)], writes=["oT"])

        if kind == 1:
            for r in range(2):
                for kv in range(4):
                    P.dma(KT[0][:, kv, r, :], G.kT_all[0, r, kv], reads=[], writes=["KT4"])
                P.dma(V[0][:, r, :, :], G.v_all4[r].rearrange("(j p) c -> p j c", p=128), reads=[], writes=["V4"])
            for j in range(NT):
                jb = j % 2
                P.dma(QT[jb][:], G.qT.rearrange("h d t -> d h t")[:, :, j * 128:(j + 1) * 128], reads=[], writes=[("QT", jb)])
                P.dma(Bias[jb][:].rearrange("p r t -> p (r t)"), G.dsab[j], reads=["dsab"], writes=[("Bias", jb)])
                for h in range(16):
                    kv = h // 4
                    core(h, j, QT[jb][:, h, :], [("QT", jb)],
                         lambda r, c0, w, kv=kv: KT[0][:, kv, r, c0:c0 + w], ["KT4"],
                         lambda r, jj, kv=kv: V[0][:, r, jj, kv * 128:(kv + 1) * 128], ["V4"],
                         lambda r, c0, w, jb=jb: Bias[jb][:, r, c0:c0 + w], [("Bias", jb)])
        else:
            load_head(0)
            for h in range(16):
                if h + 1 < 16:
                    load_head(h + 1)
                hb = h % 2
                for j in range(NT):
                    core(h, j, QT[hb][:, j * 128:(j + 1) * 128], [("QT", hb)],
                         lambda r, c0, w, hb=hb: KT[hb][:, r, c0:c0 + w], [("KT", hb)],
                         lambda r, jj, hb=hb: V[hb][:, r, jj, :], [("V", hb)],
                         (lambda r, c0, w, hb=hb: Bias[hb][:, r, c0:c0 + w]) if kind == 2 else None,
                         [("Bias", hb)] if kind == 2 else [])
        P.emit()


def phase_dsa_index(nc, P, G, l):
    with contextlib.ExitStack() as st:
        maskA = load_const(P, st, nc, "maskA", G.maskA, [128, 128], F32, "maskA")
        maskB = load_const(P, st, nc, "maskB", G.maskB, [128, 128], F32, "maskB")
        masks = (maskA, maskB)
        kiT = sb(st, nc, "kiT", [128, 2, TOK], BF16)
        for r in range(2):
            P.dma(kiT[:, r, :], G.kiT_all[r], reads=[], writes=["kiT"])
        qiT = [sb(st, nc, "qiT%d" % i, [128, 8, 128], BF16) for i in range(2)]
        wi = [sb(st, nc, "wi%d" % i, [128, 16], F32) for i in range(2)]
        sc = [sb(st, nc, "sc%d" % i, [128, 2 * TOK], F32) for i in range(2)]
        junk = sb(st, nc, "junk", [128, 2 * TOK], F32)
        mbf = [sb(st, nc, "mbf%d" % i, [128, 2 * TOK], BF16) for i in range(2)]
        rl = [sb(st, nc, "rl%d" % i, [128, 512], F32) for i in range(3)]
        bs = [sb(st, nc, "bs%d" % i, [128, 8], F32) for i in range(2)]
        lps = [ps(st, nc, "lps%d" % i, [128, 512], F32) for i in range(4)]
        cn = {"l": 0, "r": 0}
        for j in range(NT):
            p = j % 2
            hi = (j + 1) * 128
            P.dma(qiT[p][:], G.qiT.rearrange("g d t -> d g t")[:, :, j * 128:(j + 1) * 128], reads=[], writes=[("qiT", p)])
            P.dma(wi[p][:], G.wi[j * 128:(j + 1) * 128, :], reads=[], writes=[("wi", p)])
            for r in range(2):
                for c0 in range(0, hi, 512):
                    w = min(512, hi - c0)
                    key = ("sc", p, r, c0)
                    dst = sc[p][:, r * hi + c0:r * hi + c0 + w]
                    for ih in range(16):
                        g, half = ih // 2, ih % 2
                        b = cn["l"] % 4
                        cn["l"] += 1
                        P.i("pe", "matmul", [("qiT", p), "kiT"], [("lps", b)], lps[b][:, 0:w],
                            lhsT=qiT[p][half * 64:(half + 1) * 64, g, :], rhs=kiT[half * 64:(half + 1) * 64, r, c0:c0 + w],
                            start=True, stop=True)
                        q_ = cn["r"] % 3
                        cn["r"] += 1
                        P.i("act", "activation", [("lps", b)], [("rl", q_)], out=rl[q_][:, 0:w], in_=lps[b][:, 0:w], func=AF.Relu)
                        if ih == 0:
                            P.i("dve", "tensor_scalar", [("rl", q_), ("wi", p)], [key], out=dst, in0=rl[q_][:, 0:w],
                                scalar1=wi[p][:, 0:1], scalar2=None, op0=ALU.mult)
                        else:
                            P.i("dve", "scalar_tensor_tensor", [("rl", q_), ("wi", p), key], [key], out=dst, in0=rl[q_][:, 0:w],
                                scalar=wi[p][:, ih:ih + 1], in1=dst, op0=ALU.mult, op1=ALU.add)
            allk = [("sc", p, r, c0) for r in range(2) for c0 in range(0, hi, 512)]
            sk = ("scall", p)
            B = bs[p]
            bk = ("bs", p)
            P.i("dve", "tensor_reduce", allk, [bk], out=B[:, 6:7], in_=sc[p][:, 0:2 * hi], axis=AX.X, op=ALU.max,
                apply_absolute_value=True)
            for r in range(2):
                sl = sc[p][:, r * hi + j * 128:r * hi + (j + 1) * 128]
                P.i("dve", "tensor_tensor", allk + [bk, "maskA", "maskB"], [sk], out=sl, in0=sl, in1=masks[r][:], op=ALU.add)
            P.i("dve", "tensor_scalar", [bk], [bk], out=B[:, 1:2], in0=B[:, 6:7], scalar1=1.0, scalar2=None, op0=ALU.add)
            P.i("dve", "tensor_scalar", [bk], [bk], out=B[:, 0:1], in0=B[:, 1:2], scalar1=-1.0, scalar2=None, op0=ALU.mult)
            for it in range(26):
                P.i("dve", "tensor_scalar", [bk], [bk], out=B[:, 2:3], in0=B[:, 0:1], scalar1=B[:, 1:2], scalar2=0.5,
                    op0=ALU.add, op1=ALU.mult)
                P.i("dve", "tensor_scalar", [sk, bk], ["junk", bk], out=junk[:, 0:2 * hi], in0=sc[p][:, 0:2 * hi],
                    scalar1=B[:, 2:3], scalar2=0.0, op0=ALU.is_ge, op1=ALU.add, accum_out=B[:, 3:4])
                P.i("dve", "tensor_scalar", [bk], [bk], out=B[:, 4:5], in0=B[:, 3:4], scalar1=255.5, scalar2=None, op0=ALU.is_ge)
                P.i("dve", "tensor_tensor", [bk], [bk], out=B[:, 5:6], in0=B[:, 2:3], in1=B[:, 0:1], op=ALU.subtract)
                P.i("dve", "tensor_tensor", [bk], [bk], out=B[:, 7:8], in0=B[:, 1:2], in1=B[:, 2:3], op=ALU.subtract)
                P.i("dve", "scalar_tensor_tensor", [bk], [bk], out=B[:, 0:1], in0=B[:, 5:6], scalar=B[:, 4:5], in1=B[:, 0:1],
                    op0=ALU.mult, op1=ALU.add)
                P.i("dve", "scalar_tensor_tensor", [bk], [bk], out=B[:, 1:2], in0=B[:, 7:8], scalar=B[:, 4:5], in1=B[:, 2:3],
                    op0=ALU.mult, op1=ALU.add)
            P.i("dve", "tensor_scalar", [sk, bk], ["junk"], out=junk[:, 0:2 * hi], in0=sc[p][:, 0:2 * hi], scalar1=B[:, 0:1],
                scalar2=None, op0=ALU.is_ge)
            P.i("dve", "tensor_scalar", ["junk"], [("mbf", p)], out=mbf[p][:, 0:2 * hi], in0=junk[:, 0:2 * hi], scalar1=-1.0,
                scalar2=1.0e30, op0=ALU.add, op1=ALU.mult)
            for r in range(2):
                P.dma(G.dsab[j][:, r * TOK:r * TOK + hi], mbf[p][:, r * hi:(r + 1) * hi], reads=[("mbf", p)], writes=["dsab"])
        P.emit()


PAIRS_A = [[0, 1], [2, 3], [4, 5], [6, 7]]
PAIRS_B = [[0, 2], [1, 3], [4, 6], [5, 7]]
PAIRS_C = [[0, 4], [1, 5], [2, 6], [3, 7]]
QUADS = [[0, 1, 2, 3], [4, 5, 6, 7]]


def phase_out_ln1(nc, P, G, l):
    xin = G.x if l == 0 else G.xres
    with contextlib.ExitStack() as st:
        C = Ctx()
        alloc_xT(st, nc, C, npt=1)
        C.ident = load_const(P, st, nc, "ident", G.ident, [128, 128], BF16, "ident")
        C.lnp = sb(st, nc, "lnp", [128, 2, D], F32)
        P.dma(C.lnp[:], G.lnp[l][0:2, :].partition_broadcast(128), reads=[], writes=["lnp"])
        C.ln_st = [sb(st, nc, "lnst%d" % i, [128, 24], F32) for i in range(2)]
        C.ln_mv = [sb(st, nc, "lnmv%d" % i, [128, 4], F32) for i in range(2)]
        wo = sb(st, nc, "wo", [128, KC, D], BF16)
        wst = [sb(st, nc, "wost%d" % i, [128, D], F32) for i in range(2)]
        for k in range(KC):
            P.dma(wst[k % 2][:], G.w_out_full[l][k * 128:(k + 1) * 128, :], reads=[], writes=[("wost", k % 2)])
            P.i("pool", "tensor_copy", [("wost", k % 2)], [("wo", k)], out=wo[:, k, :], in_=wst[k % 2][:])
        oTt = [sb(st, nc, "oTt%d" % i, [128, 16, 128], BF16) for i in range(2)]
        xs = [sb(st, nc, "xs%d" % i, [128, D], F32) for i in range(2)]
        z = [sb(st, nc, "z%d" % i, [128, D], F32) for i in range(2)]
        x1 = [sb(st, nc, "x1_%d" % i, [128, D], F32) for i in range(2)]
        hps = [ps(st, nc, "hps%d" % i, [128, 512], F32) for i in range(4)]
        for j in range(NT):
            p = j % 2
            P.dma(oTt[p][:], G.oT.rearrange("h d t -> d h t")[:, :, j * 128:(j + 1) * 128], reads=["oT"], writes=[("oTt", p)])
            P.dma(xs[p][:], xin[j * 128:(j + 1) * 128, :], reads=[], writes=[("xs", p)])
            for n in range(4):
                for h in range(16):
                    P.i("pe", "matmul", [("oTt", p), ("wo", h)], [("hps", n)], hps[n][:], lhsT=oTt[p][:, h, :],
                        rhs=wo[:, h, n * 512:(n + 1) * 512], start=(h == 0), stop=(h == 15))
                P.i("dve", "scalar_tensor_tensor", [("hps", n), ("xs", p)], [("z", p)], out=z[p][:, n * 512:(n + 1) * 512],
                    in0=xs[p][:, n * 512:(n + 1) * 512], scalar=ALPHA, in1=hps[n][:], op0=ALU.mult, op1=ALU.add)
            emit_ln(P, C, z[p][:], ("z", p), 0, x1[p][:], ("x1", p), j)
            P.dma(G.xres[j * 128:(j + 1) * 128, :], x1[p][:], reads=[("x1", p)], writes=["xres"])
            emit_xT(P, C, x1[p][:], ("x1", p), j, G.x1T_own, "x1T")
        P.emit()
    src = G.x1T_own.rearrange("k p t -> (k p) t")
    for k in range(8):
        P.collective("AllGather", ALU.bypass, QUADS, src[k * 256:(k + 1) * 256, :],
                     G.ag1[k].rearrange("r t c -> (r t) c"), reads=[], writes=[("ag1", k)])
    P.emit()
    for k in range(8):
        for hh in range(2):
            P.collective("AllGather", ALU.bypass, PAIRS_C, G.ag1[k, 2 * hh:2 * hh + 2].rearrange("r t c -> (r t) c"),
                         G.x1T_all[k, hh].rearrange("c r t d -> (c r t) d"), reads=[], writes=[("agC", k, hh)])
    P.emit()


def phase_moe(nc, P, G, l):
    with contextlib.ExitStack() as st:
        wgu = sb(st, nc, "wgu", [128, KC, D], BF16)
        wdn = sb(st, nc, "wdn", [128, 8, D], BF16)
        wst = [sb(st, nc, "mwst%d" % i, [128, D], F32) for i in range(2)]
        xc = [sb(st, nc, "xc%d" % i, [128, KC, 512], BF16) for i in range(2)]
        actT = sb(st, nc, "actT", [128, 8, 512], BF16)
        tg = [sb(st, nc, "tg%d" % i, [128, 512], F32) for i in range(2)]
        tsg = [sb(st, nc, "tsg%d" % i, [128, 512], F32) for i in range(2)]
        tl = [sb(st, nc, "tl%d" % i, [128, 512], F32) for i in range(2)]
        ysb = [sb(st, nc, "ysb%d" % i, [128, D], F32) for i in range(2)]
        ybf = [sb(st, nc, "ybf%d" % i, [128, D], BF16) for i in range(2)]
        tmp = [sb(st, nc, "ytmp%d" % i, [128, 512], F32) for i in range(2)]
        bdn = sb(st, nc, "bdn", [128, D], F32)
        bgu = sb(st, nc, "bgu", [128, 16], F32)
        rwst = sb(st, nc, "rwst", [128, KC, 32], F32)
        rw = sb(st, nc, "rw", [128, KC, 32], BF16)
        rb = sb(st, nc, "rb", [128, 4, 32], F32)
        Gsb = sb(st, nc, "Gsb", [128, 128, 4], F32)
        lg = [sb(st, nc, "lg%d" % i, [128, 4, 32], F32) for i in range(2)]
        ex = [sb(st, nc, "ex%d" % i, [128, 32], F32) for i in range(2)]
        sel = [sb(st, nc, "sel%d" % i, [128, 32], F32) for i in range(2)]
        rsm = [sb(st, nc, "rsm%d" % i, [128, 16], F32) for i in range(2)]
        hg = [ps(st, nc, "hg%d" % i, [128, 512], F32) for i in range(2)]
        hl = [ps(st, nc, "hl%d" % i, [128, 512], F32) for i in range(2)]
        yp = [ps(st, nc, "yp%d" % i, [128, 512], F32) for i in range(3)]
        prt = ps(st, nc, "prt", [128, 512], F32)
        cn = {"w": 0, "h": 0, "y": 0, "t": 0, "r": 0}
        P.dma(rwst[:], G.rw[l].rearrange("(k p) e -> p k e", p=128), reads=[], writes=["rwst"])
        P.i("pool", "tensor_copy", ["rwst"], ["rw"], out=rw[:], in_=rwst[:])
        for t in range(4):
            P.dma(rb[:, t, :], G.rb[l][0:1, :].broadcast_to([128, 32]), reads=[], writes=["rb"])

        def load_expert(e):
            for k in range(KC):
                w_ = cn["w"] % 2
                cn["w"] += 1
                P.dma(wst[w_][:], G.wgu[l][e, k * 128:(k + 1) * 128, :], reads=[], writes=[("mwst", w_)])
                P.i("pool", "tensor_copy", [("mwst", w_)], [("wgu", k)], out=wgu[:, k, :], in_=wst[w_][:])
            for k in range(8):
                w_ = cn["w"] % 2
                cn["w"] += 1
                P.dma(wst[w_][:], G.wdn[l][e, k * 128:(k + 1) * 128, :], reads=[], writes=[("mwst", w_)])
                P.i("pool", "tensor_copy", [("mwst", w_)], [("wdn", k)], out=wdn[:, k, :], in_=wst[w_][:])
            P.dma(bdn[:], G.bdn[l][e:e + 1, :].broadcast_to([128, D]), reads=[], writes=["bdn"])
            P.dma(bgu[:], G.bgu[l][e], reads=[], writes=["bgu"])

        def x_src(tc):
            s_ = tc // 4
            lc = tc % 4
            rl, hh, rc = s_ & 1, (s_ >> 1) & 1, (s_ >> 2) & 1
            return [(G.x1T_all[k, hh, rc, rl].rearrange("(kk p) t -> p kk t", p=128)[:, :, lc * 512:(lc + 1) * 512], k)
                    for k in range(8)]

        def load_x(tc):
            b = tc % 2
            for (ap, k) in x_src(tc):
                P.dma(xc[b][:, 2 * k:2 * k + 2, :], ap, reads=[], writes=[("xc", b)])

        for e in range(4):
            load_expert(e)
            load_x(0)
            for tc in range(32):
                if tc + 1 < 32:
                    load_x(tc + 1)
                b = tc % 2
                s_ = tc // 4
                lc = tc % 4
                if e == 0:
                    r_ = cn["r"] % 2
                    cn["r"] += 1
                    for t in range(4):
                        for k in range(KC):
                            P.i("pe", "matmul", [("xc", b), "rw"], ["prt"], prt[:, t * 32:(t + 1) * 32],
                                lhsT=xc[b][:, k, t * 128:(t + 1) * 128], rhs=rw[:, k, :], start=(k == 0), stop=(k == KC - 1))
                    P.i("dve", "tensor_tensor", ["prt", "rb"], [("lg", r_)], out=lg[r_][:].rearrange("p t e -> p (t e)"),
                        in0=prt[:, 0:128], in1=rb[:].rearrange("p t e -> p (t e)"), op=ALU.add)
                    for t in range(4):
                        ti = tc * 4 + t
                        q_ = ti % 2
                        P.i("dve", "max", [("lg", r_)], [("rsm", q_)], out=rsm[q_][:, 0:8], in_=lg[r_][:, t, :])
                        P.i("dve", "tensor_scalar", [("rsm", q_)], [("rsm", q_)], out=rsm[q_][:, 8:9], in0=rsm[q_][:, 0:1],
                            scalar1=-1.0, scalar2=None, op0=ALU.mult)
                        P.i("act", "activation", [("lg", r_), ("rsm", q_)], [("ex", q_)], out=ex[q_][:], in_=lg[r_][:, t, :],
                            func=AF.Exp, bias=rsm[q_][:, 8:9], scale=1.0)
                        P.i("dve", "tensor_scalar", [("lg", r_), ("rsm", q_)], [("sel", q_)], out=sel[q_][:], in0=lg[r_][:, t, :],
                            scalar1=rsm[q_][:, 3:4], scalar2=None, op0=ALU.is_ge)
                        P.i("dve", "tensor_tensor", [("sel", q_), ("ex", q_)], [("sel", q_)], out=sel[q_][:], in0=sel[q_][:],
                            in1=ex[q_][:], op=ALU.mult)
                        P.i("dve", "tensor_reduce", [("sel", q_)], [("rsm", q_)], out=rsm[q_][:, 9:10], in_=sel[q_][:],
                            axis=AX.X, op=ALU.add)
                        P.i("dve", "reciprocal", [("rsm", q_)], [("rsm", q_)], out=rsm[q_][:, 9:10], in_=rsm[q_][:, 9:10])
                        P.i("dve", "tensor_scalar", [("sel", q_), ("rsm", q_)], [("Gsb", ti)], out=Gsb[:, ti, :],
                            in0=sel[q_][:, 0:4], scalar1=rsm[q_][:, 9:10], scalar2=None, op0=ALU.mult)
                for fo in range(8):
                    hb_ = cn["h"] % 2
                    cn["h"] += 1
                    for k in range(KC):
                        P.i("pe", "matmul", [("xc", b), ("wgu", k)], [("hg", hb_)], hg[hb_][:],
                            lhsT=wgu[:, k, fo * 128:(fo + 1) * 128], rhs=xc[b][:, k, :], start=(k == 0), stop=(k == KC - 1))
                    for k in range(KC):
                        P.i("pe", "matmul", [("xc", b), ("wgu", k)], [("hl", hb_)], hl[hb_][:],
                            lhsT=wgu[:, k, 1024 + fo * 128:1024 + (fo + 1) * 128], rhs=xc[b][:, k, :], start=(k == 0),
                            stop=(k == KC - 1))
                    P.i("dve", "tensor_scalar", [("hg", hb_), "bgu"], [("tg", hb_)], out=tg[hb_][:], in0=hg[hb_][:],
                        scalar1=bgu[:, fo:fo + 1], scalar2=7.0, op0=ALU.add, op1=ALU.min)
                    P.i("act", "activation", [("tg", hb_)], [("tsg", hb_)], out=tsg[hb_][:], in_=tg[hb_][:], func=AF.Sigmoid,
                        scale=1.702)
                    P.i("dve", "tensor_scalar", [("hl", hb_), "bgu"], [("tl", hb_)], out=tl[hb_][:], in0=hl[hb_][:],
                        scalar1=bgu[:, 8 + fo:9 + fo], scalar2=7.0, op0=ALU.add, op1=ALU.min)
                    P.i("pool", "tensor_scalar", [("tl", hb_)], [("tl", hb_)], out=tl[hb_][:], in0=tl[hb_][:], scalar1=-7.0,
                        scalar2=1.0, op0=ALU.max, op1=ALU.add)
                    P.i("pool", "tensor_tensor", [("tg", hb_), ("tsg", hb_)], [("tg", hb_)], out=tg[hb_][:], in0=tg[hb_][:],
                        in1=tsg[hb_][:], op=ALU.mult)
                    P.i("pool", "tensor_tensor", [("tg", hb_), ("tl", hb_)], [("act", fo)], out=actT[:, fo, :], in0=tg[hb_][:],
                        in1=tl[hb_][:], op=ALU.mult)
                for t in range(4):
                    ti = tc * 4 + t
                    yb_ = cn["t"] % 2
                    cn["t"] += 1
                    row = slice(s_ * TOK + lc * 512 + t * 128, s_ * TOK + lc * 512 + (t + 1) * 128)
                    if e > 0:
                        P.dma(ysb[yb_][:], G.yacc[row, :], reads=[("yacc", ti)], writes=[("ysb", yb_)])
                    for n in range(4):
                        y_ = cn["y"] % 3
                        cn["y"] += 1
                        for fc in range(8):
                            P.i("pe", "matmul", [("act", fc), ("wdn", fc)], [("yp", y_)], yp[y_][:],
                                lhsT=actT[:, fc, t * 128:(t + 1) * 128], rhs=wdn[:, fc, n * 512:(n + 1) * 512],
                                start=(fc == 0), stop=(fc == 7))
                        tm = tmp[cn["y"] % 2]
                        tk = ("ytmp", cn["y"] % 2)
                        P.i("dve", "tensor_tensor", [("yp", y_), "bdn"], [tk], out=tm[:], in0=yp[y_][:],
                            in1=bdn[:, n * 512:(n + 1) * 512], op=ALU.add)
                        if e == 0:
                            P.i("dve", "tensor_scalar", [tk, ("Gsb", ti)], [("ysb", yb_)], out=ysb[yb_][:, n * 512:(n + 1) * 512],
                                in0=tm[:], scalar1=Gsb[:, ti, e:e + 1], scalar2=None, op0=ALU.mult)
                        else:
                            P.i("dve", "scalar_tensor_tensor", [tk, ("Gsb", ti), ("ysb", yb_)], [("ysb", yb_)],
                                out=ysb[yb_][:, n * 512:(n + 1) * 512], in0=tm[:], scalar=Gsb[:, ti, e:e + 1],
                                in1=ysb[yb_][:, n * 512:(n + 1) * 512], op0=ALU.mult, op1=ALU.add)
                    if e < 3:
                        P.dma(G.yacc[row, :], ysb[yb_][:], reads=[("ysb", yb_)], writes=[("yacc", ti)])
                    else:
                        P.i("act", "activation", [("ysb", yb_)], [("ybf", yb_)], out=ybf[yb_][:], in_=ysb[yb_][:], func=AF.Copy)
                        o4, oc = s_ % 4, s_ >> 2
                        tt = lc * 4 + t
                        P.dma(G.ypart[o4, tt // 4, oc, (tt % 4) * 128:(tt % 4 + 1) * 128, :], ybf[yb_][:],
                              reads=[("ybf", yb_)], writes=["ypart"])
        P.emit()
    for o4 in range(4):
        for q4 in range(4):
            P.collective("ReduceScatter", ALU.add, PAIRS_C, G.ypart[o4, q4].rearrange("c t d -> (c t) d"),
                         G.y2[q4, o4], reads=[], writes=[("y2", q4, o4)])
    P.emit()
    for q4 in range(4):
        P.collective("ReduceScatter", ALU.add, QUADS, G.y2[q4].rearrange("c t d -> (c t) d"),
                     G.yown[q4 * 512:(q4 + 1) * 512, :], reads=[], writes=[("yown", q4)])
    P.emit()


def phase_ln2(nc, P, G, l, last):
    with contextlib.ExitStack() as st:
        C = Ctx()
        alloc_xT(st, nc, C, npt=1)
        C.ident = load_const(P, st, nc, "ident", G.ident, [128, 128], BF16, "ident")
        C.lnp = sb(st, nc, "lnp", [128, 2, D], F32)
        P.dma(C.lnp[:], G.lnp[l][2:4, :].partition_broadcast(128), reads=[], writes=["lnp"])
        C.ln_st = [sb(st, nc, "lnst%d" % i, [128, 24], F32) for i in range(2)]
        C.ln_mv = [sb(st, nc, "lnmv%d" % i, [128, 4], F32) for i in range(2)]
        xs = [sb(st, nc, "xs%d" % i, [128, D], F32) for i in range(2)]
        yb = [sb(st, nc, "yb%d" % i, [128, D], BF16) for i in range(2)]
        z = [sb(st, nc, "z%d" % i, [128, D], F32) for i in range(2)]
        x2 = [sb(st, nc, "x2_%d" % i, [128, D], F32) for i in range(2)]
        for j in range(NT):
            p = j % 2
            P.dma(xs[p][:], G.xres[j * 128:(j + 1) * 128, :], reads=[("xres", j)], writes=[("xs", p)])
            P.dma(yb[p][:], G.yown[j * 128:(j + 1) * 128, :], reads=[], writes=[("yb", p)])
            P.i("dve", "scalar_tensor_tensor", [("xs", p), ("yb", p)], [("z", p)], out=z[p][:], in0=xs[p][:], scalar=ALPHA,
                in1=yb[p][:], op0=ALU.mult, op1=ALU.add)
            emit_ln(P, C, z[p][:], ("z", p), 0, x2[p][:], ("x2", p), j)
            if last:
                P.dma(G.out[j * 128:(j + 1) * 128, :], x2[p][:], reads=[("x2", p)], writes=["out"])
            else:
                P.dma(G.xres[j * 128:(j + 1) * 128, :], x2[p][:], reads=[("x2", p), ("xs", p)], writes=[("xres", j)])
                emit_xT(P, C, x2[p][:], ("x2", p), j, G.xT, "xT")
        P.emit()


LAST_INPUT_NAMES = []


def build(nlayers=4, debug=None, stop_after=None, moe=True, only_layer=None):
    nc = bass.Bass("TRN2", target_bir_lowering=False)
    G = Ctx()
    G.stop_after = stop_after

    del LAST_INPUT_NAMES[:]

    def inp(name, shape, dt=F32):
        LAST_INPUT_NAMES.append(name)
        return nc.dram_tensor(name, list(shape), dt, kind="ExternalInput").ap()

    def scr(name, shape, dt=F32):
        return nc.dram_tensor(name, list(shape), dt).ap()

    G.x = inp("x", [TOK, D])
    G.ident = inp("ident", [128, 128], BF16)
    G.identf = inp("identf", [128, 128])
    G.cosT = inp("cosT", [128, TOK])
    G.sinT = inp("sinT", [128, TOK])
    G.cos64T = inp("cos64T", [128, TOK])
    G.sin64T = inp("sin64T", [128, TOK])
    G.kcos = inp("kcos", [128, NT, 32])
    G.ksin = inp("ksin", [128, NT, 32])
    G.maskA = inp("maskA", [128, 128])
    G.maskB = inp("maskB", [128, 128])
    G.dilb = inp("dilb", [128, 2, 9 * 128])
    G.w_in_sh, G.w_out_sh, G.w_in_b, G.w_out_b, G.w_in_full, G.w_out_full = {}, {}, {}, {}, {}, {}
    G.lnp, G.rw, G.rb, G.wgu, G.bgu, G.wdn, G.bdn = {}, {}, {}, {}, {}, {}, {}
    G.idx_g, G.idx_b, G.negbf = {}, {}, {}
    for l in range(nlayers):
        if only_layer is not None and l != only_layer:
            continue
        W = W_IN[l % 4]
        G.w_in_full[l] = inp("l%d_w_in" % l, [D, W])
        G.w_out_full[l] = inp("l%d_w_out" % l, [D, D])
        G.lnp[l] = inp("l%d_lnp" % l, [4, D])
        G.rw[l] = inp("l%d_rw" % l, [D, 32])
        G.rb[l] = inp("l%d_rb" % l, [1, 32])
        if moe:
            G.wgu[l] = inp("l%d_wgu" % l, [4, D, D])
            G.bgu[l] = inp("l%d_bgu" % l, [4, 128, 16])
            G.wdn[l] = inp("l%d_wdn" % l, [4, 1024, D])
            G.bdn[l] = inp("l%d_bdn" % l, [4, D])
        if l % 4 == 1:
            G.idx_g[l] = inp("l%d_idxg" % l, [128, 64])
            G.idx_b[l] = inp("l%d_idxb" % l, [128, 64])
        if l % 4 == 2:
            G.negbf[l] = inp("l%d_bf" % l, [16, 1])
    G.out = nc.dram_tensor("out", [TOK, D], F32, kind="ExternalOutput").ap()
    G.xres = scr("xres", [TOK, D])
    G.xT = scr("xT", [KC, 128, TOK], BF16)
    G.qT = scr("qT", [16, 128, TOK], BF16)
    G.kT_own = scr("kT_own", [16, 128, TOK], BF16)
    G.kT_all = scr("kT_all", [4, 2, 4, 128, TOK], BF16)
    G.v_own = scr("v_own", [TOK, D], BF16)
    G.v_all = scr("v_all", [4, 2, 512, D], BF16)
    G.v_own4 = scr("v_own4", [TOK, 512], BF16)
    G.v_all4 = scr("v_all4", [2, TOK, 512], BF16)
    G.qiT = scr("qiT", [8, 128, TOK], BF16)
    G.kiT_own = scr("kiT_own", [128, TOK], BF16)
    G.kiT_all = scr("kiT_all", [2, 128, TOK], BF16)
    G.wi = scr("wi", [TOK, 16])
    G.sp_own = scr("sp_own", [16, TOK])
    G.sp_all = scr("sp_all", [2, 16, TOK])
    G.negck = scr("negck", [16, 2 * TOK])
    G.dsab = scr("dsab", [NT, 128, 2 * TOK], BF16)
    G.oT = scr("oT", [16, 128, TOK], BF16)
    G.x1T_own = scr("x1T_own", [KC, 128, TOK], BF16)
    G.x1T_all = scr("x1T_allc", [8, 2, 2, 2, 256, TOK], BF16)
    G.ag1 = scr("ag1", [8, 4, 256, TOK], BF16)
    G.yacc = scr("yacc", [8 * TOK, D])
    G.ypart = scr("ypart", [4, 4, 2, 512, D], BF16)
    G.y2 = scr("y2", [4, 4, 512, D], BF16)
    G.yown = scr("yown", [TOK, D], BF16)
    dbg = {}
    if debug:
        for name, shape, dt in debug:
            dbg[name] = nc.dram_tensor("dbg_" + name, list(shape), dt, kind="ExternalOutput").ap()
    G.dbg = dbg

    with contextlib.ExitStack() as st:
        P = Prog(nc, st)
        sems = P.all_sems()
        with nc.Block() as block:
            @block.gpsimd
            def _(e):
                for s in sems:
                    e.sem_clear(s)
        phase_prep(nc, P, G)
        for l in range(nlayers):
            if stop_after in (("weights",), ("prep",)):
                break
            if only_layer is not None and l != only_layer:
                continue
            phase_proj(nc, P, G, l)
            if G.stop_after == ("proj", l):
                break
            if l % 4 == 1:
                phase_dsa_index(nc, P, G, l)
            if l % 4 == 2:
                phase_fox_cum(nc, P, G, l)
            phase_attn(nc, P, G, l)
            if G.stop_after == ("attn", l):
                break
            phase_out_ln1(nc, P, G, l)
            if G.stop_after == ("ln1", l):
                break
            phase_moe(nc, P, G, l)
            if G.stop_after == ("moe", l):
                break
            phase_ln2(nc, P, G, l, l == nlayers - 1)
        for name, ap in dbg.items():
            src = G.w_out_full[0] if name == "w_out_full0" else getattr(G, name)
            n0 = src.shape[0]
            flat_s = src if len(src.shape) == 2 else src.rearrange(
                {3: "a b c -> (a b) c", 4: "a b c d -> (a b c) d", 5: "a b c d e -> (a b c d) e"}[len(src.shape)])
            flat_d = ap if len(ap.shape) == 2 else ap.rearrange(
                {3: "a b c -> (a b) c", 4: "a b c d -> (a b c) d", 5: "a b c d e -> (a b c d) e"}[len(ap.shape)])
            P.dma(flat_d, flat_s, reads=[], writes=[("dbg", name)])
        fin = sb(st, nc, "fin", [128, 8], F32)
        P.i("dve", "memset", [("dbg", n) for n in dbg] + ["out"], ["fin"], fin[:], 0.0)
        P.emit()
    return nc


def _consts(p):
    t = np.arange(TOK)
    pos = ((2 * (t // 128) + p) * 128 + (t % 128)).astype(np.float32)
    c = {}
    c["ident"] = np.eye(128, dtype=np.float32).astype(ml_dtypes.bfloat16)
    c["identf"] = np.eye(128, dtype=np.float32)
    d = np.arange(128)
    inv = (10000.0 ** (-(np.arange(64, dtype=np.float32)) / 64)).astype(np.float32)
    ang = pos[None, :] * inv[d % 64][:, None]
    c["cosT"] = np.cos(ang).astype(np.float32)
    c["sinT"] = (np.sin(ang) * np.where(d < 64, -1.0, 1.0)[:, None]).astype(np.float32)
    inv32 = (10000.0 ** (-(np.arange(32, dtype=np.float32)) / 32)).astype(np.float32)
    ang = pos[None, :] * inv32[d % 32][:, None]
    c["cos64T"] = np.cos(ang).astype(np.float32)
    c["sin64T"] = (np.sin(ang) * np.where((d % 64) < 32, -1.0, 1.0)[:, None]).astype(np.float32)
    angk = pos.reshape(NT, 128).T[:, :, None] * inv32[None, None, :]
    c["kcos"] = np.cos(angk).astype(np.float32)
    c["ksin"] = np.sin(angk).astype(np.float32)
    qi = np.arange(128)[:, None]
    ki = np.arange(128)[None, :]
    tri = np.where(ki <= qi, 0.0, NEG).astype(np.float32)
    allneg = np.full((128, 128), NEG, np.float32)
    zer = np.zeros((128, 128), np.float32)
    c["maskA"] = tri if p == 0 else zer
    c["maskB"] = allneg if p == 0 else tri
    dil = np.zeros((128, 2, 9, 128), np.float32)
    for r in range(2):
        for kp in range(9):
            m = 2 * (8 - kp) + p - r
            dd = m * 128 + qi - ki
            cnt = ((dd >= 0) & (dd <= 128)).astype(np.int32) + ((dd >= 0) & (dd <= 512) & (dd % 4 == 0)) \
                + ((dd >= 0) & (dd <= 2048) & (dd % 16 == 0))
            dil[:, r, kp, :] = np.where(cnt > 0, np.log(np.maximum(cnt, 1)), NEG)
    c["dilb"] = dil.reshape(128, 2, 9 * 128)
    return c


def make_in_maps(inputs, nlayers=4):
    maps = []
    x = np.asarray(inputs["x"])
    for c in range(NCORES):
        b, p = c // 2, c % 2
        m = dict(_consts(p))
        m["x"] = np.ascontiguousarray(x[b].reshape(16, 2, 128, D)[:, p].reshape(TOK, D))
        for l in range(nlayers):
            pre = "l%d_" % l
            m[pre + "w_in"] = np.asarray(inputs[pre + "w_in"])
            m[pre + "w_out"] = np.asarray(inputs[pre + "w_out"])
            m[pre + "lnp"] = np.stack([inputs[pre + "ln1_g"], inputs[pre + "ln1_b"],
                                       inputs[pre + "ln2_g"], inputs[pre + "ln2_b"]]).astype(np.float32)
            perm = list(range(4 * c, 4 * c + 4)) + [e for e in range(32) if not (4 * c <= e < 4 * c + 4)]
            m[pre + "rw"] = np.ascontiguousarray(np.asarray(inputs[pre + "router_w"])[:, perm])
            m[pre + "rb"] = np.ascontiguousarray(np.asarray(inputs[pre + "router_b"])[perm][None, :])
            m[pre + "wgu"] = np.ascontiguousarray(inputs[pre + "w_gu"][4 * c:4 * c + 4])
            m[pre + "bgu"] = np.ascontiguousarray(np.asarray(inputs[pre + "b_gu"][4 * c:4 * c + 4]).reshape(4, 16, 128).transpose(0, 2, 1))
            m[pre + "wdn"] = np.ascontiguousarray(inputs[pre + "w_dn"][4 * c:4 * c + 4])
            m[pre + "bdn"] = np.ascontiguousarray(inputs[pre + "b_dn"][4 * c:4 * c + 4])
            if l % 4 == 1:
                m[pre + "idxg"] = np.ascontiguousarray(np.broadcast_to(np.asarray(inputs[pre + "idx_norm_g"])[None, :], (128, 64)))
                m[pre + "idxb"] = np.ascontiguousarray(np.broadcast_to(np.asarray(inputs[pre + "idx_norm_b"])[None, :], (128, 64)))
            if l % 4 == 2:
                m[pre + "bf"] = np.ascontiguousarray(np.asarray(inputs[pre + "b_forget"])[:, None])
        maps.append(m)
    return maps


def kernel(**inputs):
    nc = build(4)
    maps = make_in_maps(inputs, 4)
    res = run_bass_kernel_spmd(nc, maps, core_ids=list(range(NCORES)))
    out = np.zeros((4, 4096, D), np.float32)
    for c in range(NCORES):
        b, p = c // 2, c % 2
        out[b].reshape(16, 2, 128, D)[:, p] = res.results[c]["out"].reshape(16, 128, D)
    return out
```

```python
import contextlib
import numpy as np
import ml_dtypes
import concourse.bass as bass
import concourse.mybir as mybir
from concourse.bass_utils import run_bass_kernel_spmd

F32 = mybir.dt.float32
BF16 = mybir.dt.bfloat16
ALU = mybir.AluOpType
AF = mybir.ActivationFunctionType
AX = mybir.AxisListType

NCORES = 8
D = 2048
KC = 16
NT = 16
TOK = 2048
NEG = -1.0e30
ALPHA = 8 ** 0.25
LN_EPS = 1e-5
SCALE = 128 ** -0.5
W_IN = (6144, 4176, 6160, 6144)
PAIRS = [[0, 1], [2, 3], [4, 5], [6, 7]]
WORLD = [list(range(8))]


class Prog:
    CENG = ("pe", "act", "dve", "pool")
    ND = 10

    def __init__(self, nc, stack):
        self.nc = nc
        self.csem = {e: stack.enter_context(nc.semaphore("c_" + e)) for e in self.CENG}
        self.dsem = {q: [stack.enter_context(nc.semaphore("d_%s%d" % (q, i))) for i in range(self.ND)]
                     for q in ("sp", "pool", "act")}
        self.ccsem = [stack.enter_context(nc.semaphore("cc%d" % i)) for i in range(30)]
        self.reset()

    def all_sems(self):
        s = list(self.csem.values()) + list(self.ccsem)
        for q in self.dsem:
            s += self.dsem[q]
        return s

    def reset(self):
        self.ops = {e: [] for e in ("pe", "act", "dve", "pool", "sp")}
        self.cnt = {e: 0 for e in self.CENG}
        self.dcnt = {q: [0] * self.ND for q in self.dsem}
        self.drr = {q: 0 for q in self.dsem}
        self.ccn = 0
        self.last_w = {}
        self.readers = {}
        self.waited = {e: {} for e in self.ops}

    def _deps(self, eng, reads, writes):
        deps = []
        for r in reads:
            if r in self.last_w:
                deps.append(self.last_w[r])
        for w in writes:
            if w in self.last_w:
                deps.append(self.last_w[w])
            deps.extend(self.readers.get(w, ()))
        return deps

    def _finish(self, eng, deps, fn, tok, amt, reads, writes):
        best = {}
        for (sem, v) in deps:
            if eng == "pe" and sem is self.csem["pe"]:
                continue
            k = id(sem)
            if k not in best or best[k][1] < v:
                best[k] = (sem, v)
        waits = []
        wd = self.waited[eng]
        for k, (sem, v) in best.items():
            if wd.get(k, 0) >= v:
                continue
            wd[k] = v
            waits.append((sem, v))
        self.ops[eng].append((waits, fn, tok[0], amt))
        for w in writes:
            self.last_w[w] = tok
            self.readers[w] = []
        for r in reads:
            self.readers.setdefault(r, []).append(tok)

    def op(self, eng, fn, reads=(), writes=()):
        deps = self._deps(eng, reads, writes)
        self.cnt[eng] += 1
        tok = (self.csem[eng], self.cnt[eng])
        self._finish(eng, deps, fn, tok, 1, reads, writes)

    def i(self, eng, name, reads, writes, *args, **kw):
        self.op(eng, lambda e: getattr(e, name)(*args, **kw), reads=reads, writes=writes)

    def dma(self, out, in_, reads=(), writes=(), q="sp"):
        eng = q
        deps = self._deps(eng, reads, writes)
        i = self.drr[q] % self.ND
        self.drr[q] += 1
        sem = self.dsem[q][i]
        if self.dcnt[q][i] > 0:
            deps.append((sem, 16 * self.dcnt[q][i]))
        self.dcnt[q][i] += 1
        tok = (sem, 16 * self.dcnt[q][i])
        self._finish(eng, deps, lambda e: e.dma_start(out=out, in_=in_), tok, 16, reads, writes)

    def collective(self, kind, op, groups, in_ap, out_ap, reads=(), writes=()):
        deps = self._deps("pool", reads, writes)
        sem = self.ccsem[self.ccn]
        self.ccn += 1
        tok = (sem, 1)

        def fn(e):
            return e.collective_compute(kind, op, replica_groups=groups, ins=[in_ap], outs=[out_ap])
        self._finish("pool", deps, fn, tok, None, reads, writes)

    def emit(self):
        nc = self.nc
        ops = self.ops
        for q in self.dsem:
            eng = q
            waits = [(self.dsem[q][i], 16 * self.dcnt[q][i]) for i in range(self.ND) if self.dcnt[q][i] > 0]
            if q == "pool":
                waits += [(self.ccsem[i], 1) for i in range(self.ccn)]
            if waits:
                ops[eng].append((waits, None, None, None))

        def replay(e, lst):
            for waits, fn, sem, amt in lst:
                for (s, v) in waits:
                    e.wait_ge(s, v)
                if fn is None:
                    continue
                ins = fn(e)
                if amt is None:
                    ins.then_inc(sem)
                else:
                    ins.then_inc(sem, amt)

        with nc.Block() as block:
            @block.sync
            def _(e):
                replay(e, ops["sp"])

            @block.scalar
            def _(e):
                replay(e, ops["act"])

            @block.vector
            def _(e):
                replay(e, ops["dve"])

            @block.gpsimd
            def _(e):
                replay(e, ops["pool"])

            @block.tensor
            def _(e):
                replay(e, ops["pe"])
        sems = self.all_sems()
        with nc.Block() as block:
            @block.gpsimd
            def _(e):
                for s in sems:
                    e.sem_clear(s)
        self.reset()


class Ctx:
    pass


_UNIQ = [0]


def sb(st, nc, name, shape, dt):
    _UNIQ[0] += 1
    return st.enter_context(nc.sbuf_tensor("s%d_%s" % (_UNIQ[0], name), shape, dt))


def ps(st, nc, name, shape, dt):
    _UNIQ[0] += 1
    return st.enter_context(nc.psum_tensor("p%d_%s" % (_UNIQ[0], name), shape, dt))


def emit_xT(P, C, src_ap, src_key, j, dstT, tag):
    i = C.xt_i
    C.xt_i += 1
    xb = C.xb[i % 2]
    xtt = C.xtt[i % 2]
    pt = C.ptx[i % 2]
    ptk = ("ptx", i % 2 if C.ptx[0] is not C.ptx[1] else 0)
    P.i("act", "activation", [src_key], [("xb", i % 2)], out=xb[:], in_=src_ap, func=AF.Copy)
    for k in range(KC):
        P.i("pe", "transpose", [("xb", i % 2), "ident"], [ptk], out=pt[:, k * 128:(k + 1) * 128], in_=xb[:, k * 128:(k + 1) * 128],
                                              identity=C.ident[:])
    P.i("dve", "tensor_copy", [ptk], [("xtt", i % 2)], out=xtt[:], in_=pt[:])
    P.dma(dstT.rearrange("k p t -> p k t")[:, :, j * 128:(j + 1) * 128],
          xtt[:].rearrange("p (k t) -> p k t", k=KC),
          reads=[("xtt", i % 2)], writes=[(tag, j)])


def alloc_xT(st, nc, C, npt=2):
    C.xt_i = 0
    C.xb = [sb(st, nc, "xb%d" % i, [128, D], BF16) for i in range(2)]
    C.xtt = [sb(st, nc, "xtt%d" % i, [128, D], BF16) for i in range(2)]
    C.ptx = [ps(st, nc, "ptx%d" % i, [128, D], BF16) for i in range(npt)]
    if npt == 1:
        C.ptx = C.ptx * 2


def load_const(P, st, nc, name, dram_ap, shape, dt, key):
    t = sb(st, nc, name, shape, dt)
    P.dma(t[:], dram_ap, reads=[], writes=[key])
    return t


def emit_ln(P, C, z, zkey, gi, out_ap, out_key, i):
    st6 = C.ln_st[i % 2]
    mv = C.ln_mv[i % 2]
    for c in range(4):
        P.i("dve", "bn_stats", [zkey], [("lnst", i % 2)], out=st6[:, c * 6:(c + 1) * 6], in_=z[:, c * 512:(c + 1) * 512])
    P.i("dve", "bn_aggr", [("lnst", i % 2)], [("lnmv", i % 2)], out=mv[:, 0:2], in_=st6[:])
    P.i("dve", "tensor_scalar", [("lnmv", i % 2)], [("lnmv", i % 2)], out=mv[:, 2:3], in0=mv[:, 1:2], scalar1=LN_EPS, scalar2=None,
        op0=ALU.add)
    P.i("act", "activation", [("lnmv", i % 2)], [("lnmv", i % 2)], out=mv[:, 2:3], in_=mv[:, 2:3], func=AF.Sqrt)
    P.i("dve", "reciprocal", [("lnmv", i % 2)], [("lnmv", i % 2)], out=mv[:, 2:3], in_=mv[:, 2:3])
    P.i("dve", "tensor_scalar", [zkey, ("lnmv", i % 2)], [zkey], out=z, in0=z, scalar1=mv[:, 0:1], scalar2=mv[:, 2:3],
                                          op0=ALU.subtract, op1=ALU.mult)
    P.i("pool", "tensor_tensor", [zkey, "lnp"], [zkey], out=z, in0=z, in1=C.lnp[:, gi, :], op=ALU.mult)
    P.i("pool", "tensor_tensor", [zkey, "lnp"], [out_key], out=out_ap, in0=z, in1=C.lnp[:, gi + 1, :], op=ALU.add)


def phase_prep(nc, P, G):
    with contextlib.ExitStack() as st:
        C = Ctx()
        alloc_xT(st, nc, C)
        C.ident = load_const(P, st, nc, "ident", G.ident, [128, 128], BF16, "ident")
        xs = [sb(st, nc, "xs%d" % i, [128, D], F32) for i in range(2)]
        for j in range(NT):
            P.dma(xs[j % 2][:], G.x[j * 128:(j + 1) * 128, :], reads=[], writes=[("xs", j % 2)])
            emit_xT(P, C, xs[j % 2][:], ("xs", j % 2), j, G.xT, "xT")
        P.emit()


def proj_blocks(kind):
    blocks = []
    if kind in (0, 3):
        for hb in range(8):
            blocks.append(("fm_rope", hb * 256, 256, ("qT", hb * 2)))
        for hb in range(8):
            blocks.append(("fm_rope", 2048 + hb * 256, 256, ("kT", hb * 2)))
        for vb in range(8):
            blocks.append(("tm", 4096 + vb * 256, 256, ("v", vb * 256)))
    elif kind == 2:
        for hb in range(8):
            blocks.append(("fm_plain", hb * 256, 256, ("qT", hb * 2)))
        for hb in range(8):
            blocks.append(("fm_plain", 2048 + hb * 256, 256, ("kT", hb * 2)))
        for vb in range(8):
            blocks.append(("tm", 4096 + vb * 256, 256, ("v", vb * 256)))
        blocks.append(("fox", 6144, 16, None))
    else:
        for hb in range(8):
            blocks.append(("fm_rope", hb * 256, 256, ("qT", hb * 2)))
        for hb in range(2):
            blocks.append(("fm_rope", 2048 + hb * 256, 256, ("kT", hb * 2)))
        for vb in range(2):
            blocks.append(("tm", 2560 + vb * 256, 256, ("v", vb * 256)))
        for hb in range(4):
            blocks.append(("fm_rope64", 3072 + hb * 256, 256, ("qiT", hb * 2)))
        blocks.append(("kiwi", 4096, 80, None))
    return blocks


def phase_proj(nc, P, G, l):
    kind = l % 4
    w_full = G.w_in_full[l]
    W = W_IN[kind]
    with contextlib.ExitStack() as st:
        C = Ctx()
        xT_sb = sb(st, nc, "xT_sb", [128, KC, TOK], BF16)
        for k in range(KC):
            P.dma(xT_sb[:, k, :], G.xT[k], reads=[("xT", j) for j in range(NT)] if k == 0 else [],
                  writes=[("xT_sb", k)])
        xT_keys = [("xT_sb", k) for k in range(KC)]
        cosT = load_const(P, st, nc, "cosT", G.cosT, [128, TOK], F32, "cosT")
        sinT = load_const(P, st, nc, "sinT", G.sinT, [128, TOK], F32, "sinT")
        if kind == 1:
            cos64 = load_const(P, st, nc, "cos64", G.cos64T, [128, TOK], F32, "cos64")
            sin64 = load_const(P, st, nc, "sin64", G.sin64T, [128, TOK], F32, "sin64")
            kcos = load_const(P, st, nc, "kcos", G.kcos, [128, NT, 32], F32, "kcos")
            ksin = load_const(P, st, nc, "ksin", G.ksin, [128, NT, 32], F32, "ksin")
            idxg = load_const(P, st, nc, "idxg", G.idx_g[l], [128, 64], F32, "idxg")
            idxb = load_const(P, st, nc, "idxb", G.idx_b[l], [128, 64], F32, "idxb")
            identf = load_const(P, st, nc, "identf", G.identf, [128, 128], F32, "identf")
        if kind == 2:
            negbf = load_const(P, st, nc, "negbf", G.negbf[l], [16, 1], F32, "negbf")
        wst = [sb(st, nc, "wst%d" % i, [128, KC, 256], F32) for i in range(2)]
        wbf = [sb(st, nc, "wbf%d" % i, [128, KC, 256], BF16) for i in range(2)]
        wsw = [sb(st, nc, "wsw%d" % i, [128, KC, 256], BF16) for i in range(2)]
        pa = [ps(st, nc, "pa%d" % i, [128, 512], F32) for i in range(2)]
        pb = [ps(st, nc, "pb%d" % i, [128, 512], F32) for i in range(2)]
        pv = [ps(st, nc, "pv%d" % i, [128, 512], F32) for i in range(2)]
        t1 = [sb(st, nc, "t1_%d" % i, [128, 512], F32) for i in range(2)]
        t2 = [sb(st, nc, "t2_%d" % i, [128, 512], F32) for i in range(2)]
        ob = [sb(st, nc, "ob%d" % i, [128, 512], BF16) for i in range(2)]
        vb_ = [sb(st, nc, "vbo%d" % i, [128, 256], BF16) for i in range(2)]
        if kind == 1:
            kw = [sb(st, nc, "kw%d" % i, [128, 80], F32) for i in range(2)]
            kst = [sb(st, nc, "kst%d" % i, [128, 8], F32) for i in range(2)]
            kr = [sb(st, nc, "kr%d" % i, [128, 64], F32) for i in range(2)]
            kt_ = [sb(st, nc, "ktmp%d" % i, [128, 64], F32) for i in range(2)]
            kiTs = [sb(st, nc, "kiTs%d" % i, [128, 128], BF16) for i in range(2)]
            wis = [sb(st, nc, "wis%d" % i, [128, 16], F32) for i in range(2)]
            pk = [ps(st, nc, "pk%d" % i, [128, 128], F32) for i in range(2)]
        if kind == 2:
            fx = sb(st, nc, "fx", [16, TOK], F32)
        blocks = proj_blocks(kind)
        cnt = {"a": 0, "v": 0, "o": 0, "k": 0}

        def load_block(bi):
            typ, c0, ncol, _ = blocks[bi]
            s = bi % 2
            P.dma(wst[s][:, :, 0:ncol], w_full[:, c0:c0 + ncol].rearrange("(k p) c -> p k c", p=128),
                  reads=["w_in_full"], writes=[("wst", s)])
            P.i("pool", "tensor_copy", [("wst", s)], [("wbf", s)], out=wbf[s][:, :, 0:ncol], in_=wst[s][:, :, 0:ncol])
            if typ in ("fm_rope", "fm_rope64"):
                hw = 64 if typ == "fm_rope" else 32
                v4 = wbf[s][:].rearrange("p k (g two h) -> p k g two h", two=2, h=hw)
                o4 = wsw[s][:].rearrange("p k (g two h) -> p k g two h", two=2, h=hw)
                for k in range(KC):
                    P.i("pool", "tensor_copy", [("wbf", s)], [("wsw", s)], out=o4[:, k, :, 0, :], in_=v4[:, k, :, 1, :])
                    P.i("pool", "tensor_copy", [("wbf", s)], [("wsw", s)], out=o4[:, k, :, 1, :], in_=v4[:, k, :, 0, :])

        load_block(0)
        for bi, (typ, c0, ncol, dst) in enumerate(blocks):
            if bi + 1 < len(blocks):
                load_block(bi + 1)
            s = bi % 2
            if typ in ("fm_rope", "fm_plain", "fm_rope64"):
                for hh in range(2):
                    if dst[0] == "qT":
                        dram = G.qT[dst[1] + hh]
                    elif dst[0] == "kT":
                        dram = G.kT_own[dst[1] + hh]
                    else:
                        dram = G.qiT[dst[1] + hh]
                    for c4 in range(4):
                        a = cnt["a"] % 2
                        cnt["a"] += 1
                        tsl = slice(c4 * 512, (c4 + 1) * 512)
                        for k in range(KC):
                            P.i("pe", "matmul", [("wbf", s), ("xT_sb", k)], [("pa", a)], pa[a][:], lhsT=wbf[s][:, k, hh * 128:(hh + 1) * 128], rhs=xT_sb[:, k, tsl],
                                start=(k == 0), stop=(k == KC - 1))
                        o = cnt["o"] % 2
                        cnt["o"] += 1
                        if typ == "fm_plain":
                            P.i("act", "activation", [("pa", a)], [("ob", o)], out=ob[o][:], in_=pa[a][:], func=AF.Copy)
                        else:
                            for k in range(KC):
                                P.i("pe", "matmul", [("wsw", s), ("xT_sb", k)], [("pb", a)], pb[a][:], lhsT=wsw[s][:, k, hh * 128:(hh + 1) * 128], rhs=xT_sb[:, k, tsl],
                                    start=(k == 0), stop=(k == KC - 1))
                            ct, sn = (cosT, sinT) if typ == "fm_rope" else (cos64, sin64)
                            ck, sk = ("cosT", "sinT") if typ == "fm_rope" else ("cos64", "sin64")
                            P.i("dve", "tensor_tensor", [("pa", a), ck], [("t1", o)], out=t1[o][:], in0=pa[a][:], in1=ct[:, tsl], op=ALU.mult)
                            P.i("dve", "tensor_tensor", [("pb", a), sk], [("t2", o)], out=t2[o][:], in0=pb[a][:], in1=sn[:, tsl], op=ALU.mult)
                            P.i("pool", "tensor_tensor", [("t1", o), ("t2", o)], [("ob", o)], out=ob[o][:], in0=t1[o][:], in1=t2[o][:],
                                                                        op=ALU.add)
                        P.dma(dram[:, tsl], ob[o][:], reads=[("ob", o)], writes=[dst[0] + "_own"])
            elif typ == "tm":
                for j in range(NT):
                    a = cnt["v"] % 2
                    cnt["v"] += 1
                    for k in range(KC):
                        P.i("pe", "matmul", [("wbf", s), ("xT_sb", k)], [("pv", a)], pv[a][:, 0:ncol], lhsT=xT_sb[:, k, j * 128:(j + 1) * 128], rhs=wbf[s][:, k, 0:ncol],
                            start=(k == 0), stop=(k == KC - 1))
                    P.i("act", "activation", [("pv", a)], [("vbo", a)], out=vb_[a][:, 0:ncol], in_=pv[a][:, 0:ncol], func=AF.Copy)
                    P.dma((G.v_own4 if kind == 1 else G.v_own)[j * 128:(j + 1) * 128, dst[1]:dst[1] + ncol], vb_[a][:, 0:ncol],
                          reads=[("vbo", a)], writes=["v_own"])
            elif typ == "fox":
                for c4 in range(4):
                    a = cnt["a"] % 2
                    cnt["a"] += 1
                    tsl = slice(c4 * 512, (c4 + 1) * 512)
                    for k in range(KC):
                        P.i("pe", "matmul", [("wbf", s), ("xT_sb", k)], [("pa", a)], pa[a][0:16, :], lhsT=wbf[s][:, k, 0:16], rhs=xT_sb[:, k, tsl],
                            start=(k == 0), stop=(k == KC - 1))
                    P.i("dve", "tensor_scalar", [("pa", a), "negbf"], ["fx"], out=fx[:, tsl], in0=pa[a][0:16, :],
                                                                         scalar1=negbf[:, 0:1], scalar2=None, op0=ALU.add)
                P.i("act", "activation", ["fx"], ["fx"], out=fx[:], in_=fx[:], func=AF.Exp, scale=-1.0)
                P.i("act", "activation", ["fx"], ["fx"], out=fx[:], in_=fx[:], func=AF.Ln, bias=1.0, scale=1.0)
                P.dma(G.sp_own[:, :], fx[:], reads=["fx"], writes=["sp_own"])
            elif typ == "kiwi":
                for j in range(NT):
                    a = cnt["k"] % 2
                    cnt["k"] += 1
                    for k in range(KC):
                        P.i("pe", "matmul", [("wbf", s), ("xT_sb", k)], [("pv", a)], pv[a][:, 0:80], lhsT=xT_sb[:, k, j * 128:(j + 1) * 128], rhs=wbf[s][:, k, 0:80],
                            start=(k == 0), stop=(k == KC - 1))
                    P.i("act", "activation", [("pv", a)], [("kw", a)], out=kw[a][:], in_=pv[a][:, 0:80], func=AF.Copy)
                    P.i("dve", "tensor_scalar", [("kw", a)], [("wis", a)], out=wis[a][:], in0=kw[a][:, 64:80],
                                                               scalar1=1.0 / 32.0, scalar2=None, op0=ALU.mult)
                    P.dma(G.wi[j * 128:(j + 1) * 128, :], wis[a][:], reads=[("wis", a)], writes=["wi"])
                    P.i("dve", "bn_stats", [("kw", a)], [("kst", a)], out=kst[a][:, 0:6], in_=kw[a][:, 0:64])
                    P.i("dve", "bn_aggr", [("kst", a)], [("kst", a)], out=kst[a][:, 6:8], in_=kst[a][:, 0:6])
                    P.i("dve", "tensor_scalar", [("kst", a)], [("kst", a)], out=kst[a][:, 0:1], in0=kst[a][:, 7:8], scalar1=LN_EPS,
                        scalar2=None, op0=ALU.add)
                    P.i("act", "activation", [("kst", a)], [("kst", a)], out=kst[a][:, 0:1], in_=kst[a][:, 0:1], func=AF.Sqrt)
                    P.i("dve", "reciprocal", [("kst", a)], [("kst", a)], out=kst[a][:, 0:1], in_=kst[a][:, 0:1])
                    P.i("dve", "tensor_scalar", [("kw", a), ("kst", a)], [("kr", a)], out=kr[a][:], in0=kw[a][:, 0:64], scalar1=kst[a][:, 6:7],
                                                               scalar2=kst[a][:, 0:1], op0=ALU.subtract, op1=ALU.mult)
                    P.i("dve", "tensor_tensor", [("kr", a), "idxg"], [("kr", a)], out=kr[a][:], in0=kr[a][:], in1=idxg[:], op=ALU.mult)
                    P.i("dve", "tensor_tensor", [("kr", a), "idxb"], [("kr", a)], out=kr[a][:], in0=kr[a][:], in1=idxb[:], op=ALU.add)
                    x1 = kr[a][:, 0:32]
                    x2 = kr[a][:, 32:64]
                    P.i("dve", "tensor_tensor", [("kr", a), "kcos"], [("ktmp", a)], out=kt_[a][:, 0:32], in0=x1, in1=kcos[:, j, :],
                                                                           op=ALU.mult)
                    P.i("dve", "tensor_tensor", [("kr", a), "ksin"], [("ktmp", a)], out=kt_[a][:, 32:64], in0=x2, in1=ksin[:, j, :],
                                                                           op=ALU.mult)
                    P.i("dve", "tensor_tensor", [("ktmp", a)], [("kw", a)], out=kw[a][:, 0:32], in0=kt_[a][:, 0:32], in1=kt_[a][:, 32:64],
                                                               op=ALU.subtract)
                    P.i("dve", "tensor_tensor", [("kr", a), "kcos", ("kw", a)], [("ktmp", a)], out=kt_[a][:, 0:32], in0=x2, in1=kcos[:, j, :],
                                                                           op=ALU.mult)
                    P.i("dve", "tensor_tensor", [("kr", a), "ksin"], [("ktmp", a)], out=kt_[a][:, 32:64], in0=x1, in1=ksin[:, j, :],
                                                                           op=ALU.mult)
                    P.i("dve", "tensor_tensor", [("ktmp", a)], [("kw", a)], out=kw[a][:, 32:64], in0=kt_[a][:, 0:32], in1=kt_[a][:, 32:64],
                                                               op=ALU.add)
                    P.i("pe", "transpose", [("kw", a), "identf"], [("pk", a)], out=pk[a][0:64, :], in_=kw[a][:, 0:64], identity=identf[:])
                    P.i("act", "activation", [("pk", a)], [("kiTs", a)], out=kiTs[a][0:64, :], in_=pk[a][0:64, :], func=AF.Copy)
                    P.dma(G.kiT_own[0:64, j * 128:(j + 1) * 128], kiTs[a][0:64, :], reads=[("kiTs", a)],
                          writes=["kiT_own"])
                    P.dma(G.kiT_own[64:128, j * 128:(j + 1) * 128], kiTs[a][0:64, :], reads=[("kiTs", a)],
                          writes=["kiT_own"])
        nkc = 1 if kind == 1 else 4
        for k in range(nkc):
            P.collective("AllGather", ALU.bypass, PAIRS,
                         G.kT_own[4 * k:4 * k + 4].rearrange("h d t -> (h d) t"),
                         G.kT_all[k].rearrange("r h d t -> (r h d) t"),
                         reads=["kT_own"], writes=["kT_all"])
        if kind == 1:
            P.collective("AllGather", ALU.bypass, PAIRS, G.v_own4[:, :], G.v_all4.rearrange("r t c -> (r t) c"),
                         reads=["v_own"], writes=["v_all"])
            P.collective("AllGather", ALU.bypass, PAIRS, G.kiT_own[:, :], G.kiT_all.rearrange("r d t -> (r d) t"),
                         reads=["kiT_own"], writes=["kiT_all"])
        else:
            for k in range(4):
                P.collective("AllGather", ALU.bypass, PAIRS, G.v_own[k * 512:(k + 1) * 512, :],
                             G.v_all[k].rearrange("r t c -> (r t) c"),
                             reads=["v_own"], writes=["v_all"])
        if kind == 2:
            P.collective("AllGather", ALU.bypass, PAIRS, G.sp_own[:, :], G.sp_all.rearrange("r h t -> (r h) t"),
                         reads=["sp_own"], writes=["sp_all"])
        P.emit()


def phase_fox_cum(nc, P, G, l):
    with contextlib.ExitStack() as st:
        spg = sb(st, nc, "spg", [16, 2 * TOK], F32)
        ones = sb(st, nc, "ones", [16, 2 * TOK], F32)
        cum = sb(st, nc, "cum", [16, 2 * TOK], F32)
        for r in range(2):
            P.dma(spg[:].rearrange("h (j r i) -> h j r i", r=2, i=128)[:, :, r, :],
                  G.sp_all[r].rearrange("h (j i) -> h j i", i=128), reads=[], writes=["spg"])
        P.i("pool", "memset", [], ["ones"], ones[:], 1.0)
        P.i("dve", "tensor_tensor_scan", ["spg", "ones"], ["cum"], out=cum[:], data0=ones[:], data1=spg[:],
            initial=0.0, op0=ALU.mult, op1=ALU.add)
        for r in range(2):
            P.dma(G.negck.rearrange("h (r j i) -> h r j i", r=2, i=128)[:, r],
                  cum[:].rearrange("h (j r i) -> h j r i", r=2, i=128)[:, :, r, :], reads=["cum"], writes=["negck"])
        P.emit()


def phase_attn(nc, P, G, l):
    kind = l % 4
    with contextlib.ExitStack() as st:
        ident = load_const(P, st, nc, "ident", G.ident, [128, 128], BF16, "ident")
        maskA = load_const(P, st, nc, "maskA", G.maskA, [128, 128], F32, "maskA")
        maskB = load_const(P, st, nc, "maskB", G.maskB, [128, 128], F32, "maskB")
        masks = (maskA, maskB)
        if kind == 0:
            dilb = load_const(P, st, nc, "dilb", G.dilb, [128, 2, 9 * 128], F32, "dilb")
        nkv = 4 if kind == 1 else 2
        if kind == 1:
            KT = [sb(st, nc, "KT4", [128, 4, 2, TOK], BF16)]
            V = [sb(st, nc, "V4", [128, 2, NT, 512], BF16)]
            QT = [sb(st, nc, "QTj%d" % i, [128, 16, 128], BF16) for i in range(2)]
            Bias = [sb(st, nc, "Bias%d" % i, [128, 2, TOK], BF16) for i in range(2)]
        else:
            KT = [sb(st, nc, "KT%d" % i, [128, 2, TOK], BF16) for i in range(2)]
            V = [sb(st, nc, "V%d" % i, [128, 2, NT, 128], BF16) for i in range(2)]
            QT = [sb(st, nc, "QT%d" % i, [128, TOK], BF16) for i in range(2)]
        if kind == 2:
            Bias = [sb(st, nc, "Bias%d" % i, [128, 2, TOK], F32) for i in range(2)]
        S = [sb(st, nc, "S%d" % i, [128, 2, TOK], F32) for i in range(3)]
        Pb = [sb(st, nc, "Pb%d" % i, [128, 2, TOK], BF16) for i in range(3)]
        PT = [sb(st, nc, "PT%d" % i, [128, 32 * 128], BF16) for i in range(3)]
        sm = [sb(st, nc, "sm%d" % i, [128, 8], F32) for i in range(3)]
        Osb = [sb(st, nc, "Osb%d" % i, [128, 128], BF16) for i in range(3)]
        OT = [sb(st, nc, "OT%d" % i, [128, 128], BF16) for i in range(3)]
        sps = [ps(st, nc, "sps%d" % i, [128, 512], F32) for i in range(3)]
        ptp = [ps(st, nc, "ptp%d" % i, [128, 1024], BF16) for i in range(2)]
        pmisc = ps(st, nc, "pmisc", [128, 512], F32)
        otp = ps(st, nc, "otp", [128, 128], BF16)
        cn = {"sps": 0, "ptp": 0, "it": 0}

        if kind == 3:
            km2 = [sb(st, nc, "km2_%d" % i, [128, 2, NT], F32) for i in range(2)]
            kmT = [sb(st, nc, "kmT%d" % i, [128, NT], BF16) for i in range(2)]
            gm = [sb(st, nc, "gm%d" % i, [128, 16], F32) for i in range(3)]
            m8 = [sb(st, nc, "m8_%d" % i, [128, 8], F32) for i in range(3)]
            bb = [sb(st, nc, "bb%d" % i, [128, 16], F32) for i in range(3)]

        def load_head(h):
            hb = h % 2
            for r in range(2):
                P.dma(KT[hb][:, r, :], G.kT_all[h // 4, r, h % 4], reads=[], writes=[("KT", hb)])
                for ck in range(4):
                    P.dma(V[hb][:, r, ck * 4:(ck + 1) * 4, :],
                          G.v_all[ck, r, :, h * 128:(h + 1) * 128].rearrange("(jj p) d -> p jj d", p=128),
                          reads=[], writes=[("V", hb)])
            P.dma(QT[hb][:], G.qT[h], reads=[], writes=[("QT", hb)])
            if kind == 2:
                P.dma(Bias[hb][:].rearrange("p r t -> p (r t)"), G.negck[h:h + 1, :].broadcast_to([128, 2 * TOK]),
                      reads=["negck"], writes=[("Bias", hb)])
            if kind == 3:
                P.i("dve", "tensor_reduce", [("KT", hb)], [("km2", hb)], out=km2[hb][:],
                    in_=KT[hb][:].rearrange("p r (j i) -> p r j i", i=128), axis=AX.X, op=ALU.add)
                P.i("dve", "tensor_tensor", [("km2", hb)], [("km2", hb)], out=km2[hb][:, 0, :], in0=km2[hb][:, 0, :],
                    in1=km2[hb][:, 1, :], op=ALU.add)
                P.i("dve", "tensor_scalar", [("km2", hb)], [("kmT", hb)], out=kmT[hb][:], in0=km2[hb][:, 0, :],
                    scalar1=1.0 / 256.0, scalar2=None, op0=ALU.mult)

        def core(h, j, qT_ap, qkeys, kt_fn, kkeys, v_fn, vkeys, bias_fn, bkeys):
            it = cn["it"]
            cn["it"] += 1
            p = it % 3
            jlo = max(0, j - 8) if kind == 0 else 0
            lo, hi = jlo * 128, (j + 1) * 128
            skeys = []
            if kind == 3:
                hb = h % 2
                P.i("pe", "matmul", qkeys + [("kmT", hb)], [("pgate", 0)], pmisc[:, 384:400], lhsT=qT_ap, rhs=kmT[hb][:],
                    start=True, stop=True)
                P.i("pool", "memset", [], [("gm", p)], gm[p][:], NEG)
                if j > 0:
                    P.i("dve", "tensor_copy", [("pgate", 0), ("gm", p)], [("gm", p)], out=gm[p][:, 0:j], in_=pmisc[:, 384:384 + j])
                P.i("dve", "max", [("gm", p)], [("m8", p)], out=m8[p][:], in_=gm[p][:])
                P.i("dve", "tensor_scalar", [("gm", p), ("m8", p)], [("bb", p)], out=bb[p][:], in0=gm[p][:],
                    scalar1=m8[p][:, 2:3], scalar2=None, op0=ALU.is_ge)
                P.i("dve", "tensor_scalar", [("bb", p)], [("bb", p)], out=bb[p][:], in0=bb[p][:], scalar1=-1.0,
                    scalar2=1.0e30, op0=ALU.add, op1=ALU.mult)
            for r in range(2):
                if kind == 3:
                    chunks = [(c0, min(512, j * 128 - c0)) for c0 in range(0, j * 128, 512)] + [(j * 128, 128)]
                else:
                    chunks = [(c0, min(512, hi - c0)) for c0 in range(lo, hi, 512)]
                for (c0, w) in chunks:
                    b = cn["sps"] % 3
                    cn["sps"] += 1
                    P.i("pe", "matmul", qkeys + kkeys, [("sps", b)], sps[b][:, 0:w], lhsT=qT_ap, rhs=kt_fn(r, c0, w),
                        start=True, stop=True)
                    key = ("S", p, r, c0)
                    skeys.append(key)
                    if kind == 0:
                        off = c0 - (j - 8) * 128
                        P.i("dve", "scalar_tensor_tensor", [("sps", b), "dilb"], [key], out=S[p][:, r, c0:c0 + w],
                            in0=sps[b][:, 0:w], scalar=SCALE, in1=dilb[:, r, off:off + w], op0=ALU.mult, op1=ALU.add)
                    elif kind == 3:
                        if c0 == j * 128:
                            P.i("dve", "scalar_tensor_tensor", [("sps", b), "maskA", "maskB"], [key],
                                out=S[p][:, r, c0:c0 + w], in0=sps[b][:, 0:w], scalar=SCALE, in1=masks[r][:],
                                op0=ALU.mult, op1=ALU.add)
                        else:
                            nt = w // 128
                            t0 = c0 // 128
                            P.i("dve", "scalar_tensor_tensor", [("sps", b), ("bb", p)], [key],
                                out=S[p][:, r, c0:c0 + w].rearrange("p (t i) -> p t i", i=128),
                                in0=sps[b][:, 0:w].rearrange("p (t i) -> p t i", i=128), scalar=SCALE,
                                in1=bb[p][:, t0:t0 + nt].unsqueeze(2).broadcast_to([128, nt, 128]),
                                op0=ALU.mult, op1=ALU.add)
                    else:
                        P.i("dve", "scalar_tensor_tensor", [("sps", b)] + bkeys, [key], out=S[p][:, r, c0:c0 + w],
                            in0=sps[b][:, 0:w], scalar=SCALE, in1=bias_fn(r, c0, w), op0=ALU.mult, op1=ALU.add)
                if kind in (1, 2):
                    key = [k for k in skeys if k[2] == r][-1]
                    P.i("dve", "tensor_tensor", [key, "maskA", "maskB"], [key], out=S[p][:, r, j * 128:(j + 1) * 128],
                        in0=S[p][:, r, j * 128:(j + 1) * 128], in1=masks[r][:], op=ALU.add)
            P.i("dve", "tensor_reduce", skeys, [("mx", p)], out=sm[p][:, 0:1], in_=S[p][:, :, lo:hi], axis=AX.XY, op=ALU.max)
            P.i("dve", "tensor_scalar", [("mx", p)], [("nmx", p)], out=sm[p][:, 1:2], in0=sm[p][:, 0:1], scalar1=-1.0,
                scalar2=None, op0=ALU.mult)
            P.i("pool", "memset", [], [("rs", p)], sm[p][:, 2:4], 0.0)
            for r in range(2):
                P.i("act", "activation", [k for k in skeys if k[2] == r] + [("nmx", p), ("rs", p)], [("Pb", p, r), ("rs", p)],
                    out=Pb[p][:, r, lo:hi], in_=S[p][:, r, lo:hi], func=AF.Exp, bias=sm[p][:, 1:2], scale=1.0,
                    accum_out=sm[p][:, 2 + r:3 + r])
            P.i("dve", "tensor_tensor", [("rs", p)], [("rinv", p)], out=sm[p][:, 4:5], in0=sm[p][:, 2:3], in1=sm[p][:, 3:4],
                op=ALU.add)
            P.i("dve", "reciprocal", [("rinv", p)], [("rinv", p)], out=sm[p][:, 4:5], in_=sm[p][:, 4:5])
            tiles = [(r, jj) for r in range(2) for jj in range(jlo, j + 1)]
            ngr = (len(tiles) + 7) // 8
            for g in range(ngr):
                grp = tiles[g * 8:(g + 1) * 8]
                pb_ = cn["ptp"] % 2
                cn["ptp"] += 1
                for i_, (r, jj) in enumerate(grp):
                    P.i("pe", "transpose", [("Pb", p, r), "ident"], [("ptp", pb_)], out=ptp[pb_][:, i_ * 128:(i_ + 1) * 128],
                        in_=Pb[p][:, r, jj * 128:(jj + 1) * 128], identity=ident[:])
                n = len(grp)
                eng = "act" if g % 2 == 0 else "dve"
                if eng == "act":
                    P.i("act", "activation", [("ptp", pb_)], [("PT", p, g)], out=PT[p][:, g * 1024:g * 1024 + n * 128],
                        in_=ptp[pb_][:, 0:n * 128], func=AF.Copy)
                else:
                    P.i("dve", "tensor_copy", [("ptp", pb_)], [("PT", p, g)], out=PT[p][:, g * 1024:g * 1024 + n * 128],
                        in_=ptp[pb_][:, 0:n * 128])
            po = pmisc[:, p * 128:(p + 1) * 128]
            for ti, (r, jj) in enumerate(tiles):
                P.i("pe", "matmul", [("PT", p, ti // 8)] + vkeys, [("po", p)], po, lhsT=PT[p][:, ti * 128:(ti + 1) * 128],
                    rhs=v_fn(r, jj), start=(ti == 0), stop=(ti == len(tiles) - 1))
            P.i("dve", "tensor_scalar", [("po", p), ("rinv", p)], [("Osb", p)], out=Osb[p][:], in0=po, scalar1=sm[p][:, 4:5],
                scalar2=None, op0=ALU.mult)
            P.i("pe", "transpose", [("Osb", p), "ident"], [("otp", 0)], out=otp[:], in_=Osb[p][:], identity=ident[:])
            P.i("act", "activation", [("otp", 0)], [("OT", p)], out=OT[p][:], in_=otp[:], func=AF.Copy)
            P.dma(G.oT[h][:, j * 128:(j + 1) * 128], OT[p][:], reads=[("OT", p)], writes=["oT"])

        if kind == 1:
            for r in range(2):
                for kv in range(4):
                    P.dma(KT[0][:, kv, r, :], G.kT_all[0, r, kv], reads=[], writes=["KT4"])
                P.dma(V[0][:, r, :, :], G.v_all4[r].rearrange("(j p) c -> p j c", p=128), reads=[], writes=["V4"])
            for j in range(NT):
                jb = j % 2
                P.dma(QT[jb][:], G.qT.rearrange("h d t -> d h t")[:, :, j * 128:(j + 1) * 128], reads=[], writes=[("QT", jb)])
                P.dma(Bias[jb][:].rearrange("p r t -> p (r t)"), G.dsab[j], reads=["dsab"], writes=[("Bias", jb)])
                for h in range(16):
                    kv = h // 4
                    core(h, j, QT[jb][:, h, :], [("QT", jb)],
                         lambda r, c0, w, kv=kv: KT[0][:, kv, r, c0:c0 + w], ["KT4"],
                         lambda r, jj, kv=kv: V[0][:, r, jj, kv * 128:(kv + 1) * 128], ["V4"],
                         lambda r, c0, w, jb=jb: Bias[jb][:, r, c0:c0 + w], [("Bias", jb)])
        else:
            load_head(0)
            for h in range(16):
                if h + 1 < 16:
                    load_head(h + 1)
                hb = h % 2
                for j in range(NT):
                    core(h, j, QT[hb][:, j * 128:(j + 1) * 128], [("QT", hb)],
                         lambda r, c0, w, hb=hb: KT[hb][:, r, c0:c0 + w], [("KT", hb)],
                         lambda r, jj, hb=hb: V[hb][:, r, jj, :], [("V", hb)],
                         (lambda r, c0, w, hb=hb: Bias[hb][:, r, c0:c0 + w]) if kind == 2 else None,
                         [("Bias", hb)] if kind == 2 else [])
        P.emit()


def phase_dsa_index(nc, P, G, l):
    with contextlib.ExitStack() as st:
        maskA = load_const(P, st, nc, "maskA", G.maskA, [128, 128], F32, "maskA")
        maskB = load_const(P, st, nc, "maskB", G.maskB, [128, 128], F32, "maskB")
        masks = (maskA, maskB)
        kiT = sb(st, nc, "kiT", [128, 2, TOK], BF16)
        for r in range(2):
            P.dma(kiT[:, r, :], G.kiT_all[r], reads=[], writes=["kiT"])
        qiT = [sb(st, nc, "qiT%d" % i, [128, 8, 128], BF16) for i in range(2)]
        wi = [sb(st, nc, "wi%d" % i, [128, 16], F32) for i in range(2)]
        sc = [sb(st, nc, "sc%d" % i, [128, 2 * TOK], F32) for i in range(2)]
        junk = sb(st, nc, "junk", [128, 2 * TOK], F32)
        mbf = [sb(st, nc, "mbf%d" % i, [128, 2 * TOK], BF16) for i in range(2)]
        rl = [sb(st, nc, "rl%d" % i, [128, 512], F32) for i in range(3)]
        bs = [sb(st, nc, "bs%d" % i, [128, 8], F32) for i in range(2)]
        lps = [ps(st, nc, "lps%d" % i, [128, 512], F32) for i in range(4)]
        cn = {"l": 0, "r": 0}
        for j in range(NT):
            p = j % 2
            hi = (j + 1) * 128
            P.dma(qiT[p][:], G.qiT.rearrange("g d t -> d g t")[:, :, j * 128:(j + 1) * 128], reads=[], writes=[("qiT", p)])
            P.dma(wi[p][:], G.wi[j * 128:(j + 1) * 128, :], reads=[], writes=[("wi", p)])
            for r in range(2):
                for c0 in range(0, hi, 512):
                    w = min(512, hi - c0)
                    key = ("sc", p, r, c0)
                    dst = sc[p][:, r * hi + c0:r * hi + c0 + w]
                    for ih in range(16):
                        g, half = ih // 2, ih % 2
                        b = cn["l"] % 4
                        cn["l"] += 1
                        P.i("pe", "matmul", [("qiT", p), "kiT"], [("lps", b)], lps[b][:, 0:w],
                            lhsT=qiT[p][half * 64:(half + 1) * 64, g, :], rhs=kiT[half * 64:(half + 1) * 64, r, c0:c0 + w],
                            start=True, stop=True)
                        q_ = cn["r"] % 3
                        cn["r"] += 1
                        P.i("act", "activation", [("lps", b)], [("rl", q_)], out=rl[q_][:, 0:w], in_=lps[b][:, 0:w], func=AF.Relu)
                        if ih == 0:
                            P.i("dve", "tensor_scalar", [("rl", q_), ("wi", p)], [key], out=dst, in0=rl[q_][:, 0:w],
                                scalar1=wi[p][:, 0:1], scalar2=None, op0=ALU.mult)
                        else:
                            P.i("dve", "scalar_tensor_tensor", [("rl", q_), ("wi", p), key], [key], out=dst, in0=rl[q_][:, 0:w],
                                scalar=wi[p][:, ih:ih + 1], in1=dst, op0=ALU.mult, op1=ALU.add)
            allk = [("sc", p, r, c0) for r in range(2) for c0 in range(0, hi, 512)]
            sk = ("scall", p)
            B = bs[p]
            bk = ("bs", p)
            P.i("dve", "tensor_reduce", allk, [bk], out=B[:, 6:7], in_=sc[p][:, 0:2 * hi], axis=AX.X, op=ALU.max,
                apply_absolute_value=True)
            for r in range(2):
                sl = sc[p][:, r * hi + j * 128:r * hi + (j + 1) * 128]
                P.i("dve", "tensor_tensor", allk + [bk, "maskA", "maskB"], [sk], out=sl, in0=sl, in1=masks[r][:], op=ALU.add)
            P.i("dve", "tensor_scalar", [bk], [bk], out=B[:, 1:2], in0=B[:, 6:7], scalar1=1.0, scalar2=None, op0=ALU.add)
            P.i("dve", "tensor_scalar", [bk], [bk], out=B[:, 0:1], in0=B[:, 1:2], scalar1=-1.0, scalar2=None, op0=ALU.mult)
            for it in range(20):
                P.i("dve", "tensor_scalar", [bk], [bk], out=B[:, 2:3], in0=B[:, 0:1], scalar1=B[:, 1:2], scalar2=0.5,
                    op0=ALU.add, op1=ALU.mult)
                P.i("dve", "tensor_scalar", [sk, bk], ["junk", bk], out=junk[:, 0:2 * hi], in0=sc[p][:, 0:2 * hi],
                    scalar1=B[:, 2:3], scalar2=0.0, op0=ALU.is_ge, op1=ALU.add, accum_out=B[:, 3:4])
                P.i("dve", "tensor_scalar", [bk], [bk], out=B[:, 4:5], in0=B[:, 3:4], scalar1=255.5, scalar2=None, op0=ALU.is_ge)
                P.i("dve", "tensor_tensor", [bk], [bk], out=B[:, 5:6], in0=B[:, 2:3], in1=B[:, 0:1], op=ALU.subtract)
                P.i("dve", "tensor_tensor", [bk], [bk], out=B[:, 7:8], in0=B[:, 1:2], in1=B[:, 2:3], op=ALU.subtract)
                P.i("dve", "scalar_tensor_tensor", [bk], [bk], out=B[:, 0:1], in0=B[:, 5:6], scalar=B[:, 4:5], in1=B[:, 0:1],
                    op0=ALU.mult, op1=ALU.add)
                P.i("dve", "scalar_tensor_tensor", [bk], [bk], out=B[:, 1:2], in0=B[:, 7:8], scalar=B[:, 4:5], in1=B[:, 2:3],
                    op0=ALU.mult, op1=ALU.add)
            P.i("dve", "tensor_scalar", [sk, bk], ["junk"], out=junk[:, 0:2 * hi], in0=sc[p][:, 0:2 * hi], scalar1=B[:, 0:1],
                scalar2=None, op0=ALU.is_ge)
            P.i("dve", "tensor_scalar", ["junk"], [("mbf", p)], out=mbf[p][:, 0:2 * hi], in0=junk[:, 0:2 * hi], scalar1=-1.0,
                scalar2=1.0e30, op0=ALU.add, op1=ALU.mult)
            for r in range(2):
                P.dma(G.dsab[j][:, r * TOK:r * TOK + hi], mbf[p][:, r * hi:(r + 1) * hi], reads=[("mbf", p)], writes=["dsab"])
        P.emit()


PAIRS_A = [[0, 1], [2, 3], [4, 5], [6, 7]]
PAIRS_B = [[0, 2], [1, 3], [4, 6], [5, 7]]
PAIRS_C = [[0, 4], [1, 5], [2, 6], [3, 7]]
QUADS = [[0, 1, 2, 3], [4, 5, 6, 7]]


def phase_out_ln1(nc, P, G, l):
    xin = G.x if l == 0 else G.xres
    with contextlib.ExitStack() as st:
        C = Ctx()
        alloc_xT(st, nc, C, npt=1)
        C.ident = load_const(P, st, nc, "ident", G.ident, [128, 128], BF16, "ident")
        C.lnp = sb(st, nc, "lnp", [128, 2, D], F32)
        P.dma(C.lnp[:], G.lnp[l][0:2, :].partition_broadcast(128), reads=[], writes=["lnp"])
        C.ln_st = [sb(st, nc, "lnst%d" % i, [128, 24], F32) for i in range(2)]
        C.ln_mv = [sb(st, nc, "lnmv%d" % i, [128, 4], F32) for i in range(2)]
        wo = sb(st, nc, "wo", [128, KC, D], BF16)
        wst = [sb(st, nc, "wost%d" % i, [128, D], F32) for i in range(2)]
        for k in range(KC):
            P.dma(wst[k % 2][:], G.w_out_full[l][k * 128:(k + 1) * 128, :], reads=[], writes=[("wost", k % 2)])
            P.i("pool", "tensor_copy", [("wost", k % 2)], [("wo", k)], out=wo[:, k, :], in_=wst[k % 2][:])
        oTt = [sb(st, nc, "oTt%d" % i, [128, 16, 128], BF16) for i in range(2)]
        xs = [sb(st, nc, "xs%d" % i, [128, D], F32) for i in range(2)]
        z = [sb(st, nc, "z%d" % i, [128, D], F32) for i in range(2)]
        x1 = [sb(st, nc, "x1_%d" % i, [128, D], F32) for i in range(2)]
        hps = [ps(st, nc, "hps%d" % i, [128, 512], F32) for i in range(4)]
        for j in range(NT):
            p = j % 2
            P.dma(oTt[p][:], G.oT.rearrange("h d t -> d h t")[:, :, j * 128:(j + 1) * 128], reads=["oT"], writes=[("oTt", p)])
            P.dma(xs[p][:], xin[j * 128:(j + 1) * 128, :], reads=[], writes=[("xs", p)])
            for n in range(4):
                for h in range(16):
                    P.i("pe", "matmul", [("oTt", p), ("wo", h)], [("hps", n)], hps[n][:], lhsT=oTt[p][:, h, :],
                        rhs=wo[:, h, n * 512:(n + 1) * 512], start=(h == 0), stop=(h == 15))
                P.i("dve", "scalar_tensor_tensor", [("hps", n), ("xs", p)], [("z", p)], out=z[p][:, n * 512:(n + 1) * 512],
                    in0=xs[p][:, n * 512:(n + 1) * 512], scalar=ALPHA, in1=hps[n][:], op0=ALU.mult, op1=ALU.add)
            emit_ln(P, C, z[p][:], ("z", p), 0, x1[p][:], ("x1", p), j)
            P.dma(G.xres[j * 128:(j + 1) * 128, :], x1[p][:], reads=[("x1", p)], writes=["xres"])
            emit_xT(P, C, x1[p][:], ("x1", p), j, G.x1T_own, "x1T")
        P.emit()
    src = G.x1T_own.rearrange("k p t -> (k p) t")
    for k in range(8):
        P.collective("AllGather", ALU.bypass, QUADS, src[k * 256:(k + 1) * 256, :],
                     G.ag1[k].rearrange("r t c -> (r t) c"), reads=[], writes=[("ag1", k)])
    P.emit()
    for k in range(8):
        for hh in range(2):
            P.collective("AllGather", ALU.bypass, PAIRS_C, G.ag1[k, 2 * hh:2 * hh + 2].rearrange("r t c -> (r t) c"),
                         G.x1T_all[k, hh].rearrange("c r t d -> (c r t) d"), reads=[], writes=[("agC", k, hh)])
    P.emit()


def phase_moe(nc, P, G, l):
    with contextlib.ExitStack() as st:
        wgu = sb(st, nc, "wgu", [128, KC, D], BF16)
        wdn = sb(st, nc, "wdn", [128, 8, D], BF16)
        wst = [sb(st, nc, "mwst%d" % i, [128, D], F32) for i in range(2)]
        xc = [sb(st, nc, "xc%d" % i, [128, KC, 512], BF16) for i in range(2)]
        actT = sb(st, nc, "actT", [128, 8, 512], BF16)
        tg = [sb(st, nc, "tg%d" % i, [128, 512], F32) for i in range(2)]
        tsg = [sb(st, nc, "tsg%d" % i, [128, 512], F32) for i in range(2)]
        tl = [sb(st, nc, "tl%d" % i, [128, 512], F32) for i in range(2)]
        ysb = [sb(st, nc, "ysb%d" % i, [128, D], F32) for i in range(2)]
        ybf = [sb(st, nc, "ybf%d" % i, [128, D], BF16) for i in range(2)]
        tmp = [sb(st, nc, "ytmp%d" % i, [128, 512], F32) for i in range(2)]
        bdn = sb(st, nc, "bdn", [128, D], F32)
        bgu = sb(st, nc, "bgu", [128, 16], F32)
        rwst = sb(st, nc, "rwst", [128, KC, 32], F32)
        rw = sb(st, nc, "rw", [128, KC, 32], BF16)
        rb = sb(st, nc, "rb", [128, 4, 32], F32)
        Gsb = sb(st, nc, "Gsb", [128, 128, 4], F32)
        lg = [sb(st, nc, "lg%d" % i, [128, 4, 32], F32) for i in range(2)]
        ex = [sb(st, nc, "ex%d" % i, [128, 32], F32) for i in range(2)]
        sel = [sb(st, nc, "sel%d" % i, [128, 32], F32) for i in range(2)]
        rsm = [sb(st, nc, "rsm%d" % i, [128, 16], F32) for i in range(2)]
        hg = [ps(st, nc, "hg%d" % i, [128, 512], F32) for i in range(2)]
        hl = [ps(st, nc, "hl%d" % i, [128, 512], F32) for i in range(2)]
        yp = [ps(st, nc, "yp%d" % i, [128, 512], F32) for i in range(3)]
        prt = ps(st, nc, "prt", [128, 512], F32)
        cn = {"w": 0, "h": 0, "y": 0, "t": 0, "r": 0}
        P.dma(rwst[:], G.rw[l].rearrange("(k p) e -> p k e", p=128), reads=[], writes=["rwst"])
        P.i("pool", "tensor_copy", ["rwst"], ["rw"], out=rw[:], in_=rwst[:])
        for t in range(4):
            P.dma(rb[:, t, :], G.rb[l][0:1, :].broadcast_to([128, 32]), reads=[], writes=["rb"])

        def load_expert(e):
            for k in range(KC):
                w_ = cn["w"] % 2
                cn["w"] += 1
                P.dma(wst[w_][:], G.wgu[l][e, k * 128:(k + 1) * 128, :], reads=[], writes=[("mwst", w_)])
                P.i("pool", "tensor_copy", [("mwst", w_)], [("wgu", k)], out=wgu[:, k, :], in_=wst[w_][:])
            for k in range(8):
                w_ = cn["w"] % 2
                cn["w"] += 1
                P.dma(wst[w_][:], G.wdn[l][e, k * 128:(k + 1) * 128, :], reads=[], writes=[("mwst", w_)])
                P.i("pool", "tensor_copy", [("mwst", w_)], [("wdn", k)], out=wdn[:, k, :], in_=wst[w_][:])
            P.dma(bdn[:], G.bdn[l][e:e + 1, :].broadcast_to([128, D]), reads=[], writes=["bdn"])
            P.dma(bgu[:], G.bgu[l][e], reads=[], writes=["bgu"])

        def x_src(tc):
            s_ = tc // 4
            lc = tc % 4
            rl, hh, rc = s_ & 1, (s_ >> 1) & 1, (s_ >> 2) & 1
            return [(G.x1T_all[k, hh, rc, rl].rearrange("(kk p) t -> p kk t", p=128)[:, :, lc * 512:(lc + 1) * 512], k)
                    for k in range(8)]

        def load_x(tc):
            b = tc % 2
            for (ap, k) in x_src(tc):
                P.dma(xc[b][:, 2 * k:2 * k + 2, :], ap, reads=[], writes=[("xc", b)])

        for e in range(4):
            load_expert(e)
            load_x(0)
            for tc in range(32):
                if tc + 1 < 32:
                    load_x(tc + 1)
                b = tc % 2
                s_ = tc // 4
                lc = tc % 4
                if e == 0:
                    r_ = cn["r"] % 2
                    cn["r"] += 1
                    for t in range(4):
                        for k in range(KC):
                            P.i("pe", "matmul", [("xc", b), "rw"], ["prt"], prt[:, t * 32:(t + 1) * 32],
                                lhsT=xc[b][:, k, t * 128:(t + 1) * 128], rhs=rw[:, k, :], start=(k == 0), stop=(k == KC - 1))
                    P.i("dve", "tensor_tensor", ["prt", "rb"], [("lg", r_)], out=lg[r_][:].rearrange("p t e -> p (t e)"),
                        in0=prt[:, 0:128], in1=rb[:].rearrange("p t e -> p (t e)"), op=ALU.add)
                    for t in range(4):
                        ti = tc * 4 + t
                        q_ = ti % 2
                        P.i("dve", "max", [("lg", r_)], [("rsm", q_)], out=rsm[q_][:, 0:8], in_=lg[r_][:, t, :])
                        P.i("dve", "tensor_scalar", [("rsm", q_)], [("rsm", q_)], out=rsm[q_][:, 8:9], in0=rsm[q_][:, 0:1],
                            scalar1=-1.0, scalar2=None, op0=ALU.mult)
                        P.i("act", "activation", [("lg", r_), ("rsm", q_)], [("ex", q_)], out=ex[q_][:], in_=lg[r_][:, t, :],
                            func=AF.Exp, bias=rsm[q_][:, 8:9], scale=1.0)
                        P.i("dve", "tensor_scalar", [("lg", r_), ("rsm", q_)], [("sel", q_)], out=sel[q_][:], in0=lg[r_][:, t, :],
                            scalar1=rsm[q_][:, 3:4], scalar2=None, op0=ALU.is_ge)
                        P.i("dve", "tensor_tensor", [("sel", q_), ("ex", q_)], [("sel", q_)], out=sel[q_][:], in0=sel[q_][:],
                            in1=ex[q_][:], op=ALU.mult)
                        P.i("dve", "tensor_reduce", [("sel", q_)], [("rsm", q_)], out=rsm[q_][:, 9:10], in_=sel[q_][:],
                            axis=AX.X, op=ALU.add)
                        P.i("dve", "reciprocal", [("rsm", q_)], [("rsm", q_)], out=rsm[q_][:, 9:10], in_=rsm[q_][:, 9:10])
                        P.i("dve", "tensor_scalar", [("sel", q_), ("rsm", q_)], [("Gsb", ti)], out=Gsb[:, ti, :],
                            in0=sel[q_][:, 0:4], scalar1=rsm[q_][:, 9:10], scalar2=None, op0=ALU.mult)
                for fo in range(8):
                    hb_ = cn["h"] % 2
                    cn["h"] += 1
                    for k in range(KC):
                        P.i("pe", "matmul", [("xc", b), ("wgu", k)], [("hg", hb_)], hg[hb_][:],
                            lhsT=wgu[:, k, fo * 128:(fo + 1) * 128], rhs=xc[b][:, k, :], start=(k == 0), stop=(k == KC - 1))
                    for k in range(KC):
                        P.i("pe", "matmul", [("xc", b), ("wgu", k)], [("hl", hb_)], hl[hb_][:],
                            lhsT=wgu[:, k, 1024 + fo * 128:1024 + (fo + 1) * 128], rhs=xc[b][:, k, :], start=(k == 0),
                            stop=(k == KC - 1))
                    P.i("dve", "tensor_scalar", [("hg", hb_), "bgu"], [("tg", hb_)], out=tg[hb_][:], in0=hg[hb_][:],
                        scalar1=bgu[:, fo:fo + 1], scalar2=7.0, op0=ALU.add, op1=ALU.min)
                    P.i("act", "activation", [("tg", hb_)], [("tsg", hb_)], out=tsg[hb_][:], in_=tg[hb_][:], func=AF.Sigmoid,
                        scale=1.702)
                    P.i("dve", "tensor_scalar", [("hl", hb_), "bgu"], [("tl", hb_)], out=tl[hb_][:], in0=hl[hb_][:],
                        scalar1=bgu[:, 8 + fo:9 + fo], scalar2=7.0, op0=ALU.add, op1=ALU.min)
                    P.i("pool", "tensor_scalar", [("tl", hb_)], [("tl", hb_)], out=tl[hb_][:], in0=tl[hb_][:], scalar1=-7.0,
                        scalar2=1.0, op0=ALU.max, op1=ALU.add)
                    P.i("pool", "tensor_tensor", [("tg", hb_), ("tsg", hb_)], [("tg", hb_)], out=tg[hb_][:], in0=tg[hb_][:],
                        in1=tsg[hb_][:], op=ALU.mult)
                    P.i("pool", "tensor_tensor", [("tg", hb_), ("tl", hb_)], [("act", fo)], out=actT[:, fo, :], in0=tg[hb_][:],
                        in1=tl[hb_][:], op=ALU.mult)
                for t in range(4):
                    ti = tc * 4 + t
                    yb_ = cn["t"] % 2
                    cn["t"] += 1
                    row = slice(s_ * TOK + lc * 512 + t * 128, s_ * TOK + lc * 512 + (t + 1) * 128)
                    if e > 0:
                        P.dma(ysb[yb_][:], G.yacc[row, :], reads=[("yacc", ti)], writes=[("ysb", yb_)])
                    for n in range(4):
                        y_ = cn["y"] % 3
                        cn["y"] += 1
                        for fc in range(8):
                            P.i("pe", "matmul", [("act", fc), ("wdn", fc)], [("yp", y_)], yp[y_][:],
                                lhsT=actT[:, fc, t * 128:(t + 1) * 128], rhs=wdn[:, fc, n * 512:(n + 1) * 512],
                                start=(fc == 0), stop=(fc == 7))
                        tm = tmp[cn["y"] % 2]
                        tk = ("ytmp", cn["y"] % 2)
                        P.i("dve", "tensor_tensor", [("yp", y_), "bdn"], [tk], out=tm[:], in0=yp[y_][:],
                            in1=bdn[:, n * 512:(n + 1) * 512], op=ALU.add)
                        if e == 0:
                            P.i("dve", "tensor_scalar", [tk, ("Gsb", ti)], [("ysb", yb_)], out=ysb[yb_][:, n * 512:(n + 1) * 512],
                                in0=tm[:], scalar1=Gsb[:, ti, e:e + 1], scalar2=None, op0=ALU.mult)
                        else:
                            P.i("dve", "scalar_tensor_tensor", [tk, ("Gsb", ti), ("ysb", yb_)], [("ysb", yb_)],
                                out=ysb[yb_][:, n * 512:(n + 1) * 512], in0=tm[:], scalar=Gsb[:, ti, e:e + 1],
                                in1=ysb[yb_][:, n * 512:(n + 1) * 512], op0=ALU.mult, op1=ALU.add)
                    if e < 3:
                        P.dma(G.yacc[row, :], ysb[yb_][:], reads=[("ysb", yb_)], writes=[("yacc", ti)])
                    else:
                        P.i("act", "activation", [("ysb", yb_)], [("ybf", yb_)], out=ybf[yb_][:], in_=ysb[yb_][:], func=AF.Copy)
                        o4, oc = s_ % 4, s_ >> 2
                        tt = lc * 4 + t
                        P.dma(G.ypart[o4, tt // 4, oc, (tt % 4) * 128:(tt % 4 + 1) * 128, :], ybf[yb_][:],
                              reads=[("ybf", yb_)], writes=["ypart"])
        P.emit()
    for o4 in range(4):
        for q4 in range(4):
            P.collective("ReduceScatter", ALU.add, PAIRS_C, G.ypart[o4, q4].rearrange("c t d -> (c t) d"),
                         G.y2[q4, o4], reads=[], writes=[("y2", q4, o4)])
    P.emit()
    for q4 in range(4):
        P.collective("ReduceScatter", ALU.add, QUADS, G.y2[q4].rearrange("c t d -> (c t) d"),
                     G.yown[q4 * 512:(q4 + 1) * 512, :], reads=[], writes=[("yown", q4)])
    P.emit()


def phase_ln2(nc, P, G, l, last):
    with contextlib.ExitStack() as st:
        C = Ctx()
        alloc_xT(st, nc, C, npt=1)
        C.ident = load_const(P, st, nc, "ident", G.ident, [128, 128], BF16, "ident")
        C.lnp = sb(st, nc, "lnp", [128, 2, D], F32)
        P.dma(C.lnp[:], G.lnp[l][2:4, :].partition_broadcast(128), reads=[], writes=["lnp"])
        C.ln_st = [sb(st, nc, "lnst%d" % i, [128, 24], F32) for i in range(2)]
        C.ln_mv = [sb(st, nc, "lnmv%d" % i, [128, 4], F32) for i in range(2)]
        xs = [sb(st, nc, "xs%d" % i, [128, D], F32) for i in range(2)]
        yb = [sb(st, nc, "yb%d" % i, [128, D], BF16) for i in range(2)]
        z = [sb(st, nc, "z%d" % i, [128, D], F32) for i in range(2)]
        x2 = [sb(st, nc, "x2_%d" % i, [128, D], F32) for i in range(2)]
        for j in range(NT):
            p = j % 2
            P.dma(xs[p][:], G.xres[j * 128:(j + 1) * 128, :], reads=[("xres", j)], writes=[("xs", p)])
            P.dma(yb[p][:], G.yown[j * 128:(j + 1) * 128, :], reads=[], writes=[("yb", p)])
            P.i("dve", "scalar_tensor_tensor", [("xs", p), ("yb", p)], [("z", p)], out=z[p][:], in0=xs[p][:], scalar=ALPHA,
                in1=yb[p][:], op0=ALU.mult, op1=ALU.add)
            emit_ln(P, C, z[p][:], ("z", p), 0, x2[p][:], ("x2", p), j)
            if last:
                P.dma(G.out[j * 128:(j + 1) * 128, :], x2[p][:], reads=[("x2", p)], writes=["out"])
            else:
                P.dma(G.xres[j * 128:(j + 1) * 128, :], x2[p][:], reads=[("x2", p), ("xs", p)], writes=[("xres", j)])
                emit_xT(P, C, x2[p][:], ("x2", p), j, G.xT, "xT")
        P.emit()


LAST_INPUT_NAMES = []


def build(nlayers=4, debug=None, stop_after=None, moe=True, only_layer=None):
    nc = bass.Bass("TRN2", target_bir_lowering=False)
    G = Ctx()
    G.stop_after = stop_after

    del LAST_INPUT_NAMES[:]

    def inp(name, shape, dt=F32):
        LAST_INPUT_NAMES.append(name)
        return nc.dram_tensor(name, list(shape), dt, kind="ExternalInput").ap()

    def scr(name, shape, dt=F32):
        return nc.dram_tensor(name, list(shape), dt).ap()

    G.x = inp("x", [TOK, D])
    G.ident = inp("ident", [128, 128], BF16)
    G.identf = inp("identf", [128, 128])
    G.cosT = inp("cosT", [128, TOK])
    G.sinT = inp("sinT", [128, TOK])
    G.cos64T = inp("cos64T", [128, TOK])
    G.sin64T = inp("sin64T", [128, TOK])
    G.kcos = inp("kcos", [128, NT, 32])
    G.ksin = inp("ksin", [128, NT, 32])
    G.maskA = inp("maskA", [128, 128])
    G.maskB = inp("maskB", [128, 128])
    G.dilb = inp("dilb", [128, 2, 9 * 128])
    G.w_in_sh, G.w_out_sh, G.w_in_b, G.w_out_b, G.w_in_full, G.w_out_full = {}, {}, {}, {}, {}, {}
    G.lnp, G.rw, G.rb, G.wgu, G.bgu, G.wdn, G.bdn = {}, {}, {}, {}, {}, {}, {}
    G.idx_g, G.idx_b, G.negbf = {}, {}, {}
    for l in range(nlayers):
        if only_layer is not None and l != only_layer:
            continue
        W = W_IN[l % 4]
        G.w_in_full[l] = inp("l%d_w_in" % l, [D, W])
        G.w_out_full[l] = inp("l%d_w_out" % l, [D, D])
        G.lnp[l] = inp("l%d_lnp" % l, [4, D])
        G.rw[l] = inp("l%d_rw" % l, [D, 32])
        G.rb[l] = inp("l%d_rb" % l, [1, 32])
        if moe:
            G.wgu[l] = inp("l%d_wgu" % l, [4, D, D])
            G.bgu[l] = inp("l%d_bgu" % l, [4, 128, 16])
            G.wdn[l] = inp("l%d_wdn" % l, [4, 1024, D])
            G.bdn[l] = inp("l%d_bdn" % l, [4, D])
        if l % 4 == 1:
            G.idx_g[l] = inp("l%d_idxg" % l, [128, 64])
            G.idx_b[l] = inp("l%d_idxb" % l, [128, 64])
        if l % 4 == 2:
            G.negbf[l] = inp("l%d_bf" % l, [16, 1])
    G.out = nc.dram_tensor("out", [TOK, D], F32, kind="ExternalOutput").ap()
    G.xres = scr("xres", [TOK, D])
    G.xT = scr("xT", [KC, 128, TOK], BF16)
    G.qT = scr("qT", [16, 128, TOK], BF16)
    G.kT_own = scr("kT_own", [16, 128, TOK], BF16)
    G.kT_all = scr("kT_all", [4, 2, 4, 128, TOK], BF16)
    G.v_own = scr("v_own", [TOK, D], BF16)
    G.v_all = scr("v_all", [4, 2, 512, D], BF16)
    G.v_own4 = scr("v_own4", [TOK, 512], BF16)
    G.v_all4 = scr("v_all4", [2, TOK, 512], BF16)
    G.qiT = scr("qiT", [8, 128, TOK], BF16)
    G.kiT_own = scr("kiT_own", [128, TOK], BF16)
    G.kiT_all = scr("kiT_all", [2, 128, TOK], BF16)
    G.wi = scr("wi", [TOK, 16])
    G.sp_own = scr("sp_own", [16, TOK])
    G.sp_all = scr("sp_all", [2, 16, TOK])
    G.negck = scr("negck", [16, 2 * TOK])
    G.dsab = scr("dsab", [NT, 128, 2 * TOK], BF16)
    G.oT = scr("oT", [16, 128, TOK], BF16)
    G.x1T_own = scr("x1T_own", [KC, 128, TOK], BF16)
    G.x1T_all = scr("x1T_allc", [8, 2, 2, 2, 256, TOK], BF16)
    G.ag1 = scr("ag1", [8, 4, 256, TOK], BF16)
    G.yacc = scr("yacc", [8 * TOK, D])
    G.ypart = scr("ypart", [4, 4, 2, 512, D], BF16)
    G.y2 = scr("y2", [4, 4, 512, D], BF16)
    G.yown = scr("yown", [TOK, D], BF16)
    dbg = {}
    if debug:
        for name, shape, dt in debug:
            dbg[name] = nc.dram_tensor("dbg_" + name, list(shape), dt, kind="ExternalOutput").ap()
    G.dbg = dbg

    with contextlib.ExitStack() as st:
        P = Prog(nc, st)
        sems = P.all_sems()
        with nc.Block() as block:
            @block.gpsimd
            def _(e):
                for s in sems:
                    e.sem_clear(s)
        phase_prep(nc, P, G)
        for l in range(nlayers):
            if stop_after in (("weights",), ("prep",)):
                break
            if only_layer is not None and l != only_layer:
                continue
            phase_proj(nc, P, G, l)
            if G.stop_after == ("proj", l):
                break
            if l % 4 == 1:
                phase_dsa_index(nc, P, G, l)
            if l % 4 == 2:
                phase_fox_cum(nc, P, G, l)
            phase_attn(nc, P, G, l)
            if G.stop_after == ("attn", l):
                break
            phase_out_ln1(nc, P, G, l)
            if G.stop_after == ("ln1", l):
                break
            phase_moe(nc, P, G, l)
            if G.stop_after == ("moe", l):
                break
            phase_ln2(nc, P, G, l, l == nlayers - 1)
        for name, ap in dbg.items():
            src = G.w_out_full[0] if name == "w_out_full0" else getattr(G, name)
            n0 = src.shape[0]
            flat_s = src if len(src.shape) == 2 else src.rearrange(
                {3: "a b c -> (a b) c", 4: "a b c d -> (a b c) d", 5: "a b c d e -> (a b c d) e"}[len(src.shape)])
            flat_d = ap if len(ap.shape) == 2 else ap.rearrange(
                {3: "a b c -> (a b) c", 4: "a b c d -> (a b c) d", 5: "a b c d e -> (a b c d) e"}[len(ap.shape)])
            P.dma(flat_d, flat_s, reads=[], writes=[("dbg", name)])
        fin = sb(st, nc, "fin", [128, 8], F32)
        P.i("dve", "memset", [("dbg", n) for n in dbg] + ["out"], ["fin"], fin[:], 0.0)
        P.emit()
    return nc


def _consts(p):
    t = np.arange(TOK)
    pos = ((2 * (t // 128) + p) * 128 + (t % 128)).astype(np.float32)
    c = {}
    c["ident"] = np.eye(128, dtype=np.float32).astype(ml_dtypes.bfloat16)
    c["identf"] = np.eye(128, dtype=np.float32)
    d = np.arange(128)
    inv = (10000.0 ** (-(np.arange(64, dtype=np.float32)) / 64)).astype(np.float32)
    ang = pos[None, :] * inv[d % 64][:, None]
    c["cosT"] = np.cos(ang).astype(np.float32)
    c["sinT"] = (np.sin(ang) * np.where(d < 64, -1.0, 1.0)[:, None]).astype(np.float32)
    inv32 = (10000.0 ** (-(np.arange(32, dtype=np.float32)) / 32)).astype(np.float32)
    ang = pos[None, :] * inv32[d % 32][:, None]
    c["cos64T"] = np.cos(ang).astype(np.float32)
    c["sin64T"] = (np.sin(ang) * np.where((d % 64) < 32, -1.0, 1.0)[:, None]).astype(np.float32)
    angk = pos.reshape(NT, 128).T[:, :, None] * inv32[None, None, :]
    c["kcos"] = np.cos(angk).astype(np.float32)
    c["ksin"] = np.sin(angk).astype(np.float32)
    qi = np.arange(128)[:, None]
    ki = np.arange(128)[None, :]
    tri = np.where(ki <= qi, 0.0, NEG).astype(np.float32)
    allneg = np.full((128, 128), NEG, np.float32)
    zer = np.zeros((128, 128), np.float32)
    c["maskA"] = tri if p == 0 else zer
    c["maskB"] = allneg if p == 0 else tri
    dil = np.zeros((128, 2, 9, 128), np.float32)
    for r in range(2):
        for kp in range(9):
            m = 2 * (8 - kp) + p - r
            dd = m * 128 + qi - ki
            cnt = ((dd >= 0) & (dd <= 128)).astype(np.int32) + ((dd >= 0) & (dd <= 512) & (dd % 4 == 0)) \
                + ((dd >= 0) & (dd <= 2048) & (dd % 16 == 0))
            dil[:, r, kp, :] = np.where(cnt > 0, np.log(np.maximum(cnt, 1)), NEG)
    c["dilb"] = dil.reshape(128, 2, 9 * 128)
    return c


def make_in_maps(inputs, nlayers=4):
    maps = []
    x = np.asarray(inputs["x"])
    for c in range(NCORES):
        b, p = c // 2, c % 2
        m = dict(_consts(p))
        m["x"] = np.ascontiguousarray(x[b].reshape(16, 2, 128, D)[:, p].reshape(TOK, D))
        for l in range(nlayers):
            pre = "l%d_" % l
            m[pre + "w_in"] = np.asarray(inputs[pre + "w_in"])
            m[pre + "w_out"] = np.asarray(inputs[pre + "w_out"])
            m[pre + "lnp"] = np.stack([inputs[pre + "ln1_g"], inputs[pre + "ln1_b"],
                                       inputs[pre + "ln2_g"], inputs[pre + "ln2_b"]]).astype(np.float32)
            perm = list(range(4 * c, 4 * c + 4)) + [e for e in range(32) if not (4 * c <= e < 4 * c + 4)]
            m[pre + "rw"] = np.ascontiguousarray(np.asarray(inputs[pre + "router_w"])[:, perm])
            m[pre + "rb"] = np.ascontiguousarray(np.asarray(inputs[pre + "router_b"])[perm][None, :])
            m[pre + "wgu"] = np.ascontiguousarray(inputs[pre + "w_gu"][4 * c:4 * c + 4])
            m[pre + "bgu"] = np.ascontiguousarray(np.asarray(inputs[pre + "b_gu"][4 * c:4 * c + 4]).reshape(4, 16, 128).transpose(0, 2, 1))
            m[pre + "wdn"] = np.ascontiguousarray(inputs[pre + "w_dn"][4 * c:4 * c + 4])
            m[pre + "bdn"] = np.ascontiguousarray(inputs[pre + "b_dn"][4 * c:4 * c + 4])
            if l % 4 == 1:
                m[pre + "idxg"] = np.ascontiguousarray(np.broadcast_to(np.asarray(inputs[pre + "idx_norm_g"])[None, :], (128, 64)))
                m[pre + "idxb"] = np.ascontiguousarray(np.broadcast_to(np.asarray(inputs[pre + "idx_norm_b"])[None, :], (128, 64)))
            if l % 4 == 2:
                m[pre + "bf"] = np.ascontiguousarray(np.asarray(inputs[pre + "b_forget"])[:, None])
        maps.append(m)
    return maps


def kernel(**inputs):
    nc = build(4)
    maps = make_in_maps(inputs, 4)
    res = run_bass_kernel_spmd(nc, maps, core_ids=list(range(NCORES)))
    out = np.zeros((4, 4096, D), np.float32)
    for c in range(NCORES):
        b, p = c // 2, c % 2
        out[b].reshape(16, 2, 128, D)[:, p] = res.results[c]["out"].reshape(16, 128, D)
    return out
```

```python
import contextlib
import numpy as np
import ml_dtypes
import concourse.bass as bass
import concourse.mybir as mybir
from concourse.bass_utils import run_bass_kernel_spmd

F32 = mybir.dt.float32
BF16 = mybir.dt.bfloat16
ALU = mybir.AluOpType
AF = mybir.ActivationFunctionType
AX = mybir.AxisListType

NCORES = 8
D = 2048
KC = 16
NT = 16
TOK = 2048
NEG = -1.0e30
ALPHA = 8 ** 0.25
LN_EPS = 1e-5
SCALE = 128 ** -0.5
W_IN = (6144, 4176, 6160, 6144)
PAIRS = [[0, 1], [2, 3], [4, 5], [6, 7]]
WORLD = [list(range(8))]


class Prog:
    CENG = ("pe", "act", "dve", "pool")
    ND = 10

    def __init__(self, nc, stack):
        self.nc = nc
        self.csem = {e: stack.enter_context(nc.semaphore("c_" + e)) for e in self.CENG}
        self.dsem = {q: [stack.enter_context(nc.semaphore("d_%s%d" % (q, i))) for i in range(self.ND)]
                     for q in ("sp", "pool", "act")}
        self.ccsem = [stack.enter_context(nc.semaphore("cc%d" % i)) for i in range(30)]
        self.reset()

    def all_sems(self):
        s = list(self.csem.values()) + list(self.ccsem)
        for q in self.dsem:
            s += self.dsem[q]
        return s

    def reset(self):
        self.ops = {e: [] for e in ("pe", "act", "dve", "pool", "sp")}
        self.cnt = {e: 0 for e in self.CENG}
        self.dcnt = {q: [0] * self.ND for q in self.dsem}
        self.drr = {q: 0 for q in self.dsem}
        self.ccn = 0
        self.last_w = {}
        self.readers = {}
        self.waited = {e: {} for e in self.ops}

    def _deps(self, eng, reads, writes):
        deps = []
        for r in reads:
            if r in self.last_w:
                deps.append(self.last_w[r])
        for w in writes:
            if w in self.last_w:
                deps.append(self.last_w[w])
            deps.extend(self.readers.get(w, ()))
        return deps

    def _finish(self, eng, deps, fn, tok, amt, reads, writes):
        best = {}
        for (sem, v) in deps:
            if eng == "pe" and sem is self.csem["pe"]:
                continue
            k = id(sem)
            if k not in best or best[k][1] < v:
                best[k] = (sem, v)
        waits = []
        wd = self.waited[eng]
        for k, (sem, v) in best.items():
            if wd.get(k, 0) >= v:
                continue
            wd[k] = v
            waits.append((sem, v))
        self.ops[eng].append((waits, fn, tok[0], amt))
        for w in writes:
            self.last_w[w] = tok
            self.readers[w] = []
        for r in reads:
            self.readers.setdefault(r, []).append(tok)

    def op(self, eng, fn, reads=(), writes=()):
        deps = self._deps(eng, reads, writes)
        self.cnt[eng] += 1
        tok = (self.csem[eng], self.cnt[eng])
        self._finish(eng, deps, fn, tok, 1, reads, writes)

    def i(self, eng, name, reads, writes, *args, **kw):
        self.op(eng, lambda e: getattr(e, name)(*args, **kw), reads=reads, writes=writes)

    def dma(self, out, in_, reads=(), writes=(), q="sp"):
        eng = q
        deps = self._deps(eng, reads, writes)
        i = self.drr[q] % self.ND
        self.drr[q] += 1
        sem = self.dsem[q][i]
        if self.dcnt[q][i] > 0:
            deps.append((sem, 16 * self.dcnt[q][i]))
        self.dcnt[q][i] += 1
        tok = (sem, 16 * self.dcnt[q][i])
        self._finish(eng, deps, lambda e: e.dma_start(out=out, in_=in_), tok, 16, reads, writes)

    def collective(self, kind, op, groups, in_ap, out_ap, reads=(), writes=()):
        deps = self._deps("pool", reads, writes)
        sem = self.ccsem[self.ccn]
        self.ccn += 1
        tok = (sem, 1)

        def fn(e):
            return e.collective_compute(kind, op, replica_groups=groups, ins=[in_ap], outs=[out_ap])
        self._finish("pool", deps, fn, tok, None, reads, writes)

    def emit(self):
        nc = self.nc
        ops = self.ops
        for q in self.dsem:
            eng = q
            waits = [(self.dsem[q][i], 16 * self.dcnt[q][i]) for i in range(self.ND) if self.dcnt[q][i] > 0]
            if q == "pool":
                waits += [(self.ccsem[i], 1) for i in range(self.ccn)]
            if waits:
                ops[eng].append((waits, None, None, None))

        def replay(e, lst):
            for waits, fn, sem, amt in lst:
                for (s, v) in waits:
                    e.wait_ge(s, v)
                if fn is None:
                    continue
                ins = fn(e)
                if amt is None:
                    ins.then_inc(sem)
                else:
                    ins.then_inc(sem, amt)

        with nc.Block() as block:
            @block.sync
            def _(e):
                replay(e, ops["sp"])

            @block.scalar
            def _(e):
                replay(e, ops["act"])

            @block.vector
            def _(e):
                replay(e, ops["dve"])

            @block.gpsimd
            def _(e):
                replay(e, ops["pool"])

            @block.tensor
            def _(e):
                replay(e, ops["pe"])
        sems = self.all_sems()
        with nc.Block() as block:
            @block.gpsimd
            def _(e):
                for s in sems:
                    e.sem_clear(s)
        self.reset()


class Ctx:
    pass


_UNIQ = [0]


def sb(st, nc, name, shape, dt):
    _UNIQ[0] += 1
    return st.enter_context(nc.sbuf_tensor("s%d_%s" % (_UNIQ[0], name), shape, dt))


def ps(st, nc, name, shape, dt):
    _UNIQ[0] += 1
    return st.enter_context(nc.psum_tensor("p%d_%s" % (_UNIQ[0], name), shape, dt))


def emit_xT(P, C, src_ap, src_key, j, dstT, tag):
    i = C.xt_i
    C.xt_i += 1
    xb = C.xb[i % 2]
    xtt = C.xtt[i % 2]
    pt = C.ptx[i % 2]
    ptk = ("ptx", i % 2 if C.ptx[0] is not C.ptx[1] else 0)
    P.i("act", "activation", [src_key], [("xb", i % 2)], out=xb[:], in_=src_ap, func=AF.Copy)
    for k in range(KC):
        P.i("pe", "transpose", [("xb", i % 2), "ident"], [ptk], out=pt[:, k * 128:(k + 1) * 128], in_=xb[:, k * 128:(k + 1) * 128],
                                              identity=C.ident[:])
    P.i("dve", "tensor_copy", [ptk], [("xtt", i % 2)], out=xtt[:], in_=pt[:])
    P.dma(dstT.rearrange("k p t -> p k t")[:, :, j * 128:(j + 1) * 128],
          xtt[:].rearrange("p (k t) -> p k t", k=KC),
          reads=[("xtt", i % 2)], writes=[(tag, j)])


def alloc_xT(st, nc, C, npt=2):
    C.xt_i = 0
    C.xb = [sb(st, nc, "xb%d" % i, [128, D], BF16) for i in range(2)]
    C.xtt = [sb(st, nc, "xtt%d" % i, [128, D], BF16) for i in range(2)]
    C.ptx = [ps(st, nc, "ptx%d" % i, [128, D], BF16) for i in range(npt)]
    if npt == 1:
        C.ptx = C.ptx * 2


def load_const(P, st, nc, name, dram_ap, shape, dt, key):
    t = sb(st, nc, name, shape, dt)
    P.dma(t[:], dram_ap, reads=[], writes=[key])
    return t


def emit_ln(P, C, z, zkey, gi, out_ap, out_key, i):
    st6 = C.ln_st[i % 2]
    mv = C.ln_mv[i % 2]
    for c in range(4):
        P.i("dve", "bn_stats", [zkey], [("lnst", i % 2)], out=st6[:, c * 6:(c + 1) * 6], in_=z[:, c * 512:(c + 1) * 512])
    P.i("dve", "bn_aggr", [("lnst", i % 2)], [("lnmv", i % 2)], out=mv[:, 0:2], in_=st6[:])
    P.i("dve", "tensor_scalar", [("lnmv", i % 2)], [("lnmv", i % 2)], out=mv[:, 2:3], in0=mv[:, 1:2], scalar1=LN_EPS, scalar2=None,
        op0=ALU.add)
    P.i("act", "activation", [("lnmv", i % 2)], [("lnmv", i % 2)], out=mv[:, 2:3], in_=mv[:, 2:3], func=AF.Sqrt)
    P.i("dve", "reciprocal", [("lnmv", i % 2)], [("lnmv", i % 2)], out=mv[:, 2:3], in_=mv[:, 2:3])
    P.i("dve", "tensor_scalar", [zkey, ("lnmv", i % 2)], [zkey], out=z, in0=z, scalar1=mv[:, 0:1], scalar2=mv[:, 2:3],
                                          op0=ALU.subtract, op1=ALU.mult)
    P.i("pool", "tensor_tensor", [zkey, "lnp"], [zkey], out=z, in0=z, in1=C.lnp[:, gi, :], op=ALU.mult)
    P.i("pool", "tensor_tensor", [zkey, "lnp"], [out_key], out=out_ap, in0=z, in1=C.lnp[:, gi + 1, :], op=ALU.add)


def phase_prep(nc, P, G):
    with contextlib.ExitStack() as st:
        C = Ctx()
        alloc_xT(st, nc, C)
        C.ident = load_const(P, st, nc, "ident", G.ident, [128, 128], BF16, "ident")
        xs = [sb(st, nc, "xs%d" % i, [128, D], F32) for i in range(2)]
        for j in range(NT):
            P.dma(xs[j % 2][:], G.x[j * 128:(j + 1) * 128, :], reads=[], writes=[("xs", j % 2)])
            emit_xT(P, C, xs[j % 2][:], ("xs", j % 2), j, G.xT, "xT")
        P.emit()


def proj_blocks(kind):
    blocks = []
    if kind in (0, 3):
        for hb in range(8):
            blocks.append(("fm_rope", hb * 256, 256, ("qT", hb * 2)))
        for hb in range(8):
            blocks.append(("fm_rope", 2048 + hb * 256, 256, ("kT", hb * 2)))
        for vb in range(8):
            blocks.append(("tm", 4096 + vb * 256, 256, ("v", vb * 256)))
    elif kind == 2:
        for hb in range(8):
            blocks.append(("fm_plain", hb * 256, 256, ("qT", hb * 2)))
        for hb in range(8):
            blocks.append(("fm_plain", 2048 + hb * 256, 256, ("kT", hb * 2)))
        for vb in range(8):
            blocks.append(("tm", 4096 + vb * 256, 256, ("v", vb * 256)))
        blocks.append(("fox", 6144, 16, None))
    else:
        for hb in range(8):
            blocks.append(("fm_rope", hb * 256, 256, ("qT", hb * 2)))
        for hb in range(2):
            blocks.append(("fm_rope", 2048 + hb * 256, 256, ("kT", hb * 2)))
        for vb in range(2):
            blocks.append(("tm", 2560 + vb * 256, 256, ("v", vb * 256)))
        for hb in range(4):
            blocks.append(("fm_rope64", 3072 + hb * 256, 256, ("qiT", hb * 2)))
        blocks.append(("kiwi", 4096, 80, None))
    return blocks


def phase_proj(nc, P, G, l):
    kind = l % 4
    w_full = G.w_in_full[l]
    W = W_IN[kind]
    with contextlib.ExitStack() as st:
        C = Ctx()
        xT_sb = sb(st, nc, "xT_sb", [128, KC, TOK], BF16)
        for k in range(KC):
            P.dma(xT_sb[:, k, :], G.xT[k], reads=[("xT", j) for j in range(NT)] if k == 0 else [],
                  writes=[("xT_sb", k)])
        xT_keys = [("xT_sb", k) for k in range(KC)]
        cosT = load_const(P, st, nc, "cosT", G.cosT, [128, TOK], F32, "cosT")
        sinT = load_const(P, st, nc, "sinT", G.sinT, [128, TOK], F32, "sinT")
        if kind == 1:
            cos64 = load_const(P, st, nc, "cos64", G.cos64T, [128, TOK], F32, "cos64")
            sin64 = load_const(P, st, nc, "sin64", G.sin64T, [128, TOK], F32, "sin64")
            kcos = load_const(P, st, nc, "kcos", G.kcos, [128, NT, 32], F32, "kcos")
            ksin = load_const(P, st, nc, "ksin", G.ksin, [128, NT, 32], F32, "ksin")
            idxg = load_const(P, st, nc, "idxg", G.idx_g[l], [128, 64], F32, "idxg")
            idxb = load_const(P, st, nc, "idxb", G.idx_b[l], [128, 64], F32, "idxb")
            identf = load_const(P, st, nc, "identf", G.identf, [128, 128], F32, "identf")
        if kind == 2:
            negbf = load_const(P, st, nc, "negbf", G.negbf[l], [16, 1], F32, "negbf")
        wst = [sb(st, nc, "wst%d" % i, [128, KC, 256], F32) for i in range(2)]
        wbf = [sb(st, nc, "wbf%d" % i, [128, KC, 256], BF16) for i in range(2)]
        wsw = [sb(st, nc, "wsw%d" % i, [128, KC, 256], BF16) for i in range(2)]
        pa = [ps(st, nc, "pa%d" % i, [128, 512], F32) for i in range(2)]
        pb = [ps(st, nc, "pb%d" % i, [128, 512], F32) for i in range(2)]
        pv = [ps(st, nc, "pv%d" % i, [128, 512], F32) for i in range(2)]
        t1 = [sb(st, nc, "t1_%d" % i, [128, 512], F32) for i in range(2)]
        t2 = [sb(st, nc, "t2_%d" % i, [128, 512], F32) for i in range(2)]
        ob = [sb(st, nc, "ob%d" % i, [128, 512], BF16) for i in range(2)]
        vb_ = [sb(st, nc, "vbo%d" % i, [128, 256], BF16) for i in range(2)]
        if kind == 1:
            kw = [sb(st, nc, "kw%d" % i, [128, 80], F32) for i in range(2)]
            kst = [sb(st, nc, "kst%d" % i, [128, 8], F32) for i in range(2)]
            kr = [sb(st, nc, "kr%d" % i, [128, 64], F32) for i in range(2)]
            kt_ = [sb(st, nc, "ktmp%d" % i, [128, 64], F32) for i in range(2)]
            kiTs = [sb(st, nc, "kiTs%d" % i, [128, 128], BF16) for i in range(2)]
            wis = [sb(st, nc, "wis%d" % i, [128, 16], F32) for i in range(2)]
            pk = [ps(st, nc, "pk%d" % i, [128, 128], F32) for i in range(2)]
        if kind == 2:
            fx = sb(st, nc, "fx", [16, TOK], F32)
        blocks = proj_blocks(kind)
        cnt = {"a": 0, "v": 0, "o": 0, "k": 0}

        def load_block(bi):
            typ, c0, ncol, _ = blocks[bi]
            s = bi % 2
            P.dma(wst[s][:, :, 0:ncol], w_full[:, c0:c0 + ncol].rearrange("(k p) c -> p k c", p=128),
                  reads=["w_in_full"], writes=[("wst", s)])
            P.i("pool", "tensor_copy", [("wst", s)], [("wbf", s)], out=wbf[s][:, :, 0:ncol], in_=wst[s][:, :, 0:ncol])
            if typ in ("fm_rope", "fm_rope64"):
                hw = 64 if typ == "fm_rope" else 32
                v4 = wbf[s][:].rearrange("p k (g two h) -> p k g two h", two=2, h=hw)
                o4 = wsw[s][:].rearrange("p k (g two h) -> p k g two h", two=2, h=hw)
                for k in range(KC):
                    P.i("pool", "tensor_copy", [("wbf", s)], [("wsw", s)], out=o4[:, k, :, 0, :], in_=v4[:, k, :, 1, :])
                    P.i("pool", "tensor_copy", [("wbf", s)], [("wsw", s)], out=o4[:, k, :, 1, :], in_=v4[:, k, :, 0, :])

        load_block(0)
        for bi, (typ, c0, ncol, dst) in enumerate(blocks):
            if bi + 1 < len(blocks):
                load_block(bi + 1)
            s = bi % 2
            if typ in ("fm_rope", "fm_plain", "fm_rope64"):
                for hh in range(2):
                    if dst[0] == "qT":
                        dram = G.qT[dst[1] + hh]
                    elif dst[0] == "kT":
                        dram = G.kT_own[dst[1] + hh]
                    else:
                        dram = G.qiT[dst[1] + hh]
                    for c4 in range(4):
                        a = cnt["a"] % 2
                        cnt["a"] += 1
                        tsl = slice(c4 * 512, (c4 + 1) * 512)
                        for k in range(KC):
                            P.i("pe", "matmul", [("wbf", s), ("xT_sb", k)], [("pa", a)], pa[a][:], lhsT=wbf[s][:, k, hh * 128:(hh + 1) * 128], rhs=xT_sb[:, k, tsl],
                                start=(k == 0), stop=(k == KC - 1))
                        o = cnt["o"] % 2
                        cnt["o"] += 1
                        if typ == "fm_plain":
                            P.i("act", "activation", [("pa", a)], [("ob", o)], out=ob[o][:], in_=pa[a][:], func=AF.Copy)
                        else:
                            for k in range(KC):
                                P.i("pe", "matmul", [("wsw", s), ("xT_sb", k)], [("pb", a)], pb[a][:], lhsT=wsw[s][:, k, hh * 128:(hh + 1) * 128], rhs=xT_sb[:, k, tsl],
                                    start=(k == 0), stop=(k == KC - 1))
                            ct, sn = (cosT, sinT) if typ == "fm_rope" else (cos64, sin64)
                            ck, sk = ("cosT", "sinT") if typ == "fm_rope" else ("cos64", "sin64")
                            P.i("dve", "tensor_tensor", [("pa", a), ck], [("t1", o)], out=t1[o][:], in0=pa[a][:], in1=ct[:, tsl], op=ALU.mult)
                            P.i("dve", "tensor_tensor", [("pb", a), sk], [("t2", o)], out=t2[o][:], in0=pb[a][:], in1=sn[:, tsl], op=ALU.mult)
                            P.i("pool", "tensor_tensor", [("t1", o), ("t2", o)], [("ob", o)], out=ob[o][:], in0=t1[o][:], in1=t2[o][:],
                                                                        op=ALU.add)
                        P.dma(dram[:, tsl], ob[o][:], reads=[("ob", o)], writes=[dst[0] + "_own"])
            elif typ == "tm":
                for j in range(NT):
                    a = cnt["v"] % 2
                    cnt["v"] += 1
                    for k in range(KC):
                        P.i("pe", "matmul", [("wbf", s), ("xT_sb", k)], [("pv", a)], pv[a][:, 0:ncol], lhsT=xT_sb[:, k, j * 128:(j + 1) * 128], rhs=wbf[s][:, k, 0:ncol],
                            start=(k == 0), stop=(k == KC - 1))
                    P.i("act", "activation", [("pv", a)], [("vbo", a)], out=vb_[a][:, 0:ncol], in_=pv[a][:, 0:ncol], func=AF.Copy)
                    P.dma((G.v_own4 if kind == 1 else G.v_own)[j * 128:(j + 1) * 128, dst[1]:dst[1] + ncol], vb_[a][:, 0:ncol],
                          reads=[("vbo", a)], writes=["v_own"])
            elif typ == "fox":
                for c4 in range(4):
                    a = cnt["a"] % 2
                    cnt["a"] += 1
                    tsl = slice(c4 * 512, (c4 + 1) * 512)
                    for k in range(KC):
                        P.i("pe", "matmul", [("wbf", s), ("xT_sb", k)], [("pa", a)], pa[a][0:16, :], lhsT=wbf[s][:, k, 0:16], rhs=xT_sb[:, k, tsl],
                            start=(k == 0), stop=(k == KC - 1))
                    P.i("dve", "tensor_scalar", [("pa", a), "negbf"], ["fx"], out=fx[:, tsl], in0=pa[a][0:16, :],
                                                                         scalar1=negbf[:, 0:1], scalar2=None, op0=ALU.add)
                P.i("act", "activation", ["fx"], ["fx"], out=fx[:], in_=fx[:], func=AF.Exp, scale=-1.0)
                P.i("act", "activation", ["fx"], ["fx"], out=fx[:], in_=fx[:], func=AF.Ln, bias=1.0, scale=1.0)
                P.dma(G.sp_own[:, :], fx[:], reads=["fx"], writes=["sp_own"])
            elif typ == "kiwi":
                for j in range(NT):
                    a = cnt["k"] % 2
                    cnt["k"] += 1
                    for k in range(KC):
                        P.i("pe", "matmul", [("wbf", s), ("xT_sb", k)], [("pv", a)], pv[a][:, 0:80], lhsT=xT_sb[:, k, j * 128:(j + 1) * 128], rhs=wbf[s][:, k, 0:80],
                            start=(k == 0), stop=(k == KC - 1))
                    P.i("act", "activation", [("pv", a)], [("kw", a)], out=kw[a][:], in_=pv[a][:, 0:80], func=AF.Copy)
                    P.i("dve", "tensor_scalar", [("kw", a)], [("wis", a)], out=wis[a][:], in0=kw[a][:, 64:80],
                                                               scalar1=1.0 / 32.0, scalar2=None, op0=ALU.mult)
                    P.dma(G.wi[j * 128:(j + 1) * 128, :], wis[a][:], reads=[("wis", a)], writes=["wi"])
                    P.i("dve", "bn_stats", [("kw", a)], [("kst", a)], out=kst[a][:, 0:6], in_=kw[a][:, 0:64])
                    P.i("dve", "bn_aggr", [("kst", a)], [("kst", a)], out=kst[a][:, 6:8], in_=kst[a][:, 0:6])
                    P.i("dve", "tensor_scalar", [("kst", a)], [("kst", a)], out=kst[a][:, 0:1], in0=kst[a][:, 7:8], scalar1=LN_EPS,
                        scalar2=None, op0=ALU.add)
                    P.i("act", "activation", [("kst", a)], [("kst", a)], out=kst[a][:, 0:1], in_=kst[a][:, 0:1], func=AF.Sqrt)
                    P.i("dve", "reciprocal", [("kst", a)], [("kst", a)], out=kst[a][:, 0:1], in_=kst[a][:, 0:1])
                    P.i("dve", "tensor_scalar", [("kw", a), ("kst", a)], [("kr", a)], out=kr[a][:], in0=kw[a][:, 0:64], scalar1=kst[a][:, 6:7],
                                                               scalar2=kst[a][:, 0:1], op0=ALU.subtract, op1=ALU.mult)
                    P.i("dve", "tensor_tensor", [("kr", a), "idxg"], [("kr", a)], out=kr[a][:], in0=kr[a][:], in1=idxg[:], op=ALU.mult)
                    P.i("dve", "tensor_tensor", [("kr", a), "idxb"], [("kr", a)], out=kr[a][:], in0=kr[a][:], in1=idxb[:], op=ALU.add)
                    x1 = kr[a][:, 0:32]
                    x2 = kr[a][:, 32:64]
                    P.i("dve", "tensor_tensor", [("kr", a), "kcos"], [("ktmp", a)], out=kt_[a][:, 0:32], in0=x1, in1=kcos[:, j, :],
                                                                           op=ALU.mult)
                    P.i("dve", "tensor_tensor", [("kr", a), "ksin"], [("ktmp", a)], out=kt_[a][:, 32:64], in0=x2, in1=ksin[:, j, :],
                                                                           op=ALU.mult)
                    P.i("dve", "tensor_tensor", [("ktmp", a)], [("kw", a)], out=kw[a][:, 0:32], in0=kt_[a][:, 0:32], in1=kt_[a][:, 32:64],
                                                               op=ALU.subtract)
                    P.i("dve", "tensor_tensor", [("kr", a), "kcos", ("kw", a)], [("ktmp", a)], out=kt_[a][:, 0:32], in0=x2, in1=kcos[:, j, :],
                                                                           op=ALU.mult)
                    P.i("dve", "tensor_tensor", [("kr", a), "ksin"], [("ktmp", a)], out=kt_[a][:, 32:64], in0=x1, in1=ksin[:, j, :],
                                                                           op=ALU.mult)
                    P.i("dve", "tensor_tensor", [("ktmp", a)], [("kw", a)], out=kw[a][:, 32:64], in0=kt_[a][:, 0:32], in1=kt_[a][:, 32:64],
                                                               op=ALU.add)
                    P.i("pe", "transpose", [("kw", a), "identf"], [("pk", a)], out=pk[a][0:64, :], in_=kw[a][:, 0:64], identity=identf[:])
                    P.i("act", "activation", [("pk", a)], [("kiTs", a)], out=kiTs[a][0:64, :], in_=pk[a][0:64, :], func=AF.Copy)
                    P.dma(G.kiT_own[0:64, j * 128:(j + 1) * 128], kiTs[a][0:64, :], reads=[("kiTs", a)],
                          writes=["kiT_own"])
                    P.dma(G.kiT_own[64:128, j * 128:(j + 1) * 128], kiTs[a][0:64, :], reads=[("kiTs", a)],
                          writes=["kiT_own"])
        nkc = 1 if kind == 1 else 4
        for k in range(nkc):
            P.collective("AllGather", ALU.bypass, PAIRS,
                         G.kT_own[4 * k:4 * k + 4].rearrange("h d t -> (h d) t"),
                         G.kT_all[k].rearrange("r h d t -> (r h d) t"),
                         reads=["kT_own"], writes=["kT_all"])
        if kind == 1:
            P.collective("AllGather", ALU.bypass, PAIRS, G.v_own4[:, :], G.v_all4.rearrange("r t c -> (r t) c"),
                         reads=["v_own"], writes=["v_all"])
            P.collective("AllGather", ALU.bypass, PAIRS, G.kiT_own[:, :], G.kiT_all.rearrange("r d t -> (r d) t"),
                         reads=["kiT_own"], writes=["kiT_all"])
        else:
            for k in range(4):
                P.collective("AllGather", ALU.bypass, PAIRS, G.v_own[k * 512:(k + 1) * 512, :],
                             G.v_all[k].rearrange("r t c -> (r t) c"),
                             reads=["v_own"], writes=["v_all"])
        if kind == 2:
            P.collective("AllGather", ALU.bypass, PAIRS, G.sp_own[:, :], G.sp_all.rearrange("r h t -> (r h) t"),
                         reads=["sp_own"], writes=["sp_all"])
        P.emit()


def phase_fox_cum(nc, P, G, l):
    with contextlib.ExitStack() as st:
        spg = sb(st, nc, "spg", [16, 2 * TOK], F32)
        ones = sb(st, nc, "ones", [16, 2 * TOK], F32)
        cum = sb(st, nc, "cum", [16, 2 * TOK], F32)
        for r in range(2):
            P.dma(spg[:].rearrange("h (j r i) -> h j r i", r=2, i=128)[:, :, r, :],
                  G.sp_all[r].rearrange("h (j i) -> h j i", i=128), reads=[], writes=["spg"])
        P.i("pool", "memset", [], ["ones"], ones[:], 1.0)
        P.i("dve", "tensor_tensor_scan", ["spg", "ones"], ["cum"], out=cum[:], data0=ones[:], data1=spg[:],
            initial=0.0, op0=ALU.mult, op1=ALU.add)
        for r in range(2):
            P.dma(G.negck.rearrange("h (r j i) -> h r j i", r=2, i=128)[:, r],
                  cum[:].rearrange("h (j r i) -> h j r i", r=2, i=128)[:, :, r, :], reads=["cum"], writes=["negck"])
        P.emit()


def phase_attn(nc, P, G, l):
    kind = l % 4
    with contextlib.ExitStack() as st:
        ident = load_const(P, st, nc, "ident", G.ident, [128, 128], BF16, "ident")
        maskA = load_const(P, st, nc, "maskA", G.maskA, [128, 128], F32, "maskA")
        maskB = load_const(P, st, nc, "maskB", G.maskB, [128, 128], F32, "maskB")
        masks = (maskA, maskB)
        if kind == 0:
            dilb = load_const(P, st, nc, "dilb", G.dilb, [128, 2, 9 * 128], F32, "dilb")
        nkv = 4 if kind == 1 else 2
        if kind == 1:
            KT = [sb(st, nc, "KT4", [128, 4, 2, TOK], BF16)]
            V = [sb(st, nc, "V4", [128, 2, NT, 512], BF16)]
            QT = [sb(st, nc, "QTj%d" % i, [128, 16, 128], BF16) for i in range(2)]
            Bias = [sb(st, nc, "Bias%d" % i, [128, 2, TOK], BF16) for i in range(2)]
        else:
            KT = [sb(st, nc, "KT%d" % i, [128, 2, TOK], BF16) for i in range(2)]
            V = [sb(st, nc, "V%d" % i, [128, 2, NT, 128], BF16) for i in range(2)]
            QT = [sb(st, nc, "QT%d" % i, [128, TOK], BF16) for i in range(2)]
        if kind == 2:
            Bias = [sb(st, nc, "Bias%d" % i, [128, 2, TOK], F32) for i in range(2)]
        S = [sb(st, nc, "S%d" % i, [128, 2, TOK], F32) for i in range(3)]
        Pb = [sb(st, nc, "Pb%d" % i, [128, 2, TOK], BF16) for i in range(3)]
        PT = [sb(st, nc, "PT%d" % i, [128, 32 * 128], BF16) for i in range(3)]
        sm = [sb(st, nc, "sm%d" % i, [128, 8], F32) for i in range(3)]
        Osb = [sb(st, nc, "Osb%d" % i, [128, 128], BF16) for i in range(3)]
        OT = [sb(st, nc, "OT%d" % i, [128, 128], BF16) for i in range(3)]
        sps = [ps(st, nc, "sps%d" % i, [128, 512], F32) for i in range(3)]
        ptp = [ps(st, nc, "ptp%d" % i, [128, 1024], BF16) for i in range(2)]
        pmisc = ps(st, nc, "pmisc", [128, 512], F32)
        otp = ps(st, nc, "otp", [128, 128], BF16)
        cn = {"sps": 0, "ptp": 0, "it": 0}

        if kind == 3:
            km2 = [sb(st, nc, "km2_%d" % i, [128, 2, NT], F32) for i in range(2)]
            kmT = [sb(st, nc, "kmT%d" % i, [128, NT], BF16) for i in range(2)]
            gm = [sb(st, nc, "gm%d" % i, [128, 16], F32) for i in range(3)]
            m8 = [sb(st, nc, "m8_%d" % i, [128, 8], F32) for i in range(3)]
            bb = [sb(st, nc, "bb%d" % i, [128, 16], F32) for i in range(3)]

        def load_head(h):
            hb = h % 2
            for r in range(2):
                P.dma(KT[hb][:, r, :], G.kT_all[h // 4, r, h % 4], reads=[], writes=[("KT", hb)])
                for ck in range(4):
                    P.dma(V[hb][:, r, ck * 4:(ck + 1) * 4, :],
                          G.v_all[ck, r, :, h * 128:(h + 1) * 128].rearrange("(jj p) d -> p jj d", p=128),
                          reads=[], writes=[("V", hb)])
            P.dma(QT[hb][:], G.qT[h], reads=[], writes=[("QT", hb)])
            if kind == 2:
                P.dma(Bias[hb][:].rearrange("p r t -> p (r t)"), G.negck[h:h + 1, :].broadcast_to([128, 2 * TOK]),
                      reads=["negck"], writes=[("Bias", hb)])
            if kind == 3:
                P.i("dve", "tensor_reduce", [("KT", hb)], [("km2", hb)], out=km2[hb][:],
                    in_=KT[hb][:].rearrange("p r (j i) -> p r j i", i=128), axis=AX.X, op=ALU.add)
                P.i("dve", "tensor_tensor", [("km2", hb)], [("km2", hb)], out=km2[hb][:, 0, :], in0=km2[hb][:, 0, :],
                    in1=km2[hb][:, 1, :], op=ALU.add)
                P.i("dve", "tensor_scalar", [("km2", hb)], [("kmT", hb)], out=kmT[hb][:], in0=km2[hb][:, 0, :],
                    scalar1=1.0 / 256.0, scalar2=None, op0=ALU.mult)

        def core(h, j, qT_ap, qkeys, kt_fn, kkeys, v_fn, vkeys, bias_fn, bkeys):
            it = cn["it"]
            cn["it"] += 1
            p = it % 3
            jlo = max(0, j - 8) if kind == 0 else 0
            lo, hi = jlo * 128, (j + 1) * 128
            skeys = []
            if kind == 3:
                hb = h % 2
                P.i("pe", "matmul", qkeys + [("kmT", hb)], [("pgate", 0)], pmisc[:, 384:400], lhsT=qT_ap, rhs=kmT[hb][:],
                    start=True, stop=True)
                P.i("pool", "memset", [], [("gm", p)], gm[p][:], NEG)
                if j > 0:
                    P.i("dve", "tensor_copy", [("pgate", 0), ("gm", p)], [("gm", p)], out=gm[p][:, 0:j], in_=pmisc[:, 384:384 + j])
                P.i("dve", "max", [("gm", p)], [("m8", p)], out=m8[p][:], in_=gm[p][:])
                P.i("dve", "tensor_scalar", [("gm", p), ("m8", p)], [("bb", p)], out=bb[p][:], in0=gm[p][:],
                    scalar1=m8[p][:, 2:3], scalar2=None, op0=ALU.is_ge)
                P.i("dve", "tensor_scalar", [("bb", p)], [("bb", p)], out=bb[p][:], in0=bb[p][:], scalar1=-1.0,
                    scalar2=1.0e30, op0=ALU.add, op1=ALU.mult)
            for r in range(2):
                if kind == 3:
                    chunks = [(c0, min(512, j * 128 - c0)) for c0 in range(0, j * 128, 512)] + [(j * 128, 128)]
                else:
                    chunks = [(c0, min(512, hi - c0)) for c0 in range(lo, hi, 512)]
                for (c0, w) in chunks:
                    b = cn["sps"] % 3
                    cn["sps"] += 1
                    P.i("pe", "matmul", qkeys + kkeys, [("sps", b)], sps[b][:, 0:w], lhsT=qT_ap, rhs=kt_fn(r, c0, w),
                        start=True, stop=True)
                    key = ("S", p, r, c0)
                    skeys.append(key)
                    if kind == 0:
                        off = c0 - (j - 8) * 128
                        P.i("dve", "scalar_tensor_tensor", [("sps", b), "dilb"], [key], out=S[p][:, r, c0:c0 + w],
                            in0=sps[b][:, 0:w], scalar=SCALE, in1=dilb[:, r, off:off + w], op0=ALU.mult, op1=ALU.add)
                    elif kind == 3:
                        if c0 == j * 128:
                            P.i("dve", "scalar_tensor_tensor", [("sps", b), "maskA", "maskB"], [key],
                                out=S[p][:, r, c0:c0 + w], in0=sps[b][:, 0:w], scalar=SCALE, in1=masks[r][:],
                                op0=ALU.mult, op1=ALU.add)
                        else:
                            nt = w // 128
                            t0 = c0 // 128
                            P.i("dve", "scalar_tensor_tensor", [("sps", b), ("bb", p)], [key],
                                out=S[p][:, r, c0:c0 + w].rearrange("p (t i) -> p t i", i=128),
                                in0=sps[b][:, 0:w].rearrange("p (t i) -> p t i", i=128), scalar=SCALE,
                                in1=bb[p][:, t0:t0 + nt].unsqueeze(2).broadcast_to([128, nt, 128]),
                                op0=ALU.mult, op1=ALU.add)
                    else:
                        P.i("dve", "scalar_tensor_tensor", [("sps", b)] + bkeys, [key], out=S[p][:, r, c0:c0 + w],
                            in0=sps[b][:, 0:w], scalar=SCALE, in1=bias_fn(r, c0, w), op0=ALU.mult, op1=ALU.add)
                if kind in (1, 2):
                    key = [k for k in skeys if k[2] == r][-1]
                    P.i("dve", "tensor_tensor", [key, "maskA", "maskB"], [key], out=S[p][:, r, j * 128:(j + 1) * 128],
                        in0=S[p][:, r, j * 128:(j + 1) * 128], in1=masks[r][:], op=ALU.add)
            P.i("dve", "tensor_reduce", skeys, [("mx", p)], out=sm[p][:, 0:1], in_=S[p][:, :, lo:hi], axis=AX.XY, op=ALU.max)
            P.i("dve", "tensor_scalar", [("mx", p)], [("nmx", p)], out=sm[p][:, 1:2], in0=sm[p][:, 0:1], scalar1=-1.0,
                scalar2=None, op0=ALU.mult)
            P.i("pool", "memset", [], [("rs", p)], sm[p][:, 2:4], 0.0)
            for r in range(2):
                P.i("act", "activation", [k for k in skeys if k[2] == r] + [("nmx", p), ("rs", p)], [("Pb", p, r), ("rs", p)],
                    out=Pb[p][:, r, lo:hi], in_=S[p][:, r, lo:hi], func=AF.Exp, bias=sm[p][:, 1:2], scale=1.0,
                    accum_out=sm[p][:, 2 + r:3 + r])
            P.i("dve", "tensor_tensor", [("rs", p)], [("rinv", p)], out=sm[p][:, 4:5], in0=sm[p][:, 2:3], in1=sm[p][:, 3:4],
                op=ALU.add)
            P.i("dve", "reciprocal", [("rinv", p)], [("rinv", p)], out=sm[p][:, 4:5], in_=sm[p][:, 4:5])
            tiles = [(r, jj) for r in range(2) for jj in range(jlo, j + 1)]
            ngr = (len(tiles) + 7) // 8
            for g in range(ngr):
                grp = tiles[g * 8:(g + 1) * 8]
                pb_ = cn["ptp"] % 2
                cn["ptp"] += 1
                for i_, (r, jj) in enumerate(grp):
                    P.i("pe", "transpose", [("Pb", p, r), "ident"], [("ptp", pb_)], out=ptp[pb_][:, i_ * 128:(i_ + 1) * 128],
                        in_=Pb[p][:, r, jj * 128:(jj + 1) * 128], identity=ident[:])
                n = len(grp)
                eng = "act" if g % 2 == 0 else "dve"
                if eng == "act":
                    P.i("act", "activation", [("ptp", pb_)], [("PT", p, g)], out=PT[p][:, g * 1024:g * 1024 + n * 128],
                        in_=ptp[pb_][:, 0:n * 128], func=AF.Copy)
                else:
                    P.i("dve", "tensor_copy", [("ptp", pb_)], [("PT", p, g)], out=PT[p][:, g * 1024:g * 1024 + n * 128],
                        in_=ptp[pb_][:, 0:n * 128])
            po = pmisc[:, p * 128:(p + 1) * 128]
            for ti, (r, jj) in enumerate(tiles):
                P.i("pe", "matmul", [("PT", p, ti // 8)] + vkeys, [("po", p)], po, lhsT=PT[p][:, ti * 128:(ti + 1) * 128],
                    rhs=v_fn(r, jj), start=(ti == 0), stop=(ti == len(tiles) - 1))
            P.i("dve", "tensor_scalar", [("po", p), ("rinv", p)], [("Osb", p)], out=Osb[p][:], in0=po, scalar1=sm[p][:, 4:5],
                scalar2=None, op0=ALU.mult)
            P.i("pe", "transpose", [("Osb", p), "ident"], [("otp", 0)], out=otp[:], in_=Osb[p][:], identity=ident[:])
            P.i("act", "activation", [("otp", 0)], [("OT", p)], out=OT[p][:], in_=otp[:], func=AF.Copy)
            P.dma(G.oT[h][:, j * 128:(j + 1) * 128], OT[p][:], reads=[("OT", p)], writes=["oT"])

        if kind == 1:
            for r in range(2):
                for kv in range(4):
                    P.dma(KT[0][:, kv, r, :], G.kT_all[0, r, kv], reads=[], writes=["KT4"])
                P.dma(V[0][:, r, :, :], G.v_all4[r].rearrange("(j p) c -> p j c", p=128), reads=[], writes=["V4"])
            for j in range(NT):
                jb = j % 2
                P.dma(QT[jb][:], G.qT.rearrange("h d t -> d h t")[:, :, j * 128:(j + 1) * 128], reads=[], writes=[("QT", jb)])
                P.dma(Bias[jb][:].rearrange("p r t -> p (r t)"), G.dsab[j], reads=["dsab"], writes=[("Bias", jb)])
                for h in range(16):
                    kv = h // 4
                    core(h, j, QT[jb][:, h, :], [("QT", jb)],
                         lambda r, c0, w, kv=kv: KT[0][:, kv, r, c0:c0 + w], ["KT4"],
                         lambda r, jj, kv=kv: V[0][:, r, jj, kv * 128:(kv + 1) * 128], ["V4"],
                         lambda r, c0, w, jb=jb: Bias[jb][:, r, c0:c0 + w], [("Bias", jb)])
        else:
            load_head(0)
            for h in range(16):
                if h + 1 < 16:
                    load_head(h + 1)
                hb = h % 2
                for j in range(NT):
                    core(h, j, QT[hb][:, j * 128:(j + 1) * 128], [("QT", hb)],
                         lambda r, c0, w, hb=hb: KT[hb][:, r, c0:c0 + w], [("KT", hb)],
                         lambda r, jj, hb=hb: V[hb][:, r, jj, :], [("V", hb)],
                         (lambda r, c0, w, hb=hb: Bias[hb][:, r, c0:c0 + w]) if kind == 2 else None,
                         [("Bias", hb)] if kind == 2 else [])
        P.emit()


def phase_dsa_index(nc, P, G, l):
    with contextlib.ExitStack() as st:
        maskA = load_const(P, st, nc, "maskA", G.maskA, [128, 128], F32, "maskA")
        maskB = load_const(P, st, nc, "maskB", G.maskB, [128, 128], F32, "maskB")
        masks = (maskA, maskB)
        kiT = sb(st, nc, "kiT", [128, 2, TOK], BF16)
        for r in range(2):
            P.dma(kiT[:, r, :], G.kiT_all[r], reads=[], writes=["kiT"])
        qiT = [sb(st, nc, "qiT%d" % i, [128, 8, 128], BF16) for i in range(2)]
        wi = [sb(st, nc, "wi%d" % i, [128, 16], F32) for i in range(2)]
        sc = [sb(st, nc, "sc%d" % i, [128, 2 * TOK], F32) for i in range(2)]
        junk = sb(st, nc, "junk", [128, 2 * TOK], F32)
        mbf = [sb(st, nc, "mbf%d" % i, [128, 2 * TOK], BF16) for i in range(2)]
        rl = [sb(st, nc, "rl%d" % i, [128, 512], F32) for i in range(3)]
        bs = [sb(st, nc, "bs%d" % i, [128, 8], F32) for i in range(2)]
        lps = [ps(st, nc, "lps%d" % i, [128, 512], F32) for i in range(4)]
        cn = {"l": 0, "r": 0}
        for j in range(NT):
            p = j % 2
            hi = (j + 1) * 128
            P.dma(qiT[p][:], G.qiT.rearrange("g d t -> d g t")[:, :, j * 128:(j + 1) * 128], reads=[], writes=[("qiT", p)])
            P.dma(wi[p][:], G.wi[j * 128:(j + 1) * 128, :], reads=[], writes=[("wi", p)])
            for r in range(2):
                for c0 in range(0, hi, 512):
                    w = min(512, hi - c0)
                    key = ("sc", p, r, c0)
                    dst = sc[p][:, r * hi + c0:r * hi + c0 + w]
                    for ih in range(16):
                        g, half = ih // 2, ih % 2
                        b = cn["l"] % 4
                        cn["l"] += 1
                        P.i("pe", "matmul", [("qiT", p), "kiT"], [("lps", b)], lps[b][:, 0:w],
                            lhsT=qiT[p][half * 64:(half + 1) * 64, g, :], rhs=kiT[half * 64:(half + 1) * 64, r, c0:c0 + w],
                            start=True, stop=True)
                        q_ = cn["r"] % 3
                        cn["r"] += 1
                        P.i("act", "activation", [("lps", b)], [("rl", q_)], out=rl[q_][:, 0:w], in_=lps[b][:, 0:w], func=AF.Relu)
                        if ih == 0:
                            P.i("dve", "tensor_scalar", [("rl", q_), ("wi", p)], [key], out=dst, in0=rl[q_][:, 0:w],
                                scalar1=wi[p][:, 0:1], scalar2=None, op0=ALU.mult)
                        else:
                            P.i("dve", "scalar_tensor_tensor", [("rl", q_), ("wi", p), key], [key], out=dst, in0=rl[q_][:, 0:w],
                                scalar=wi[p][:, ih:ih + 1], in1=dst, op0=ALU.mult, op1=ALU.add)
            allk = [("sc", p, r, c0) for r in range(2) for c0 in range(0, hi, 512)]
            sk = ("scall", p)
            B = bs[p]
            bk = ("bs", p)
            P.i("dve", "tensor_reduce", allk, [bk], out=B[:, 6:7], in_=sc[p][:, 0:2 * hi], axis=AX.X, op=ALU.max,
                apply_absolute_value=True)
            for r in range(2):
                sl = sc[p][:, r * hi + j * 128:r * hi + (j + 1) * 128]
                P.i("dve", "tensor_tensor", allk + [bk, "maskA", "maskB"], [sk], out=sl, in0=sl, in1=masks[r][:], op=ALU.add)
            P.i("dve", "tensor_scalar", [bk], [bk], out=B[:, 1:2], in0=B[:, 6:7], scalar1=1.0, scalar2=None, op0=ALU.add)
            P.i("dve", "tensor_scalar", [bk], [bk], out=B[:, 0:1], in0=B[:, 1:2], scalar1=-1.0, scalar2=None, op0=ALU.mult)
            for it in range(17):
                P.i("dve", "tensor_scalar", [bk], [bk], out=B[:, 2:3], in0=B[:, 0:1], scalar1=B[:, 1:2], scalar2=0.5,
                    op0=ALU.add, op1=ALU.mult)
                P.i("dve", "tensor_scalar", [sk, bk], ["junk", bk], out=junk[:, 0:2 * hi], in0=sc[p][:, 0:2 * hi],
                    scalar1=B[:, 2:3], scalar2=0.0, op0=ALU.is_ge, op1=ALU.add, accum_out=B[:, 3:4])
                P.i("dve", "tensor_scalar", [bk], [bk], out=B[:, 4:5], in0=B[:, 3:4], scalar1=255.5, scalar2=None, op0=ALU.is_ge)
                P.i("dve", "tensor_tensor", [bk], [bk], out=B[:, 5:6], in0=B[:, 2:3], in1=B[:, 0:1], op=ALU.subtract)
                P.i("dve", "tensor_tensor", [bk], [bk], out=B[:, 7:8], in0=B[:, 1:2], in1=B[:, 2:3], op=ALU.subtract)
                P.i("dve", "scalar_tensor_tensor", [bk], [bk], out=B[:, 0:1], in0=B[:, 5:6], scalar=B[:, 4:5], in1=B[:, 0:1],
                    op0=ALU.mult, op1=ALU.add)
                P.i("dve", "scalar_tensor_tensor", [bk], [bk], out=B[:, 1:2], in0=B[:, 7:8], scalar=B[:, 4:5], in1=B[:, 2:3],
                    op0=ALU.mult, op1=ALU.add)
            P.i("dve", "tensor_scalar", [sk, bk], ["junk"], out=junk[:, 0:2 * hi], in0=sc[p][:, 0:2 * hi], scalar1=B[:, 0:1],
                scalar2=None, op0=ALU.is_ge)
            P.i("dve", "tensor_scalar", ["junk"], [("mbf", p)], out=mbf[p][:, 0:2 * hi], in0=junk[:, 0:2 * hi], scalar1=-1.0,
                scalar2=1.0e30, op0=ALU.add, op1=ALU.mult)
            for r in range(2):
                P.dma(G.dsab[j][:, r * TOK:r * TOK + hi], mbf[p][:, r * hi:(r + 1) * hi], reads=[("mbf", p)], writes=["dsab"])
        P.emit()


PAIRS_A = [[0, 1], [2, 3], [4, 5], [6, 7]]
PAIRS_B = [[0, 2], [1, 3], [4, 6], [5, 7]]
PAIRS_C = [[0, 4], [1, 5], [2, 6], [3, 7]]
QUADS = [[0, 1, 2, 3], [4, 5, 6, 7]]


def phase_out_ln1(nc, P, G, l):
    xin = G.x if l == 0 else G.xres
    with contextlib.ExitStack() as st:
        C = Ctx()
        alloc_xT(st, nc, C, npt=1)
        C.ident = load_const(P, st, nc, "ident", G.ident, [128, 128], BF16, "ident")
        C.lnp = sb(st, nc, "lnp", [128, 2, D], F32)
        P.dma(C.lnp[:], G.lnp[l][0:2, :].partition_broadcast(128), reads=[], writes=["lnp"])
        C.ln_st = [sb(st, nc, "lnst%d" % i, [128, 24], F32) for i in range(2)]
        C.ln_mv = [sb(st, nc, "lnmv%d" % i, [128, 4], F32) for i in range(2)]
        wo = sb(st, nc, "wo", [128, KC, D], BF16)
        wst = [sb(st, nc, "wost%d" % i, [128, D], F32) for i in range(2)]
        for k in range(KC):
            P.dma(wst[k % 2][:], G.w_out_full[l][k * 128:(k + 1) * 128, :], reads=[], writes=[("wost", k % 2)])
            P.i("pool", "tensor_copy", [("wost", k % 2)], [("wo", k)], out=wo[:, k, :], in_=wst[k % 2][:])
        oTt = [sb(st, nc, "oTt%d" % i, [128, 16, 128], BF16) for i in range(2)]
        xs = [sb(st, nc, "xs%d" % i, [128, D], F32) for i in range(2)]
        z = [sb(st, nc, "z%d" % i, [128, D], F32) for i in range(2)]
        x1 = [sb(st, nc, "x1_%d" % i, [128, D], F32) for i in range(2)]
        hps = [ps(st, nc, "hps%d" % i, [128, 512], F32) for i in range(4)]
        for j in range(NT):
            p = j % 2
            P.dma(oTt[p][:], G.oT.rearrange("h d t -> d h t")[:, :, j * 128:(j + 1) * 128], reads=["oT"], writes=[("oTt", p)])
            P.dma(xs[p][:], xin[j * 128:(j + 1) * 128, :], reads=[], writes=[("xs", p)])
            for n in range(4):
                for h in range(16):
                    P.i("pe", "matmul", [("oTt", p), ("wo", h)], [("hps", n)], hps[n][:], lhsT=oTt[p][:, h, :],
                        rhs=wo[:, h, n * 512:(n + 1) * 512], start=(h == 0), stop=(h == 15))
                P.i("dve", "scalar_tensor_tensor", [("hps", n), ("xs", p)], [("z", p)], out=z[p][:, n * 512:(n + 1) * 512],
                    in0=xs[p][:, n * 512:(n + 1) * 512], scalar=ALPHA, in1=hps[n][:], op0=ALU.mult, op1=ALU.add)
            emit_ln(P, C, z[p][:], ("z", p), 0, x1[p][:], ("x1", p), j)
            P.dma(G.xres[j * 128:(j + 1) * 128, :], x1[p][:], reads=[("x1", p)], writes=["xres"])
            emit_xT(P, C, x1[p][:], ("x1", p), j, G.x1T_own, "x1T")
        P.emit()
    src = G.x1T_own.rearrange("k p t -> (k p) t")
    for k in range(8):
        P.collective("AllGather", ALU.bypass, QUADS, src[k * 256:(k + 1) * 256, :],
                     G.ag1[k].rearrange("r t c -> (r t) c"), reads=[], writes=[("ag1", k)])
    P.emit()
    for k in range(8):
        for hh in range(2):
            P.collective("AllGather", ALU.bypass, PAIRS_C, G.ag1[k, 2 * hh:2 * hh + 2].rearrange("r t c -> (r t) c"),
                         G.x1T_all[k, hh].rearrange("c r t d -> (c r t) d"), reads=[], writes=[("agC", k, hh)])
    P.emit()


def phase_moe(nc, P, G, l):
    with contextlib.ExitStack() as st:
        wgu = sb(st, nc, "wgu", [128, KC, D], BF16)
        wdn = sb(st, nc, "wdn", [128, 8, D], BF16)
        wst = [sb(st, nc, "mwst%d" % i, [128, D], F32) for i in range(2)]
        xc = [sb(st, nc, "xc%d" % i, [128, KC, 512], BF16) for i in range(2)]
        actT = sb(st, nc, "actT", [128, 8, 512], BF16)
        tg = [sb(st, nc, "tg%d" % i, [128, 512], F32) for i in range(2)]
        tsg = [sb(st, nc, "tsg%d" % i, [128, 512], F32) for i in range(2)]
        tl = [sb(st, nc, "tl%d" % i, [128, 512], F32) for i in range(2)]
        ysb = [sb(st, nc, "ysb%d" % i, [128, D], F32) for i in range(2)]
        ybf = [sb(st, nc, "ybf%d" % i, [128, D], BF16) for i in range(2)]
        tmp = [sb(st, nc, "ytmp%d" % i, [128, 512], F32) for i in range(2)]
        bdn = sb(st, nc, "bdn", [128, D], F32)
        bgu = sb(st, nc, "bgu", [128, 16], F32)
        rwst = sb(st, nc, "rwst", [128, KC, 32], F32)
        rw = sb(st, nc, "rw", [128, KC, 32], BF16)
        rb = sb(st, nc, "rb", [128, 4, 32], F32)
        Gsb = sb(st, nc, "Gsb", [128, 128, 4], F32)
        lg = [sb(st, nc, "lg%d" % i, [128, 4, 32], F32) for i in range(2)]
        ex = [sb(st, nc, "ex%d" % i, [128, 32], F32) for i in range(2)]
        sel = [sb(st, nc, "sel%d" % i, [128, 32], F32) for i in range(2)]
        rsm = [sb(st, nc, "rsm%d" % i, [128, 16], F32) for i in range(2)]
        hg = [ps(st, nc, "hg%d" % i, [128, 512], F32) for i in range(2)]
        hl = [ps(st, nc, "hl%d" % i, [128, 512], F32) for i in range(2)]
        yp = [ps(st, nc, "yp%d" % i, [128, 512], F32) for i in range(3)]
        prt = ps(st, nc, "prt", [128, 512], F32)
        cn = {"w": 0, "h": 0, "y": 0, "t": 0, "r": 0}
        P.dma(rwst[:], G.rw[l].rearrange("(k p) e -> p k e", p=128), reads=[], writes=["rwst"])
        P.i("pool", "tensor_copy", ["rwst"], ["rw"], out=rw[:], in_=rwst[:])
        for t in range(4):
            P.dma(rb[:, t, :], G.rb[l][0:1, :].broadcast_to([128, 32]), reads=[], writes=["rb"])

        def load_expert(e):
            for k in range(KC):
                w_ = cn["w"] % 2
                cn["w"] += 1
                P.dma(wst[w_][:], G.wgu[l][e, k * 128:(k + 1) * 128, :], reads=[], writes=[("mwst", w_)])
                P.i("pool", "tensor_copy", [("mwst", w_)], [("wgu", k)], out=wgu[:, k, :], in_=wst[w_][:])
            for k in range(8):
                w_ = cn["w"] % 2
                cn["w"] += 1
                P.dma(wst[w_][:], G.wdn[l][e, k * 128:(k + 1) * 128, :], reads=[], writes=[("mwst", w_)])
                P.i("pool", "tensor_copy", [("mwst", w_)], [("wdn", k)], out=wdn[:, k, :], in_=wst[w_][:])
            P.dma(bdn[:], G.bdn[l][e:e + 1, :].broadcast_to([128, D]), reads=[], writes=["bdn"])
            P.dma(bgu[:], G.bgu[l][e], reads=[], writes=["bgu"])

        def x_src(tc):
            s_ = tc // 4
            lc = tc % 4
            rl, hh, rc = s_ & 1, (s_ >> 1) & 1, (s_ >> 2) & 1
            return [(G.x1T_all[k, hh, rc, rl].rearrange("(kk p) t -> p kk t", p=128)[:, :, lc * 512:(lc + 1) * 512], k)
                    for k in range(8)]

        def load_x(tc):
            b = tc % 2
            for (ap, k) in x_src(tc):
                P.dma(xc[b][:, 2 * k:2 * k + 2, :], ap, reads=[], writes=[("xc", b)])

        for e in range(4):
            load_expert(e)
            load_x(0)
            for tc in range(32):
                if tc + 1 < 32:
                    load_x(tc + 1)
                b = tc % 2
                s_ = tc // 4
                lc = tc % 4
                if e == 0:
                    r_ = cn["r"] % 2
                    cn["r"] += 1
                    for t in range(4):
                        for k in range(KC):
                            P.i("pe", "matmul", [("xc", b), "rw"], ["prt"], prt[:, t * 32:(t + 1) * 32],
                                lhsT=xc[b][:, k, t * 128:(t + 1) * 128], rhs=rw[:, k, :], start=(k == 0), stop=(k == KC - 1))
                    P.i("dve", "tensor_tensor", ["prt", "rb"], [("lg", r_)], out=lg[r_][:].rearrange("p t e -> p (t e)"),
                        in0=prt[:, 0:128], in1=rb[:].rearrange("p t e -> p (t e)"), op=ALU.add)
                    for t in range(4):
                        ti = tc * 4 + t
                        q_ = ti % 2
                        P.i("dve", "max", [("lg", r_)], [("rsm", q_)], out=rsm[q_][:, 0:8], in_=lg[r_][:, t, :])
                        P.i("dve", "tensor_scalar", [("rsm", q_)], [("rsm", q_)], out=rsm[q_][:, 8:9], in0=rsm[q_][:, 0:1],
                            scalar1=-1.0, scalar2=None, op0=ALU.mult)
                        P.i("act", "activation", [("lg", r_), ("rsm", q_)], [("ex", q_)], out=ex[q_][:], in_=lg[r_][:, t, :],
                            func=AF.Exp, bias=rsm[q_][:, 8:9], scale=1.0)
                        P.i("dve", "tensor_scalar", [("lg", r_), ("rsm", q_)], [("sel", q_)], out=sel[q_][:], in0=lg[r_][:, t, :],
                            scalar1=rsm[q_][:, 3:4], scalar2=None, op0=ALU.is_ge)
                        P.i("dve", "tensor_tensor", [("sel", q_), ("ex", q_)], [("sel", q_)], out=sel[q_][:], in0=sel[q_][:],
                            in1=ex[q_][:], op=ALU.mult)
                        P.i("dve", "tensor_reduce", [("sel", q_)], [("rsm", q_)], out=rsm[q_][:, 9:10], in_=sel[q_][:],
                            axis=AX.X, op=ALU.add)
                        P.i("dve", "reciprocal", [("rsm", q_)], [("rsm", q_)], out=rsm[q_][:, 9:10], in_=rsm[q_][:, 9:10])
                        P.i("dve", "tensor_scalar", [("sel", q_), ("rsm", q_)], [("Gsb", ti)], out=Gsb[:, ti, :],
                            in0=sel[q_][:, 0:4], scalar1=rsm[q_][:, 9:10], scalar2=None, op0=ALU.mult)
                for fo in range(8):
                    hb_ = cn["h"] % 2
                    cn["h"] += 1
                    for k in range(KC):
                        P.i("pe", "matmul", [("xc", b), ("wgu", k)], [("hg", hb_)], hg[hb_][:],
                            lhsT=wgu[:, k, fo * 128:(fo + 1) * 128], rhs=xc[b][:, k, :], start=(k == 0), stop=(k == KC - 1))
                    for k in range(KC):
                        P.i("pe", "matmul", [("xc", b), ("wgu", k)], [("hl", hb_)], hl[hb_][:],
                            lhsT=wgu[:, k, 1024 + fo * 128:1024 + (fo + 1) * 128], rhs=xc[b][:, k, :], start=(k == 0),
                            stop=(k == KC - 1))
                    P.i("dve", "tensor_scalar", [("hg", hb_), "bgu"], [("tg", hb_)], out=tg[hb_][:], in0=hg[hb_][:],
                        scalar1=bgu[:, fo:fo + 1], scalar2=7.0, op0=ALU.add, op1=ALU.min)
                    P.i("act", "activation", [("tg", hb_)], [("tsg", hb_)], out=tsg[hb_][:], in_=tg[hb_][:], func=AF.Sigmoid,
                        scale=1.702)
                    P.i("dve", "tensor_scalar", [("hl", hb_), "bgu"], [("tl", hb_)], out=tl[hb_][:], in0=hl[hb_][:],
                        scalar1=bgu[:, 8 + fo:9 + fo], scalar2=7.0, op0=ALU.add, op1=ALU.min)
                    P.i("pool", "tensor_scalar", [("tl", hb_)], [("tl", hb_)], out=tl[hb_][:], in0=tl[hb_][:], scalar1=-7.0,
                        scalar2=1.0, op0=ALU.max, op1=ALU.add)
                    P.i("pool", "tensor_tensor", [("tg", hb_), ("tsg", hb_)], [("tg", hb_)], out=tg[hb_][:], in0=tg[hb_][:],
                        in1=tsg[hb_][:], op=ALU.mult)
                    P.i("pool", "tensor_tensor", [("tg", hb_), ("tl", hb_)], [("act", fo)], out=actT[:, fo, :], in0=tg[hb_][:],
                        in1=tl[hb_][:], op=ALU.mult)
                for t in range(4):
                    ti = tc * 4 + t
                    yb_ = cn["t"] % 2
                    cn["t"] += 1
                    row = slice(s_ * TOK + lc * 512 + t * 128, s_ * TOK + lc * 512 + (t + 1) * 128)
                    if e > 0:
                        P.dma(ysb[yb_][:], G.yacc[row, :], reads=[("yacc", ti)], writes=[("ysb", yb_)])
                    for n in range(4):
                        y_ = cn["y"] % 3
                        cn["y"] += 1
                        for fc in range(8):
                            P.i("pe", "matmul", [("act", fc), ("wdn", fc)], [("yp", y_)], yp[y_][:],
                                lhsT=actT[:, fc, t * 128:(t + 1) * 128], rhs=wdn[:, fc, n * 512:(n + 1) * 512],
                                start=(fc == 0), stop=(fc == 7))
                        tm = tmp[cn["y"] % 2]
                        tk = ("ytmp", cn["y"] % 2)
                        P.i("dve", "tensor_tensor", [("yp", y_), "bdn"], [tk], out=tm[:], in0=yp[y_][:],
                            in1=bdn[:, n * 512:(n + 1) * 512], op=ALU.add)
                        if e == 0:
                            P.i("dve", "tensor_scalar", [tk, ("Gsb", ti)], [("ysb", yb_)], out=ysb[yb_][:, n * 512:(n + 1) * 512],
                                in0=tm[:], scalar1=Gsb[:, ti, e:e + 1], scalar2=None, op0=ALU.mult)
                        else:
                            P.i("dve", "scalar_tensor_tensor", [tk, ("Gsb", ti), ("ysb", yb_)], [("ysb", yb_)],
                                out=ysb[yb_][:, n * 512:(n + 1) * 512], in0=tm[:], scalar=Gsb[:, ti, e:e + 1],
                                in1=ysb[yb_][:, n * 512:(n + 1) * 512], op0=ALU.mult, op1=ALU.add)
                    if e < 3:
                        P.dma(G.yacc[row, :], ysb[yb_][:], reads=[("ysb", yb_)], writes=[("yacc", ti)])
                    else:
                        P.i("act", "activation", [("ysb", yb_)], [("ybf", yb_)], out=ybf[yb_][:], in_=ysb[yb_][:], func=AF.Copy)
                        o4, oc = s_ % 4, s_ >> 2
                        tt = lc * 4 + t
                        P.dma(G.ypart[o4, tt // 4, oc, (tt % 4) * 128:(tt % 4 + 1) * 128, :], ybf[yb_][:],
                              reads=[("ybf", yb_)], writes=["ypart"])
        P.emit()
    for o4 in range(4):
        for q4 in range(4):
            P.collective("ReduceScatter", ALU.add, PAIRS_C, G.ypart[o4, q4].rearrange("c t d -> (c t) d"),
                         G.y2[q4, o4], reads=[], writes=[("y2", q4, o4)])
    P.emit()
    for q4 in range(4):
        P.collective("ReduceScatter", ALU.add, QUADS, G.y2[q4].rearrange("c t d -> (c t) d"),
                     G.yown[q4 * 512:(q4 + 1) * 512, :], reads=[], writes=[("yown", q4)])
    P.emit()


def phase_ln2(nc, P, G, l, last):
    with contextlib.ExitStack() as st:
        C = Ctx()
        alloc_xT(st, nc, C, npt=1)
        C.ident = load_const(P, st, nc, "ident", G.ident, [128, 128], BF16, "ident")
        C.lnp = sb(st, nc, "lnp", [128, 2, D], F32)
        P.dma(C.lnp[:], G.lnp[l][2:4, :].partition_broadcast(128), reads=[], writes=["lnp"])
        C.ln_st = [sb(st, nc, "lnst%d" % i, [128, 24], F32) for i in range(2)]
        C.ln_mv = [sb(st, nc, "lnmv%d" % i, [128, 4], F32) for i in range(2)]
        xs = [sb(st, nc, "xs%d" % i, [128, D], F32) for i in range(2)]
        yb = [sb(st, nc, "yb%d" % i, [128, D], BF16) for i in range(2)]
        z = [sb(st, nc, "z%d" % i, [128, D], F32) for i in range(2)]
        x2 = [sb(st, nc, "x2_%d" % i, [128, D], F32) for i in range(2)]
        for j in range(NT):
            p = j % 2
            P.dma(xs[p][:], G.xres[j * 128:(j + 1) * 128, :], reads=[("xres", j)], writes=[("xs", p)])
            P.dma(yb[p][:], G.yown[j * 128:(j + 1) * 128, :], reads=[], writes=[("yb", p)])
            P.i("dve", "scalar_tensor_tensor", [("xs", p), ("yb", p)], [("z", p)], out=z[p][:], in0=xs[p][:], scalar=ALPHA,
                in1=yb[p][:], op0=ALU.mult, op1=ALU.add)
            emit_ln(P, C, z[p][:], ("z", p), 0, x2[p][:], ("x2", p), j)
            if last:
                P.dma(G.out[j * 128:(j + 1) * 128, :], x2[p][:], reads=[("x2", p)], writes=["out"])
            else:
                P.dma(G.xres[j * 128:(j + 1) * 128, :], x2[p][:], reads=[("x2", p), ("xs", p)], writes=[("xres", j)])
                emit_xT(P, C, x2[p][:], ("x2", p), j, G.xT, "xT")
        P.emit()


LAST_INPUT_NAMES = []


def build(nlayers=4, debug=None, stop_after=None, moe=True, only_layer=None):
    nc = bass.Bass("TRN2", target_bir_lowering=False)
    G = Ctx()
    G.stop_after = stop_after

    del LAST_INPUT_NAMES[:]

    def inp(name, shape, dt=F32):
        LAST_INPUT_NAMES.append(name)
        return nc.dram_tensor(name, list(shape), dt, kind="ExternalInput").ap()

    def scr(name, shape, dt=F32):
        return nc.dram_tensor(name, list(shape), dt).ap()

    G.x = inp("x", [TOK, D])
    G.ident = inp("ident", [128, 128], BF16)
    G.identf = inp("identf", [128, 128])
    G.cosT = inp("cosT", [128, TOK])
    G.sinT = inp("sinT", [128, TOK])
    G.cos64T = inp("cos64T", [128, TOK])
    G.sin64T = inp("sin64T", [128, TOK])
    G.kcos = inp("kcos", [128, NT, 32])
    G.ksin = inp("ksin", [128, NT, 32])
    G.maskA = inp("maskA", [128, 128])
    G.maskB = inp("maskB", [128, 128])
    G.dilb = inp("dilb", [128, 2, 9 * 128])
    G.w_in_sh, G.w_out_sh, G.w_in_b, G.w_out_b, G.w_in_full, G.w_out_full = {}, {}, {}, {}, {}, {}
    G.lnp, G.rw, G.rb, G.wgu, G.bgu, G.wdn, G.bdn = {}, {}, {}, {}, {}, {}, {}
    G.idx_g, G.idx_b, G.negbf = {}, {}, {}
    for l in range(nlayers):
        if only_layer is not None and l != only_layer:
            continue
        W = W_IN[l % 4]
        G.w_in_full[l] = inp("l%d_w_in" % l, [D, W])
        G.w_out_full[l] = inp("l%d_w_out" % l, [D, D])
        G.lnp[l] = inp("l%d_lnp" % l, [4, D])
        G.rw[l] = inp("l%d_rw" % l, [D, 32])
        G.rb[l] = inp("l%d_rb" % l, [1, 32])
        if moe:
            G.wgu[l] = inp("l%d_wgu" % l, [4, D, D])
            G.bgu[l] = inp("l%d_bgu" % l, [4, 128, 16])
            G.wdn[l] = inp("l%d_wdn" % l, [4, 1024, D])
            G.bdn[l] = inp("l%d_bdn" % l, [4, D])
        if l % 4 == 1:
            G.idx_g[l] = inp("l%d_idxg" % l, [128, 64])
            G.idx_b[l] = inp("l%d_idxb" % l, [128, 64])
        if l % 4 == 2:
            G.negbf[l] = inp("l%d_bf" % l, [16, 1])
    G.out = nc.dram_tensor("out", [TOK, D], F32, kind="ExternalOutput").ap()
    G.xres = scr("xres", [TOK, D])
    G.xT = scr("xT", [KC, 128, TOK], BF16)
    G.qT = scr("qT", [16, 128, TOK], BF16)
    G.kT_own = scr("kT_own", [16, 128, TOK], BF16)
    G.kT_all = scr("kT_all", [4, 2, 4, 128, TOK], BF16)
    G.v_own = scr("v_own", [TOK, D], BF16)
    G.v_all = scr("v_all", [4, 2, 512, D], BF16)
    G.v_own4 = scr("v_own4", [TOK, 512], BF16)
    G.v_all4 = scr("v_all4", [2, TOK, 512], BF16)
    G.qiT = scr("qiT", [8, 128, TOK], BF16)
    G.kiT_own = scr("kiT_own", [128, TOK], BF16)
    G.kiT_all = scr("kiT_all", [2, 128, TOK], BF16)
    G.wi = scr("wi", [TOK, 16])
    G.sp_own = scr("sp_own", [16, TOK])
    G.sp_all = scr("sp_all", [2, 16, TOK])
    G.negck = scr("negck", [16, 2 * TOK])
    G.dsab = scr("dsab", [NT, 128, 2 * TOK], BF16)
    G.oT = scr("oT", [16, 128, TOK], BF16)
    G.x1T_own = scr("x1T_own", [KC, 128, TOK], BF16)
    G.x1T_all = scr("x1T_allc", [8, 2, 2, 2, 256, TOK], BF16)
    G.ag1 = scr("ag1", [8, 4, 256, TOK], BF16)
    G.yacc = scr("yacc", [8 * TOK, D])
    G.ypart = scr("ypart", [4, 4, 2, 512, D], BF16)
    G.y2 = scr("y2", [4, 4, 512, D], BF16)
    G.yown = scr("yown", [TOK, D], BF16)
    dbg = {}
    if debug:
        for name, shape, dt in debug:
            dbg[name] = nc.dram_tensor("dbg_" + name, list(shape), dt, kind="ExternalOutput").ap()
    G.dbg = dbg

    with contextlib.ExitStack() as st:
        P = Prog(nc, st)
        sems = P.all_sems()
        with nc.Block() as block:
            @block.gpsimd
            def _(e):
                for s in sems:
                    e.sem_clear(s)
        phase_prep(nc, P, G)
        for l in range(nlayers):
            if stop_after in (("weights",), ("prep",)):
                break
            if only_layer is not None and l != only_layer:
                continue
            phase_proj(nc, P, G, l)
            if G.stop_after == ("proj", l):
                break
            if l % 4 == 1:
                phase_dsa_index(nc, P, G, l)
            if l % 4 == 2:
                phase_fox_cum(nc, P, G, l)
            phase_attn(nc, P, G, l)
            if G.stop_after == ("attn", l):
                break
            phase_out_ln1(nc, P, G, l)
            if G.stop_after == ("ln1", l):
                break
            phase_moe(nc, P, G, l)
            if G.stop_after == ("moe", l):
                break
            phase_ln2(nc, P, G, l, l == nlayers - 1)
        for name, ap in dbg.items():
            src = G.w_out_full[0] if name == "w_out_full0" else getattr(G, name)
            n0 = src.shape[0]
            flat_s = src if len(src.shape) == 2 else src.rearrange(
                {3: "a b c -> (a b) c", 4: "a b c d -> (a b c) d", 5: "a b c d e -> (a b c d) e"}[len(src.shape)])
            flat_d = ap if len(ap.shape) == 2 else ap.rearrange(
                {3: "a b c -> (a b) c", 4: "a b c d -> (a b c) d", 5: "a b c d e -> (a b c d) e"}[len(ap.shape)])
            P.dma(flat_d, flat_s, reads=[], writes=[("dbg", name)])
        fin = sb(st, nc, "fin", [128, 8], F32)
        P.i("dve", "memset", [("dbg", n) for n in dbg] + ["out"], ["fin"], fin[:], 0.0)
        P.emit()
    return nc


def _consts(p):
    t = np.arange(TOK)
    pos = ((2 * (t // 128) + p) * 128 + (t % 128)).astype(np.float32)
    c = {}
    c["ident"] = np.eye(128, dtype=np.float32).astype(ml_dtypes.bfloat16)
    c["identf"] = np.eye(128, dtype=np.float32)
    d = np.arange(128)
    inv = (10000.0 ** (-(np.arange(64, dtype=np.float32)) / 64)).astype(np.float32)
    ang = pos[None, :] * inv[d % 64][:, None]
    c["cosT"] = np.cos(ang).astype(np.float32)
    c["sinT"] = (np.sin(ang) * np.where(d < 64, -1.0, 1.0)[:, None]).astype(np.float32)
    inv32 = (10000.0 ** (-(np.arange(32, dtype=np.float32)) / 32)).astype(np.float32)
    ang = pos[None, :] * inv32[d % 32][:, None]
    c["cos64T"] = np.cos(ang).astype(np.float32)
    c["sin64T"] = (np.sin(ang) * np.where((d % 64) < 32, -1.0, 1.0)[:, None]).astype(np.float32)
    angk = pos.reshape(NT, 128).T[:, :, None] * inv32[None, None, :]
    c["kcos"] = np.cos(angk).astype(np.float32)
    c["ksin"] = np.sin(angk).astype(np.float32)
    qi = np.arange(128)[:, None]
    ki = np.arange(128)[None, :]
    tri = np.where(ki <= qi, 0.0, NEG).astype(np.float32)
    allneg = np.full((128, 128), NEG, np.float32)
    zer = np.zeros((128, 128), np.float32)
    c["maskA"] = tri if p == 0 else zer
    c["maskB"] = allneg if p == 0 else tri
    dil = np.zeros((128, 2, 9, 128), np.float32)
    for r in range(2):
        for kp in range(9):
            m = 2 * (8 - kp) + p - r
            dd = m * 128 + qi - ki
            cnt = ((dd >= 0) & (dd <= 128)).astype(np.int32) + ((dd >= 0) & (dd <= 512) & (dd % 4 == 0)) \
                + ((dd >= 0) & (dd <= 2048) & (dd % 16 == 0))
            dil[:, r, kp, :] = np.where(cnt > 0, np.log(np.maximum(cnt, 1)), NEG)
    c["dilb"] = dil.reshape(128, 2, 9 * 128)
    return c


def make_in_maps(inputs, nlayers=4):
    maps = []
    x = np.asarray(inputs["x"])
    for c in range(NCORES):
        b, p = c // 2, c % 2
        m = dict(_consts(p))
        m["x"] = np.ascontiguousarray(x[b].reshape(16, 2, 128, D)[:, p].reshape(TOK, D))
        for l in range(nlayers):
            pre = "l%d_" % l
            m[pre + "w_in"] = np.asarray(inputs[pre + "w_in"])
            m[pre + "w_out"] = np.asarray(inputs[pre + "w_out"])
            m[pre + "lnp"] = np.stack([inputs[pre + "ln1_g"], inputs[pre + "ln1_b"],
                                       inputs[pre + "ln2_g"], inputs[pre + "ln2_b"]]).astype(np.float32)
            perm = list(range(4 * c, 4 * c + 4)) + [e for e in range(32) if not (4 * c <= e < 4 * c + 4)]
            m[pre + "rw"] = np.ascontiguousarray(np.asarray(inputs[pre + "router_w"])[:, perm])
            m[pre + "rb"] = np.ascontiguousarray(np.asarray(inputs[pre + "router_b"])[perm][None, :])
            m[pre + "wgu"] = np.ascontiguousarray(inputs[pre + "w_gu"][4 * c:4 * c + 4])
            m[pre + "bgu"] = np.ascontiguousarray(np.asarray(inputs[pre + "b_gu"][4 * c:4 * c + 4]).reshape(4, 16, 128).transpose(0, 2, 1))
            m[pre + "wdn"] = np.ascontiguousarray(inputs[pre + "w_dn"][4 * c:4 * c + 4])
            m[pre + "bdn"] = np.ascontiguousarray(inputs[pre + "b_dn"][4 * c:4 * c + 4])
            if l % 4 == 1:
                m[pre + "idxg"] = np.ascontiguousarray(np.broadcast_to(np.asarray(inputs[pre + "idx_norm_g"])[None, :], (128, 64)))
                m[pre + "idxb"] = np.ascontiguousarray(np.broadcast_to(np.asarray(inputs[pre + "idx_norm_b"])[None, :], (128, 64)))
            if l % 4 == 2:
                m[pre + "bf"] = np.ascontiguousarray(np.asarray(inputs[pre + "b_forget"])[:, None])
        maps.append(m)
    return maps


def kernel(**inputs):
    nc = build(4)
    maps = make_in_maps(inputs, 4)
    res = run_bass_kernel_spmd(nc, maps, core_ids=list(range(NCORES)))
    out = np.zeros((4, 4096, D), np.float32)
    for c in range(NCORES):
        b, p = c // 2, c % 2
        out[b].reshape(16, 2, 128, D)[:, p] = res.results[c]["out"].reshape(16, 128, D)
    return out
```
